# Optimizing a Trainium2 kernel written in Bass

```python
import jax
import jax.numpy as jnp
from jax import lax
import numpy as np

D_MODEL = 2048
BATCH = 4
SEQ = 4096
DEPTH = 1
DEC_BATCH = 16
DEC_SEQ = 16
PAST_LEN = 4096

CHUNK = 64
Q_BLOCK = 128
EPS = 1e-6

MLA_HEADS = D_MODEL // 128
Q_LORA = 512
KV_LORA = 512
NOPE_DIM = 128
ROPE_DIM = 64
V_DIM = 128
ROPE_BASE = 10000.0
ATTN_SCALE = (NOPE_DIM + ROPE_DIM) ** -0.5

GDN_HEADS = D_MODEL // 128
GDN_DK = 128
GDN_DV = 128
CONV_W = 4
GDN_QKV = GDN_HEADS * (2 * GDN_DK + GDN_DV)

N_GROUPS = 8
EXPERTS_PER_GROUP = 8
N_EXPERTS = N_GROUPS * EXPERTS_PER_GROUP
TOP_K = 2
D_EXPERT = 512
MOE_BLOCK = 128

IN_SPLITS = (Q_LORA, KV_LORA, ROPE_DIM, GDN_QKV, GDN_HEADS, GDN_HEADS, GDN_HEADS * GDN_DV, D_MODEL, D_MODEL)
IN_TOTAL = Q_LORA + KV_LORA + ROPE_DIM + GDN_QKV + 2 * GDN_HEADS + GDN_HEADS * GDN_DV + 2 * D_MODEL

kernel_name = 'hybrid_mla_gdn_hmoe_stream_step'


def rmsnorm(x, g):
    xf = x.astype(jnp.float32)
    y = xf * lax.rsqrt(jnp.mean(xf * xf, axis=-1, keepdims=True) + EPS)
    return (y * g.astype(jnp.float32)).astype(x.dtype)


def l2norm(x):
    xf = x.astype(jnp.float32)
    return xf * lax.rsqrt(jnp.sum(xf * xf, axis=-1, keepdims=True) + EPS)


def rope(x, pos):
    half = ROPE_DIM // 2
    inv = ROPE_BASE ** (-jnp.arange(half, dtype=jnp.float32) / half)
    ang = pos.astype(jnp.float32)[:, None] * inv[None, :]
    shp = (ang.shape[0],) + (1,) * (x.ndim - 3) + (half,)
    cos = jnp.cos(ang).reshape(shp)
    sin = jnp.sin(ang).reshape(shp)
    x1 = x[..., :half].astype(jnp.float32)
    x2 = x[..., half:].astype(jnp.float32)
    return jnp.concatenate([x1 * cos - x2 * sin, x2 * cos + x1 * sin], axis=-1).astype(x.dtype)


def split_in(u):
    cuts, acc = [], 0
    for size in IN_SPLITS[:-1]:
        acc += size
        cuts.append(acc)
    return jnp.split(u, cuts, axis=-1)


def mla_project(q_lat, kv_lat, k_r, pos, q_norm_g, w_uq, kv_norm_g):
    q = jnp.einsum('bsc,chd->bshd', rmsnorm(q_lat, q_norm_g), w_uq)
    q_nope = q[..., :NOPE_DIM]
    q_rope = rope(q[..., NOPE_DIM:], pos)
    ckv = rmsnorm(kv_lat, kv_norm_g)
    kr = rope(k_r, pos)
    return q_nope, q_rope, ckv, kr


def mla_prompt(q_nope, q_rope, ckv, kr, pos, w_uk, w_uv):
    b, s = ckv.shape[:2]
    k_nope = jnp.einsum('bkc,chd->bkhd', ckv, w_uk)
    v = jnp.einsum('bkc,chd->bkhd', ckv, w_uv)
    key_chunk = pos // CHUNK
    nb = s // Q_BLOCK

    def blocks(t):
        return jnp.moveaxis(t.reshape((b, nb, Q_BLOCK) + t.shape[2:]), 1, 0)

    def attend(args):
        qn, qr, qc = args
        sc = (jnp.einsum('bqhd,bkhd->bhqk', qn, k_nope, preferred_element_type=jnp.float32)
              + jnp.einsum('bqhr,bkr->bhqk', qr, kr, preferred_element_type=jnp.float32)) * ATTN_SCALE
        sc = jnp.where(key_chunk[None, :] <= (qc // CHUNK)[:, None], sc, -jnp.inf)
        p = jax.nn.softmax(sc, axis=-1).astype(v.dtype)
        return jnp.einsum('bhqk,bkhd->bqhd', p, v)

    o = lax.map(attend, (blocks(q_nope), blocks(q_rope), pos.reshape(nb, Q_BLOCK)))
    return jnp.moveaxis(o, 0, 1).reshape(b, s, MLA_HEADS * V_DIM)


def mla_sample(q_nope, q_rope, ckv, kr, pos, ckv_past, kr_past, w_uk, w_uv):
    b, s = ckv.shape[:2]
    past = ckv_past.shape[1]
    q_abs = jnp.einsum('bqhd,chd->bqhc', q_nope, w_uk)
    s_past = (jnp.einsum('bqhc,bkc->bhqk', q_abs, ckv_past, preferred_element_type=jnp.float32)
              + jnp.einsum('bqhr,bkr->bhqk', q_rope, kr_past, preferred_element_type=jnp.float32)) * ATTN_SCALE
    s_new = (jnp.einsum('bqhc,bkc->bhqk', q_abs, ckv, preferred_element_type=jnp.float32)
             + jnp.einsum('bqhr,bkr->bhqk', q_rope, kr, preferred_element_type=jnp.float32)) * ATTN_SCALE
    qc = pos // CHUNK
    s_new = jnp.where(qc[None, :] <= qc[:, None], s_new, -jnp.inf)
    p = jax.nn.softmax(jnp.concatenate([s_past, s_new], axis=-1), axis=-1).astype(ckv.dtype)
    o_lat = (jnp.einsum('bhqk,bkc->bqhc', p[..., :past], ckv_past.astype(ckv.dtype))
             + jnp.einsum('bhqk,bkc->bqhc', p[..., past:], ckv))
    return jnp.einsum('bqhc,chd->bqhd', o_lat, w_uv).reshape(b, s, MLA_HEADS * V_DIM)


def causal_conv(x, conv0, conv_w):
    s = x.shape[1]
    xp = jnp.concatenate([conv0.astype(x.dtype), x], axis=1)
    y = xp[:, 0:s] * conv_w[0]
    for j in range(1, CONV_W):
        y = y + xp[:, j:j + s] * conv_w[j]
    return jax.nn.silu(y), xp[:, s:]


def gdn_chunk(S, q, k, v, g, beta):
    L = q.shape[2]
    G = jnp.cumsum(g, axis=-1)
    tri = jnp.tril(jnp.ones((L, L), dtype=bool))
    strict = jnp.tril(jnp.ones((L, L), dtype=bool), -1)
    diff = G[..., :, None] - G[..., None, :]
    decay = jnp.where(tri, jnp.exp(jnp.where(tri, diff, 0.0)), 0.0)
    kb = k * beta[..., None]
    lmat = jnp.where(strict, jnp.einsum('bhid,bhjd->bhij', kb, k) * decay, 0.0)
    eye = jnp.eye(L, dtype=jnp.float32)
    T = lax.linalg.triangular_solve(eye + lmat, jnp.broadcast_to(eye, lmat.shape),
                                    left_side=True, lower=True, unit_diagonal=True)
    eG = jnp.exp(G)[..., None]
    u = T @ (v * beta[..., None])
    w = T @ (kb * eG)
    v_new = u - w @ S
    attn = jnp.where(tri, jnp.einsum('bhid,bhjd->bhij', q, k) * decay, 0.0)
    o = (q * eG) @ S + attn @ v_new
    gL = G[..., -1]
    S_new = S * jnp.exp(gL)[..., None, None] + jnp.einsum(
        'bhld,bhle->bhde', k * jnp.exp(gL[..., None] - G)[..., None], v_new)
    return S_new, o


def gdn_branch(qkv, a, bb, z, S0, conv0, conv_w, a_log, dt_bias, gdn_norm_g):
    b, s, _ = qkv.shape
    qkv_c, conv_new = causal_conv(qkv, conv0, conv_w)
    q, k, v = jnp.split(qkv_c, [GDN_HEADS * GDN_DK, 2 * GDN_HEADS * GDN_DK], axis=-1)
    q = l2norm(q.reshape(b, s, GDN_HEADS, GDN_DK)) * (GDN_DK ** -0.5)
    k = l2norm(k.reshape(b, s, GDN_HEADS, GDN_DK))
    v = v.reshape(b, s, GDN_HEADS, GDN_DV).astype(jnp.float32)
    g = -jnp.exp(a_log.astype(jnp.float32)) * jax.nn.softplus(a.astype(jnp.float32) + dt_bias.astype(jnp.float32))
    beta = jax.nn.sigmoid(bb.astype(jnp.float32))
    L = min(s, CHUNK)
    n = s // L

    def to_chunks(t):
        t = t.reshape((b, n, L) + t.shape[2:])
        return jnp.moveaxis(t, (1, 3), (0, 2))

    S_new, o = lax.scan(lambda S, inp: gdn_chunk(S, *inp), S0.astype(jnp.float32),
                        (to_chunks(q), to_chunks(k), to_chunks(v), to_chunks(g), to_chunks(beta)))
    o = jnp.moveaxis(o, (0, 2), (1, 3)).reshape(b, s, GDN_HEADS, GDN_DV)
    zf = z.reshape(b, s, GDN_HEADS, GDN_DV).astype(jnp.float32)
    o = rmsnorm(o, gdn_norm_g) * jax.nn.silu(zf)
    return o.reshape(b, s, GDN_HEADS * GDN_DV).astype(qkv.dtype), S_new, conv_new


def route(xt, w_group, b_group, w_router, b_router):
    xf = xt.astype(jnp.float32)
    pg = jax.nn.softmax(xf @ w_group.astype(jnp.float32) + b_group.astype(jnp.float32), axis=-1)
    pg_top, grp = lax.top_k(pg, 1)
    le = jnp.einsum('nd,gde->nge', xf, w_router.astype(jnp.float32)) + b_router.astype(jnp.float32)
    le_sel = jnp.take_along_axis(le, grp[:, :, None], axis=1)[:, 0]
    top_v, top_i = lax.top_k(le_sel, TOP_K)
    weight = pg_top * jax.nn.softmax(top_v, axis=-1)
    expert = grp * EXPERTS_PER_GROUP + top_i
    return expert, weight


def moe_ffn(h, w_group, b_group, w_router, b_router, w_gate, w_up, w_down):
    b, s, d = h.shape
    xt = h.reshape(-1, d)
    n = xt.shape[0]
    expert, weight = route(xt, w_group, b_group, w_router, b_router)
    a = n * TOP_K
    e_flat = expert.reshape(-1)
    w_flat = weight.reshape(-1)
    tok = jnp.repeat(jnp.arange(n, dtype=jnp.int32), TOP_K)
    order = jnp.argsort(e_flat)
    e_s, tok_s, w_s = e_flat[order], tok[order], w_flat[order]
    counts = jnp.zeros((N_EXPERTS,), jnp.int32).at[e_flat].add(1)
    start = jnp.cumsum(counts) - counts
    padded = (counts + MOE_BLOCK - 1) // MOE_BLOCK * MOE_BLOCK
    pend = jnp.cumsum(padded)
    pstart = pend - padded
    dest = pstart[e_s] + (jnp.arange(a, dtype=jnp.int32) - start[e_s])
    n_blocks = (a + N_EXPERTS * (MOE_BLOCK - 1) + MOE_BLOCK - 1) // MOE_BLOCK
    n_slots = n_blocks * MOE_BLOCK
    slot_tok = jnp.full((n_slots,), n, jnp.int32).at[dest].set(tok_s)
    slot_w = jnp.zeros((n_slots,), h.dtype).at[dest].set(w_s.astype(h.dtype))
    block_e = jnp.minimum(jnp.searchsorted(pend, jnp.arange(n_blocks, dtype=jnp.int32) * MOE_BLOCK, side='right'),
                          N_EXPERTS - 1)
    x_pad = jnp.concatenate([xt, jnp.zeros((1, d), xt.dtype)], axis=0)

    def run_block(args):
        tok_b, w_b, e = args
        xb = x_pad[tok_b]
        hid = jax.nn.silu(xb @ w_gate[e]) * (xb @ w_up[e])
        return (hid @ w_down[e]) * w_b[:, None]

    out = lax.map(run_block, (slot_tok.reshape(n_blocks, MOE_BLOCK), slot_w.reshape(n_blocks, MOE_BLOCK), block_e))
    y = jnp.zeros((n + 1, d), h.dtype).at[slot_tok].add(out.reshape(n_slots, d))
    return y[:n].reshape(b, s, d)


def layer(x, pos, ckv_past, kr_past, S0, conv0, lw):
    (attn_norm_g, w_in, q_norm_g, w_uq, kv_norm_g, w_uk, w_uv, conv_w, a_log, dt_bias, gdn_norm_g, w_out,
     ffn_norm_g, w_group, b_group, w_router, b_router, w_gate, w_up, w_down) = lw
    h = rmsnorm(x, attn_norm_g)
    u = h @ w_in
    q_lat, kv_lat, k_r, qkv, a, bb, z, gate_a, gate_b = split_in(u)
    q_nope, q_rope, ckv, kr = mla_project(q_lat, kv_lat, k_r, pos, q_norm_g, w_uq, kv_norm_g)
    if ckv_past is None:
        o_a = mla_prompt(q_nope, q_rope, ckv, kr, pos, w_uk, w_uv)
    else:
        o_a = mla_sample(q_nope, q_rope, ckv, kr, pos, ckv_past, kr_past, w_uk, w_uv)
    o_b, S_new, conv_new = gdn_branch(qkv, a, bb, z, S0, conv0, conv_w, a_log, dt_bias, gdn_norm_g)
    merged = jax.nn.sigmoid(gate_a) * o_a + jax.nn.sigmoid(gate_b) * o_b
    x = x + merged @ w_out
    x = x + moe_ffn(rmsnorm(x, ffn_norm_g), w_group, b_group, w_router, b_router, w_gate, w_up, w_down)
    return x, ckv, kr, S_new, conv_new


def setup_inputs(seed: int = 0) -> dict:
    key = jax.random.key(seed)
    ks = jax.random.split(key, 32)
    f32 = jnp.float32
    L = DEPTH

    def nrm(k, shape, scale):
        return jax.random.normal(k, shape, f32) * scale

    def gain(k, shape):
        return 1.0 + 0.01 * jax.random.normal(k, shape, f32)

    dt = jnp.exp(jax.random.uniform(ks[14], (L, GDN_HEADS), f32, np.log(1e-3), np.log(1e-1)))
    return {
        'x_prompt': nrm(ks[0], (BATCH, SEQ, D_MODEL), 1.0),
        'x_sample': nrm(ks[1], (DEC_BATCH, DEC_SEQ, D_MODEL), 1.0),
        'cache_ckv': nrm(ks[2], (L, DEC_BATCH, PAST_LEN, KV_LORA), 1.0),
        'cache_k_rope': nrm(ks[3], (L, DEC_BATCH, PAST_LEN, ROPE_DIM), 1.0),
        'state_gdn': nrm(ks[4], (L, DEC_BATCH, GDN_HEADS, GDN_DK, GDN_DV), 0.1),
        'state_conv': nrm(ks[5], (L, DEC_BATCH, CONV_W - 1, GDN_QKV), 1.0),
        'attn_norm_g': gain(ks[6], (L, D_MODEL)),
        'w_in': nrm(ks[7], (L, D_MODEL, IN_TOTAL), D_MODEL ** -0.5),
        'q_norm_g': gain(ks[8], (L, Q_LORA)),
        'w_uq': nrm(ks[9], (L, Q_LORA, MLA_HEADS, NOPE_DIM + ROPE_DIM), Q_LORA ** -0.5),
        'kv_norm_g': gain(ks[10], (L, KV_LORA)),
        'w_uk': nrm(ks[11], (L, KV_LORA, MLA_HEADS, NOPE_DIM), KV_LORA ** -0.5),
        'w_uv': nrm(ks[12], (L, KV_LORA, MLA_HEADS, V_DIM), KV_LORA ** -0.5),
        'conv_w': nrm(ks[13], (L, CONV_W, GDN_QKV), CONV_W ** -0.5),
        'a_log': jnp.log(jax.random.uniform(ks[15], (L, GDN_HEADS), f32, 1.0, 16.0)),
        'dt_bias': dt + jnp.log(-jnp.expm1(-dt)),
        'gdn_norm_g': gain(ks[16], (L, GDN_DV)),
        'w_out': nrm(ks[17], (L, D_MODEL, D_MODEL), D_MODEL ** -0.5),
        'ffn_norm_g': gain(ks[18], (L, D_MODEL)),
        'w_group': nrm(ks[19], (L, D_MODEL, N_GROUPS), D_MODEL ** -0.5),
        'b_group': nrm(ks[20], (L, N_GROUPS), 0.01),
        'w_router': nrm(ks[21], (L, N_GROUPS, D_MODEL, EXPERTS_PER_GROUP), D_MODEL ** -0.5),
        'b_router': nrm(ks[22], (L, N_GROUPS, EXPERTS_PER_GROUP), 0.01),
        'w_gate': nrm(ks[23], (L, N_EXPERTS, D_MODEL, D_EXPERT), D_MODEL ** -0.5),
        'w_up': nrm(ks[24], (L, N_EXPERTS, D_MODEL, D_EXPERT), D_MODEL ** -0.5),
        'w_down': nrm(ks[25], (L, N_EXPERTS, D_EXPERT, D_MODEL), D_EXPERT ** -0.5),
        'final_norm_g': gain(ks[26], (D_MODEL,)),
    }


def reference(x_prompt, x_sample, cache_ckv, cache_k_rope, state_gdn, state_conv,
              attn_norm_g, w_in, q_norm_g, w_uq, kv_norm_g, w_uk, w_uv, conv_w, a_log, dt_bias, gdn_norm_g, w_out,
              ffn_norm_g, w_group, b_group, w_router, b_router, w_gate, w_up, w_down, final_norm_g):
    b_p = x_prompt.shape[0]
    pos_p = jnp.arange(x_prompt.shape[1], dtype=jnp.int32)
    pos_s = PAST_LEN + jnp.arange(x_sample.shape[1], dtype=jnp.int32)
    xp, xs = x_prompt, x_sample
    ckv_p, kr_p, sg_p, sc_p = [], [], [], []
    ckv_s, kr_s, sg_s, sc_s = [], [], [], []
    for l in range(DEPTH):
        lw = (attn_norm_g[l], w_in[l], q_norm_g[l], w_uq[l], kv_norm_g[l], w_uk[l], w_uv[l], conv_w[l], a_log[l],
              dt_bias[l], gdn_norm_g[l], w_out[l], ffn_norm_g[l], w_group[l], b_group[l], w_router[l], b_router[l],
              w_gate[l], w_up[l], w_down[l])
        s0 = jnp.zeros((b_p, GDN_HEADS, GDN_DK, GDN_DV), jnp.float32)
        c0 = jnp.zeros((b_p, CONV_W - 1, GDN_QKV), x_prompt.dtype)
        xp, ckv, kr, sg, sc = layer(xp, pos_p, None, None, s0, c0, lw)
        ckv_p.append(ckv)
        kr_p.append(kr)
        sg_p.append(sg.astype(x_prompt.dtype))
        sc_p.append(sc)
        xs, ckv, kr, sg, sc = layer(xs, pos_s, cache_ckv[l], cache_k_rope[l], state_gdn[l], state_conv[l], lw)
        ckv_s.append(ckv)
        kr_s.append(kr)
        sg_s.append(sg.astype(x_sample.dtype))
        sc_s.append(sc)
    y_prompt = rmsnorm(xp, final_norm_g)
    y_sample = rmsnorm(xs, final_norm_g)
    new_ckv_prompt = jnp.stack(ckv_p)
    new_k_rope_prompt = jnp.stack(kr_p)
    new_gdn_prompt = jnp.stack(sg_p)
    new_conv_prompt = jnp.stack(sc_p)
    new_ckv_sample = jnp.stack(ckv_s)
    new_k_rope_sample = jnp.stack(kr_s)
    new_gdn_sample = jnp.stack(sg_s)
    new_conv_sample = jnp.stack(sc_s)
    return (y_prompt, y_sample, new_ckv_prompt, new_k_rope_prompt, new_gdn_prompt, new_conv_prompt,
            new_ckv_sample, new_k_rope_sample, new_gdn_sample, new_conv_sample)
```

```python
import numpy as np
from contextlib import ExitStack
import ml_dtypes
import concourse.bass as bass
import concourse.mybir as mybir
from concourse.bass_utils import run_bass_kernel_spmd

F32 = mybir.dt.float32
BF16 = mybir.dt.bfloat16
I32 = mybir.dt.int32
AF = mybir.ActivationFunctionType
ALU = mybir.AluOpType
AX = mybir.AxisListType

D = 2048
KC = D // 128
EPS = 1e-6
NH = 16
QL = 512
KVL = 512
RD = 64
GQKV = 6144
IN_TOTAL = 13408
C_QL, C_KV, C_KR, C_QKV, C_A, C_B, C_Z, C_GA, C_GB = 0, 512, 1024, 1088, 7232, 7248, 7264, 9312, 11360
NS = 32
import os
CSTOP = float(os.environ.get('CSTOP', '99'))


class Buf:
    def __init__(self, t, psum=False):
        self.t = t
        self.w = None
        self.r = []
        self.psum = psum


class Ctx:
    def __init__(self, nc):
        self.nc = nc
        self.E = dict(pe=nc.tensor, act=nc.scalar, dve=nc.vector, pool=nc.gpsimd, sp=nc.sync)
        self.sems = {}
        self.cnt = {}
        self.seen = {}
        for e in self.E:
            self.newsem(e)
        self.n_inst = 0
        self.gstack = ExitStack()
        self.pstack = ExitStack()
        self.mstack = ExitStack()

    def newsem(self, name):
        self.sems[name] = self.nc.alloc_semaphore(name)
        self.cnt[name] = 0

    def sb(self, name, shape, dt, persist=False):
        st = self.gstack if persist is True else (self.mstack if persist == 'mid' else self.pstack)
        return Buf(st.enter_context(self.nc.sbuf_tensor("sb_" + name, list(shape), dt)))

    def ps(self, name, shape, dt=F32):
        return Buf(self.pstack.enter_context(self.nc.psum_tensor("ps_" + name, list(shape), dt)), psum=True)

    def barrier(self):
        toks = [(n, v) for n, v in self.cnt.items() if v > 0]
        for e in self.E:
            self.wait(e, toks)

    def end_phase(self):
        self.barrier()
        self.pstack.close()
        self.pstack = ExitStack()

    def dram(self, name, shape, dt):
        if getattr(self, 'debug', False):
            return Buf(self.nc.dram_tensor(name, list(shape), dt, kind="ExternalOutput"))
        return Buf(self.nc.dram_tensor(name, list(shape), dt))

    def wait(self, eng, toks):
        for t in toks:
            if t is None:
                continue
            name, val = t
            if self.seen.get((eng, name), 0) >= val:
                continue
            self.E[eng].wait_ge(self.sems[name], val)
            self.seen[(eng, name)] = val

    def _deps(self, eng, outs, ins, extra=()):
        toks = list(extra)
        for b in ins:
            toks.append(b.w)
            if b.psum:
                toks.extend(t for t in b.r if t[0] != eng)
        for b in outs:
            toks.append(b.w)
            toks.extend(b.r)
        self.wait(eng, toks)

    def _mark(self, tok, outs, ins):
        for b in ins:
            b.r.append(tok)
            if len(b.r) > 24:
                b.r = b.r[-24:] if False else self._compact(b.r)
        for b in outs:
            b.w = tok
            b.r = []

    @staticmethod
    def _compact(r):
        best = {}
        for n, v in r:
            if best.get(n, 0) < v:
                best[n] = v
        return list(best.items())

    def op(self, eng, fn, outs=(), ins=(), deps=(), **kw):
        self._deps(eng, outs, ins, deps)
        inst = fn(**kw)
        self.cnt[eng] += 1
        inst.then_inc(self.sems[eng], 1)
        tok = (eng, self.cnt[eng])
        self._mark(tok, outs, ins)
        self.n_inst += 1
        return tok

    def mm(self, out, ops, outs, ins, transpose=False):
        self._deps('pe', outs, ins)
        n = len(ops)
        inst = None
        for i, (l, r) in enumerate(ops):
            inst = self.nc.tensor.matmul(out, lhsT=l, rhs=r, start=(i == 0), stop=(i == n - 1))
            self.n_inst += 1
        self.cnt['pe'] += 1
        inst.then_inc(self.sems['pe'], 1)
        tok = ('pe', self.cnt['pe'])
        self._mark(tok, outs, ins)
        return tok

    def tr(self, out, in_, ident, outs, ins):
        self._deps('pe', outs, ins)
        inst = self.nc.tensor.transpose(out=out, in_=in_, identity=ident)
        self.cnt['pe'] += 1
        inst.then_inc(self.sems['pe'], 1)
        tok = ('pe', self.cnt['pe'])
        self._mark(tok, outs, ins)
        self.n_inst += 1
        return tok

    def dma(self, eng, sem, out, in_, outs=(), ins=(), deps=(), **kw):
        if sem not in self.sems:
            self.newsem(sem)
        self._deps(eng, outs, ins, deps)
        inst = self.E[eng].dma_start(out=out, in_=in_, **kw)
        self.cnt[sem] += 16
        inst.then_inc(self.sems[sem], 16)
        tok = (sem, self.cnt[sem])
        self._mark(tok, outs, ins)
        self.n_inst += 1
        return tok

    def tt(self, ob, o, ab, a, bb, b, op, eng='dve'):
        fn = self.nc.vector.tensor_tensor if eng == 'dve' else self.nc.gpsimd.tensor_tensor
        return self.op(eng, fn, outs=[ob], ins=[ab, bb], out=o, in0=a, in1=b, op=op)

    def ts(self, ob, o, ab, a, sbuf, sc, op, sc2=None, op2=None, eng='dve'):
        fn = self.nc.vector.tensor_scalar if eng == 'dve' else self.nc.gpsimd.tensor_scalar
        ins = [ab] + ([sbuf] if sbuf is not None else [])
        if op2 is None:
            return self.op(eng, fn, outs=[ob], ins=ins, out=o, in0=a, scalar1=sc, scalar2=None, op0=op)
        return self.op(eng, fn, outs=[ob], ins=ins, out=o, in0=a, scalar1=sc, scalar2=sc2, op0=op, op1=op2)

    def stt(self, ob, o, ab, a, sbuf, sc, bb, b, op0, op1):
        return self.op('dve', self.nc.vector.scalar_tensor_tensor, outs=[ob], ins=[ab, sbuf, bb], out=o, in0=a,
                       scalar=sc, in1=b, op0=op0, op1=op1)

    def act(self, ob, o, ib, i, func, bias=None, scale=1.0, accum=None):
        ins = [ib]
        kw = dict(out=o, in_=i, func=func, scale=scale)
        outs = [ob]
        if bias is not None:
            ins.append(bias[0])
            kw['bias'] = bias[1]
        if accum is not None:
            outs.append(accum[0])
            kw['accum_out'] = accum[1]
        return self.op('act', self.nc.scalar.activation, outs=outs, ins=ins, **kw)

    def cp(self, eng, ob, o, ib, i):
        if eng == 'act':
            return self.op('act', self.nc.scalar.copy, outs=[ob], ins=[ib], out=o, in_=i)
        fn = self.nc.vector.tensor_copy if eng == 'dve' else self.nc.gpsimd.tensor_copy
        return self.op(eng, fn, outs=[ob], ins=[ib], out=o, in_=i)

    def idma(self, sem, out, out_off, in_, in_off, nrows, outs=(), ins=()):
        if sem not in self.sems:
            self.newsem(sem)
        self._deps('pool', outs, ins)
        oo = bass.IndirectOffsetOnAxis(ap=out_off, axis=0) if out_off is not None else None
        io = bass.IndirectOffsetOnAxis(ap=in_off, axis=0) if in_off is not None else None
        if not hasattr(self, 'breg'):
            self.breg = {}
        if nrows not in self.breg:
            r = self.nc.gpsimd.alloc_register()
            self.nc.gpsimd.reg_mov(r, nrows - 1)
            self.breg[nrows] = r
        inst = self.nc.gpsimd.indirect_dma_start(out=out, out_offset=oo, in_=in_, in_offset=io,
                                                 bounds_check=self.breg[nrows], oob_is_err=False)
        self.cnt[sem] += 16
        inst.then_inc(self.sems[sem], 16)
        tok = (sem, self.cnt[sem])
        self._mark(tok, outs, ins)
        self.n_inst += 1
        return tok

    def dma_group(self, eng, sem, pairs, outs=(), ins=(), deps=(), **kw):
        if sem not in self.sems:
            self.newsem(sem)
        self._deps(eng, outs, ins, deps)
        for o, i in pairs:
            inst = self.E[eng].dma_start(out=o, in_=i, **kw)
            self.cnt[sem] += 16
            inst.then_inc(self.sems[sem], 16)
            self.n_inst += 1
        tok = (sem, self.cnt[sem])
        self._mark(tok, outs, ins)
        return tok


def rope_tables(pos):
    half = RD // 2
    inv = (10000.0 ** (-np.arange(half, dtype=np.float32) / half)).astype(np.float32)
    ang = pos.astype(np.float32)[:, None] * inv[None, :]
    cos = np.cos(ang).astype(np.float32)
    sin = np.sin(ang).astype(np.float32)
    cosT = np.concatenate([cos, cos], axis=1).T.copy()
    sinT = np.concatenate([sin, sin], axis=1).T.copy()
    return cosT, sinT


class Prog:
    def __init__(self, SEQ, PAST, NG=8, phases="ABCDEF", debug=False):
        self.SEQ, self.PAST, self.NG = SEQ, PAST, NG
        self.debug = debug
        self.T = SEQ + NS
        self.NT = (self.T + 127) // 128
        self.phases = phases
        nc = bass.Bass("TRN2", target_bir_lowering=False)
        self.nc = nc
        self.k = Ctx(nc)
        self.k.debug = debug
        self.inp = {}
        self.out = {}
        self.build()

    def din(self, name, shape, dt=F32):
        t = self.nc.dram_tensor(name, list(shape), dt, kind="ExternalInput")
        self.inp[name] = t
        return t

    def dout(self, name, shape, dt=F32):
        t = self.nc.dram_tensor(name, list(shape), dt, kind="ExternalOutput")
        self.out[name] = t
        return t

    def build(self):
        nc, k = self.nc, self.k
        T, SEQ = self.T, self.SEQ
        self.x_all = self.din("x_all", [T, D])
        self.w_in = self.din("w_in", [D, IN_TOTAL])
        self.attn_g = self.din("attn_g", [128, KC])
        self.qn_g = self.din("qn_g", [128, 4])
        self.kvn_g = self.din("kvn_g", [128, 4])
        self.ident_in = self.din("ident", [128, 128])
        self.cosT = self.din("cosT", [RD, T])
        self.sinT = self.din("sinT", [RD, T])
        self.conv_w = self.din("conv_w", [128, 48, 4])
        self.conv0 = self.din("conv0", [128, 48, 2, 3])
        self.w_uq = self.din("w_uq", [QL, NH * 192])
        self.w_uk = self.din("w_uk", [KVL, NH * 128])
        self.w_uv = self.din("w_uv", [KVL, NH * 128])
        self.cache_ckv = self.din("cache_ckv", [2, self.PAST, KVL])
        self.cache_kr = self.din("cache_kr", [2, self.PAST, RD])
        self.dmask_in = self.din("dmask", [128, 128])
        self.gpar = self.din("gpar", [128, 33])
        self.gmask = self.din("gmask", [2, 128, 514])
        self.state_gdn = self.din("state_gdn", [2, NH, 128, 128])
        NE = self.NG * 8
        self.NE = NE
        self.CAP = 256
        self.NROW = NE * self.CAP + 128
        self.w_out = self.din("w_out", [D, D])
        self.ffn_g = self.din("ffn_g", [128, D])
        self.fin_g = self.din("fin_g", [128, D])
        self.w_rt = self.din("w_rt", [D, 8 + 64])
        self.rbias = self.din("rbias", [128, 72])
        self.mconst = self.din("mconst", [128, 64 + 128])
        self.w_gate = self.din("w_gate", [NE, D, 512])
        self.w_up = self.din("w_up", [NE, D, 512])
        self.w_down = self.din("w_down", [NE, 512, D])
        self.o_y = self.dout("o_y", [T, D])
        self.o_ckv = self.dout("o_ckv", [T, KVL])
        self.o_kr = self.dout("o_kr", [T, RD])
        self.o_conv = self.dout("o_conv", [3, 3, GQKV])
        self.o_gdn = self.dout("o_gdn", [3, NH, 128, 128])
        self.s_qlT = k.dram("s_qlT", [QL, T], BF16)
        self.s_qkvT = k.dram("s_qkvT", [GQKV, T], BF16)
        self.s_zT = k.dram("s_zT", [D, T], BF16)
        self.s_gaT = k.dram("s_gaT", [D, T], BF16)
        self.s_gbT = k.dram("s_gbT", [D, T], BF16)
        self.s_ab = k.dram("s_ab", [T, 32], F32)
        self.s_oaT = k.dram("s_oaT", [D, T], BF16)
        self.s_mT = k.dram("s_mT", [D, T], BF16)
        self.s_x1 = k.dram("s_x1", [T, D], F32)
        self.s_xs = k.dram("s_xs", [self.NROW, D], BF16)
        self.s_eo = k.dram("s_eo", [self.NROW, D], F32)
        self.ident_f = k.sb("ident_f", [128, 128], F32, persist=True)
        self.ident_b = k.sb("ident_b", [128, 128], BF16, persist=True)
        self.ones_b = k.sb("ones_b", [128, 128], BF16, persist=True)
        self.ckvT = k.sb("ckvT", [128, 4, T], BF16, persist='mid')
        self.krT = k.sb("krT", [RD, T], BF16, persist='mid')
        k.dma('sp', 'c_ident', self.ident_f.t[:, :], self.ident_in.ap(), outs=[self.ident_f])
        k.op('dve', nc.vector.tensor_copy, outs=[self.ident_b], ins=[self.ident_f],
             out=self.ident_b.t[:, :], in_=self.ident_f.t[:, :])
        k.op('dve', nc.vector.memset, outs=[self.ones_b], ap=self.ones_b.t[:, :], constant=1.0)
        self.out_toks = []
        if "A" in self.phases:
            self.phase_a()
            k.end_phase()
        if "B" in self.phases:
            self.phase_b()
            k.end_phase()
        k.mstack.close()
        if "C" in self.phases:
            self.phase_c()
            k.end_phase()
        if "D" in self.phases:
            self.phase_d()
            k.end_phase()
            self.phase_e()
            k.end_phase()
            self.phase_f()
            k.end_phase()
        k.wait('sp', self.out_toks)

    def token_groups(self):
        g = []
        for s in range(0, self.SEQ, 512):
            g.append((s, min(512, self.SEQ - s)))
        g.append((self.SEQ, NS))
        return g

    def phase_a(self):
        nc, k = self.nc, self.k
        T, SEQ = self.T, self.SEQ
        ag = k.sb("ag", [128, KC], F32)
        qg = k.sb("qg", [128, 4], F32)
        kvg = k.sb("kvg", [128, 4], F32)
        cw = k.sb("cw", [128, 48, 4], F32)
        c0 = k.sb("c0", [128, 48, 2, 3], F32)
        histP = k.sb("histP", [128, 48, 3], F32)
        k.dma('sp', 'c_ag', ag.t[:, :], self.attn_g.ap(), outs=[ag])
        k.dma('sp', 'c_qg', qg.t[:, :], self.qn_g.ap(), outs=[qg])
        k.dma('sp', 'c_kvg', kvg.t[:, :], self.kvn_g.ap(), outs=[kvg])
        k.dma('sp', 'c_cw', cw.t[:, :, :], self.conv_w.ap(), outs=[cw])
        k.dma('sp', 'c_c0', c0.t[:, :, :, :], self.conv0.ap(), outs=[c0])
        k.op('dve', nc.vector.memset, outs=[histP], ap=histP.t[:, :, :], constant=0.0)
        for g_ in (qg, kvg):
            k.op('dve', nc.vector.tensor_scalar, outs=[g_], ins=[g_], out=g_.t[:, :], in0=g_.t[:, :],
                 scalar1=float(np.sqrt(512.0)), scalar2=None, op0=ALU.mult)

        HT = max(128, (SEQ // 2 + 511) // 512 * 512) if SEQ > 512 else SEQ
        halves = [(0, HT)] if HT >= SEQ else [(0, HT), (HT, SEQ)]
        TH = max(h[1] - h[0] for h in halves) + NS
        xnT = k.sb("xnT", [128, KC, TH], BF16)
        xs = [k.sb(f"xs{i}", [128, D], F32) for i in range(2)]
        xb = [k.sb(f"xb{i}", [128, D], BF16) for i in range(2)]
        ssq = [k.sb(f"ssq{i}", [128, 1], F32) for i in range(2)]
        rstd = [k.sb(f"rstd{i}", [128, 1], F32) for i in range(2)]
        ptr = [k.ps(f"ptr{i}", [128, 4, 128], BF16) for i in range(2)]
        pb = [k.ps(f"pb{i}", [128, 512], F32) for i in range(6)]
        self.pb, self.ptr = pb, ptr
        wb = [k.sb(f"wb{i}", [128, KC, 512], BF16) for i in range(2)]
        ckvT, krT = self.ckvT, self.krT
        cst = [k.sb(f"cst{i}", [128, 515], F32) for i in range(2)]
        acc = [k.sb(f"cacc{i}", [128, 512], F32) for i in range(2)]
        ob = [k.sb(f"ob{i}", [128, 512], BF16) for i in range(4)]
        of = [k.sb(f"of{i}", [128, 512], F32) for i in range(4)]
        sq = [k.sb(f"sq{i}", [128, 512], BF16) for i in range(4)]
        rbc = k.sb("rbc", [128, 512], F32)
        cs_t = k.sb("cs_t", [RD, 2, 512], F32)
        otr = [k.sb(f"otr{i}", [128, 512], F32) for i in range(2)]
        wkr = k.sb("wkr", [128, KC, 128], BF16)
        wabt = k.sb("wabt", [128, KC, 32], BF16)
        cnt = dict(ob=0, of=0, cst=0, pb=0, otr=0, sq=0)

        def nxt(name, lst):
            cnt[name] += 1
            return lst[cnt[name] % len(lst)]

        blocks = [(C_QL, 512, 'ql'), (C_KV, 512, 'kv'), (C_KR, 64, 'kr')]
        blocks += [(C_QKV + i * 512, 512, 'qkv') for i in range(12)]
        blocks += [(C_A, 32, 'ab')]
        blocks += [(C_Z + i * 512, 512, 'z') for i in range(4)]
        blocks += [(C_GA + i * 512, 512, 'ga') for i in range(4)]
        blocks += [(C_GB + i * 512, 512, 'gb') for i in range(4)]
        w_view = self.w_in.ap().rearrange("(kc p) c -> p kc c", p=128)

        for hi, (h0, h1) in enumerate(halves):
            last = hi == len(halves) - 1
            ranges = [(h0, h1)] + ([(SEQ, SEQ + NS)] if last else [])
            loc = {}
            nloc = 0
            for (a, b) in ranges:
                for t in range(a, b, 128):
                    loc[t] = nloc
                    nloc += min(128, b - t)
            tiles = [(t, min(128, b - t)) for (a, b) in ranges for t in range(a, b, 128)]
            for ti, (t0, n) in enumerate(tiles):
                s_ = ti % 2
                l0 = loc[t0]
                k.dma('sp', f'xs{s_}', xs[s_].t[:n, :], self.x_all[t0:t0 + n, :], outs=[xs[s_]])
                k.op('act', nc.scalar.activation, outs=[xb[s_], ssq[s_]], ins=[xs[s_]],
                     out=xb[s_].t[:n, :], in_=xs[s_].t[:n, :], func=AF.Square, accum_out=ssq[s_].t[:n, :])
                k.op('dve', nc.vector.tensor_scalar, outs=[rstd[s_]], ins=[ssq[s_]],
                     out=rstd[s_].t[:n, :], in0=ssq[s_].t[:n, :], scalar1=1.0 / D, scalar2=EPS,
                     op0=ALU.mult, op1=ALU.add)
                k.op('act', nc.scalar.activation, outs=[rstd[s_]], ins=[rstd[s_]],
                     out=rstd[s_].t[:n, :], in_=rstd[s_].t[:n, :], func=AF.Sqrt)
                k.op('dve', nc.vector.reciprocal, outs=[rstd[s_]], ins=[rstd[s_]],
                     out=rstd[s_].t[:n, :], in_=rstd[s_].t[:n, :])
                k.op('dve', nc.vector.tensor_scalar, outs=[xb[s_]], ins=[xs[s_], rstd[s_]],
                     out=xb[s_].t[:n, :], in0=xs[s_].t[:n, :], scalar1=rstd[s_].t[:n, 0:1], scalar2=None,
                     op0=ALU.mult)
                for q4 in range(KC // 4):
                    p = ptr[q4 % 2]
                    for j in range(4):
                        kc = q4 * 4 + j
                        k.tr(p.t[:, j, :n], xb[s_].t[:n, kc * 128:(kc + 1) * 128], self.ident_b.t[:n, :n],
                             outs=[p], ins=[xb[s_], self.ident_b])
                    for j in range(4):
                        kc = q4 * 4 + j
                        if j % 2 == 0:
                            k.op('dve', nc.vector.tensor_scalar, outs=[xnT], ins=[p, ag],
                                 out=xnT.t[:, kc, l0:l0 + n], in0=p.t[:, j, :n], scalar1=ag.t[:, kc:kc + 1],
                                 scalar2=None, op0=ALU.mult)
                        else:
                            k.op('act', nc.scalar.activation, outs=[xnT], ins=[p, ag],
                                 out=xnT.t[:, kc, l0:l0 + n], in_=p.t[:, j, :n], func=AF.Copy,
                                 scale=ag.t[:, kc:kc + 1])
            groups = []
            for t in range(h0, h1, 512):
                groups.append((t, min(512, h1 - t), loc[t - (t - h0) % 128] + (t - h0) % 128 if False else loc[h0] + (t - h0), 'p'))
            if last:
                groups.append((SEQ, 16, loc[SEQ], 's0'))
                groups.append((SEQ + 16, 16, loc[SEQ] + 16, 's1'))

            def chan_mm(ps_, wbuf, c_lo, ncol, l0, n):
                ops = [(wbuf.t[:, kc, c_lo:c_lo + ncol], xnT.t[:, kc, l0:l0 + n]) for kc in range(KC)]
                return k.mm(ps_.t[:ncol, :n], ops, outs=[ps_], ins=[wbuf, xnT])

            for bi, (c_lo, ncol, kind) in enumerate(blocks):
                if kind == 'kr':
                    wbuf = wkr
                    k.dma('pool', 'wkr', wkr.t[:, :, 0:64], w_view[:, :, c_lo:c_lo + 64], outs=[wkr])
                    k.op('dve', nc.vector.tensor_scalar, outs=[wkr], ins=[wkr], out=wkr.t[:, :, 64:96],
                         in0=wkr.t[:, :, 32:64], scalar1=-1.0, scalar2=None, op0=ALU.mult)
                    k.op('dve', nc.vector.tensor_copy, outs=[wkr], ins=[wkr], out=wkr.t[:, :, 96:128],
                         in_=wkr.t[:, :, 0:32])
                elif kind == 'ab':
                    wbuf = wabt
                    k.dma('pool', 'wabt', wabt.t[:, :, :], w_view[:, :, c_lo:c_lo + 32], outs=[wabt])
                else:
                    wbuf = wb[bi % 2]
                    k.dma_group('pool', f'wb{bi % 2}',
                                [(wbuf.t[:, q * 4:(q + 1) * 4, :], w_view[:, q * 4:(q + 1) * 4, c_lo:c_lo + 512])
                                 for q in range(4)], outs=[wbuf])
                if kind in ('ql', 'kv'):
                    gain = qg if kind == 'ql' else kvg
                    for (t0, n, l0, sk) in groups:
                        pss = [pb[j] for j in range(4)]
                        for j in range(4):
                            chan_mm(pss[j], wbuf, j * 128, 128, l0, n)
                        ssb = pb[4]
                        for j in range(4):
                            k.op('act', nc.scalar.activation, outs=[sq[j]], ins=[pss[j]], out=sq[j].t[:, :n],
                                 in_=pss[j].t[:, :n], func=AF.Square)
                        k.mm(ssb.t[:, :n], [(self.ones_b.t[:, :], sq[j].t[:, :n]) for j in range(4)],
                             outs=[ssb], ins=[self.ones_b] + sq)
                        k.op('dve', nc.vector.tensor_scalar, outs=[rbc], ins=[ssb], out=rbc.t[:, :n],
                             in0=ssb.t[:, :n], scalar1=512.0 * EPS, scalar2=None, op0=ALU.add)
                        k.op('act', nc.scalar.activation, outs=[rbc], ins=[rbc], out=rbc.t[:, :n],
                             in_=rbc.t[:, :n], func=AF.Sqrt)
                        k.op('dve', nc.vector.reciprocal, outs=[rbc], ins=[rbc], out=rbc.t[:, :n], in_=rbc.t[:, :n])
                        for j in range(4):
                            if kind == 'ql':
                                o_ = nxt('ob', ob)
                                k.op('dve', nc.vector.scalar_tensor_tensor, outs=[o_], ins=[pss[j], gain, rbc],
                                     out=o_.t[:, :n], in0=pss[j].t[:, :n], scalar=gain.t[:, j:j + 1],
                                     in1=rbc.t[:, :n], op0=ALU.mult, op1=ALU.mult)
                                k.dma('sp', f'ob{cnt["ob"] % 4}', self.s_qlT.t[j * 128:(j + 1) * 128, t0:t0 + n],
                                      o_.t[:, :n], ins=[o_], outs=[self.s_qlT])
                            else:
                                k.op('dve', nc.vector.scalar_tensor_tensor, outs=[of[j]], ins=[pss[j], gain, rbc],
                                     out=of[j].t[:, :n], in0=pss[j].t[:, :n], scalar=gain.t[:, j:j + 1],
                                     in1=rbc.t[:, :n], op0=ALU.mult, op1=ALU.mult)
                                k.op('act', nc.scalar.copy, outs=[ckvT], ins=[of[j]], out=ckvT.t[:, j, t0:t0 + n],
                                     in_=of[j].t[:, :n])
                        if kind == 'kv':
                            for a in range(0, n, 128):
                                m = min(128, n - a)
                                pt = pb[5]
                                for j in range(4):
                                    k.tr(pt.t[:m, j * 128:(j + 1) * 128], of[j].t[:, a:a + m], self.ident_f.t[:, :],
                                         outs=[pt], ins=[of[j], self.ident_f])
                                o_ = nxt('otr', otr)
                                k.op('act', nc.scalar.copy, outs=[o_], ins=[pt], out=o_.t[:m, :], in_=pt.t[:m, :])
                                self.out_toks.append(k.dma('sp', f'otr{cnt["otr"] % 2}',
                                                           self.o_ckv[t0 + a:t0 + a + m, :], o_.t[:m, :], ins=[o_]))
                elif kind == 'kr':
                    for (t0, n, l0, sk) in groups:
                        p1, p2 = pb[4], pb[5]
                        chan_mm(p1, wbuf, 0, 64, l0, n)
                        chan_mm(p2, wbuf, 64, 64, l0, n)
                        k.dma_group('sp', 'cs_t', [(cs_t.t[:, 0, :n], self.cosT[:, t0:t0 + n]),
                                                   (cs_t.t[:, 1, :n], self.sinT[:, t0:t0 + n])], outs=[cs_t])
                        o1 = nxt('of', of)
                        o2 = nxt('of', of)
                        k.op('dve', nc.vector.tensor_tensor, outs=[o1], ins=[p1, cs_t], out=o1.t[:RD, :n],
                             in0=p1.t[:RD, :n], in1=cs_t.t[:, 0, :n], op=ALU.mult)
                        k.op('dve', nc.vector.tensor_tensor, outs=[o2], ins=[p2, cs_t], out=o2.t[:RD, :n],
                             in0=p2.t[:RD, :n], in1=cs_t.t[:, 1, :n], op=ALU.mult)
                        k.op('dve', nc.vector.tensor_tensor, outs=[o1], ins=[o1, o2], out=o1.t[:RD, :n],
                             in0=o1.t[:RD, :n], in1=o2.t[:RD, :n], op=ALU.add)
                        k.op('act', nc.scalar.copy, outs=[krT], ins=[o1], out=krT.t[:, t0:t0 + n], in_=o1.t[:RD, :n])
                        for a in range(0, n, 128):
                            m = min(128, n - a)
                            pt = pb[3]
                            k.tr(pt.t[:m, :RD], o1.t[:RD, a:a + m], self.ident_f.t[:RD, :RD], outs=[pt],
                                 ins=[o1, self.ident_f])
                            o_ = nxt('otr', otr)
                            k.op('act', nc.scalar.copy, outs=[o_], ins=[pt], out=o_.t[:m, :RD], in_=pt.t[:m, :RD])
                            self.out_toks.append(k.dma('sp', f'otr{cnt["otr"] % 2}', self.o_kr[t0 + a:t0 + a + m, :],
                                                       o_.t[:m, :RD], ins=[o_]))
                elif kind == 'ab':
                    for (a, b) in ranges:
                        for t0 in range(a, b, 128):
                            n = min(128, b - t0)
                            l0 = loc[t0]
                            pt = pb[5]
                            ops = [(xnT.t[:, kc, l0:l0 + n], wbuf.t[:, kc, :]) for kc in range(KC)]
                            k.mm(pt.t[:n, :32], ops, outs=[pt], ins=[wbuf, xnT])
                            o_ = nxt('otr', otr)
                            k.op('act', nc.scalar.copy, outs=[o_], ins=[pt], out=o_.t[:n, :32], in_=pt.t[:n, :32])
                            k.dma('sp', f'otr{cnt["otr"] % 2}', self.s_ab.t[t0:t0 + n, :], o_.t[:n, :32],
                                  ins=[o_], outs=[self.s_ab])
                else:
                    for j in range(4):
                        ch = (c_lo - {'qkv': C_QKV, 'z': C_Z, 'ga': C_GA, 'gb': C_GB}[kind]) // 128 + j
                        for (t0, n, l0, sk) in groups:
                            ps_ = nxt('pb', pb[:4])
                            chan_mm(ps_, wbuf, j * 128, 128, l0, n)
                            o_ = nxt('ob', ob)
                            osem = f'ob{cnt["ob"] % 4}'
                            if kind == 'qkv':
                                cs_ = nxt('cst', cst)
                                ac_ = acc[cnt['cst'] % 2]
                                hist = histP.t[:, ch, :] if sk == 'p' else c0.t[:, ch, int(sk[1]), :]
                                hb = histP if sk == 'p' else c0
                                k.op('act', nc.scalar.copy, outs=[cs_], ins=[ps_], out=cs_.t[:, 3:3 + n],
                                     in_=ps_.t[:, :n])
                                k.op('pool', nc.gpsimd.tensor_copy, outs=[cs_], ins=[hb], out=cs_.t[:, 0:3], in_=hist)
                                if sk == 'p':
                                    k.op('pool', nc.gpsimd.tensor_copy, outs=[histP], ins=[cs_],
                                         out=histP.t[:, ch, :], in_=cs_.t[:, n:n + 3])
                                if sk != 'p' or t0 + n == SEQ:
                                    si = 0 if sk == 'p' else 1 + int(sk[1])
                                    self.out_toks.append(k.dma(
                                        'sp', f'cst{cnt["cst"] % 2}',
                                        self.o_conv[si, :, ch * 128:(ch + 1) * 128].rearrange("j p -> p j"),
                                        cs_.t[:, n:n + 3], ins=[cs_], allow_slow_non_contiguous=True))
                                k.op('dve', nc.vector.tensor_scalar, outs=[ac_], ins=[cs_, cw], out=ac_.t[:, :n],
                                     in0=cs_.t[:, 0:n], scalar1=cw.t[:, ch, 0:1], scalar2=None, op0=ALU.mult)
                                for jj in range(1, 4):
                                    k.op('dve', nc.vector.scalar_tensor_tensor, outs=[ac_], ins=[cs_, cw, ac_],
                                         out=ac_.t[:, :n], in0=cs_.t[:, jj:jj + n], scalar=cw.t[:, ch, jj:jj + 1],
                                         in1=ac_.t[:, :n], op0=ALU.mult, op1=ALU.add)
                                k.op('act', nc.scalar.activation, outs=[o_], ins=[ac_], out=o_.t[:, :n],
                                     in_=ac_.t[:, :n], func=AF.Silu)
                                dst = self.s_qkvT
                            else:
                                func = AF.Silu if kind == 'z' else AF.Sigmoid
                                k.op('act', nc.scalar.activation, outs=[o_], ins=[ps_], out=o_.t[:, :n],
                                     in_=ps_.t[:, :n], func=func)
                                dst = {'z': self.s_zT, 'ga': self.s_gaT, 'gb': self.s_gbT}[kind]
                            k.dma('sp', osem, dst.t[ch * 128:(ch + 1) * 128, t0:t0 + n], o_.t[:, :n], ins=[o_],
                                  outs=[dst])


    def phase_b(self):
        nc, k = self.nc, self.k
        T, SEQ, PAST = self.T, self.SEQ, self.PAST
        SC = float(192.0 ** -0.5)
        NKMAX = max(SEQ, PAST + 16)
        NTK = (NKMAX + 127) // 128
        qlT = k.sb("qlT", [128, 4, T], BF16)
        k.dma_group('sp', 'qlT', [(qlT.t[:, j, :], self.s_qlT.t[j * 128:(j + 1) * 128, :]) for j in range(4)],
                    outs=[qlT], ins=[self.s_qlT])
        dmask_f = k.sb("dmask_f", [128, 128], F32)
        dmask = k.sb("dmask", [128, 128], BF16)
        k.dma('sp', 'dmask', dmask_f.t[:, :], self.dmask_in.ap(), outs=[dmask_f])
        k.op('dve', nc.vector.tensor_copy, outs=[dmask], ins=[dmask_f], out=dmask.t[:, :], in_=dmask_f.t[:, :])
        pastT = k.sb("pastT", [128, 4, PAST + 16], BF16)
        krpT = k.sb("krpT", [RD, PAST + 16], BF16)
        stg = [k.sb(f"stg{i}", [128, 512], F32) for i in range(2)]
        wq = [k.sb(f"wq{i}", [128, 4, 256], BF16) for i in range(2)]
        wk = [k.sb(f"wk{i}", [128, 4, 128], BF16) for i in range(2)]
        wv = [k.sb(f"wv{i}", [128, 4, 128], BF16) for i in range(2)]
        KT = k.sb("KT", [128, NKMAX], BF16)
        V = k.sb("V", [128, NTK, 128], BF16)
        QT = k.sb("QT", [128, SEQ], BF16)
        QrT = k.sb("QrT", [RD, SEQ], BF16)
        q1 = k.sb("q1", [RD, 512], F32)
        q2 = k.sb("q2", [RD, 512], F32)
        cs_t = k.sb("cs_tb", [RD, 2, 512], F32)
        gaT1 = k.sb("gaT0", [128, T], BF16)
        oaT1 = k.sb("oaT0", [128, T], BF16)
        gaT = [gaT1, gaT1]
        oaT = [oaT1, oaT1]
        Pb = [k.sb(f"Pb{i}", [128, 512], BF16) for i in range(2)]
        PT = [k.sb(f"PT{i}", [128, 4, 128], BF16) for i in range(2)]
        Oacc = k.sb("Oacc", [128, 128], F32)
        On = k.sb("On", [128, 128], BF16)
        st_m = [k.sb(f"st_m{i}", [128, 1], F32) for i in range(2)]
        st_l = k.sb("st_l", [128, 1], F32)
        gmax = k.sb("gmax", [128, 1], F32)
        negm = k.sb("negm", [128, 1], F32)
        alpha = k.sb("alpha", [128, 1], F32)
        rsum = k.sb("rsum", [128, 1], F32)
        pS = [k.ps(f"pS{i}", [128, 512], F32) for i in range(2)]
        pT = [k.ps(f"pT{i}", [128, 8, 128], BF16) for i in range(2)]
        pO = [k.ps(f"pO{i}", [128, 128], F32) for i in range(2)]
        pM = [k.ps(f"pM{i}", [128, 512], F32) for i in range(2)]
        c = dict(pM=0, pS=0, stg=0)

        def nx(name, lst):
            c[name] += 1
            return lst[c[name] % len(lst)]

        wq_v = self.w_uq.ap().rearrange("(cc p) x -> p cc x", p=128)
        wk_v = self.w_uk.ap().rearrange("(cc p) x -> p cc x", p=128)
        wv_v = self.w_uv.ap().rearrange("(cc p) x -> p cc x", p=128)

        def load_head_w(h):
            s_ = h % 2
            k.dma('pool', f'wq{s_}', wq[s_].t[:, :, 0:192], wq_v[:, :, h * 192:(h + 1) * 192], outs=[wq[s_]])
            k.op('dve', nc.vector.tensor_scalar, outs=[wq[s_]], ins=[wq[s_]], out=wq[s_].t[:, :, 192:224],
                 in0=wq[s_].t[:, :, 160:192], scalar1=-1.0, scalar2=None, op0=ALU.mult)
            k.op('dve', nc.vector.tensor_copy, outs=[wq[s_]], ins=[wq[s_]], out=wq[s_].t[:, :, 224:256],
                 in_=wq[s_].t[:, :, 128:160])
            k.dma('pool', f'wk{s_}', wk[s_].t[:, :, :], wk_v[:, :, h * 128:(h + 1) * 128], outs=[wk[s_]])
            k.dma('pool', f'wv{s_}', wv[s_].t[:, :, :], wv_v[:, :, h * 128:(h + 1) * 128], outs=[wv[s_]])

        def build_kv(h, segs):
            s_ = h % 2
            for (src, a0, n, d0) in segs:
                for a in range(0, n, 512):
                    m = min(512, n - a)
                    p = nx('pM', pM)
                    k.mm(p.t[:, :m], [(wk[s_].t[:, cc, :], src.t[:, cc, a0 + a:a0 + a + m]) for cc in range(4)],
                         outs=[p], ins=[wk[s_], src])
                    k.op('act', nc.scalar.copy, outs=[KT], ins=[p], out=KT.t[:, d0 + a:d0 + a + m], in_=p.t[:, :m])
                assert d0 % 128 == 0
                tl = [(a, min(128, n - a)) for a in range(0, n, 128)]
                for g0 in range(0, len(tl), 4):
                    p = nx('pM', pM)
                    grp = tl[g0:g0 + 4]
                    for j, (a, m) in enumerate(grp):
                        k.mm(p.t[:m, j * 128:(j + 1) * 128],
                             [(src.t[:, cc, a0 + a:a0 + a + m], wv[s_].t[:, cc, :]) for cc in range(4)],
                             outs=[p], ins=[wv[s_], src])
                    t_ = (d0 + grp[0][0]) // 128
                    if all(m == 128 for (_, m) in grp):
                        k.op('dve', nc.vector.tensor_copy, outs=[V], ins=[p],
                             out=V.t[:, t_:t_ + len(grp), :],
                             in_=p.t[:, :len(grp) * 128].rearrange("p (j d) -> p j d", d=128))
                    else:
                        for j, (a, m) in enumerate(grp):
                            k.op('dve', nc.vector.tensor_copy, outs=[V], ins=[p], out=V.t[:m, t_ + j, :],
                                 in_=p.t[:m, j * 128:(j + 1) * 128])

        def build_q(h, q0, nq):
            s_ = h % 2
            for a in range(0, nq, 512):
                m = min(512, nq - a)
                t0 = q0 + a
                p = nx('pM', pM)
                k.mm(p.t[:, :m], [(wq[s_].t[:, cc, 0:128], qlT.t[:, cc, t0:t0 + m]) for cc in range(4)],
                     outs=[p], ins=[wq[s_], qlT])
                k.op('act', nc.scalar.activation, outs=[QT], ins=[p], out=QT.t[:, a:a + m], in_=p.t[:, :m],
                     func=AF.Copy, scale=SC)
                p1 = nx('pM', pM)
                k.mm(p1.t[:RD, :m], [(wq[s_].t[:, cc, 128:192], qlT.t[:, cc, t0:t0 + m]) for cc in range(4)],
                     outs=[p1], ins=[wq[s_], qlT])
                k.dma_group('sp', 'cs_tb', [(cs_t.t[:, 0, :m], self.cosT[:, t0:t0 + m]),
                                            (cs_t.t[:, 1, :m], self.sinT[:, t0:t0 + m])], outs=[cs_t])
                k.op('dve', nc.vector.tensor_tensor, outs=[q1], ins=[p1, cs_t], out=q1.t[:, :m], in0=p1.t[:RD, :m],
                     in1=cs_t.t[:, 0, :m], op=ALU.mult)
                p2 = nx('pM', pM)
                k.mm(p2.t[:RD, :m], [(wq[s_].t[:, cc, 192:256], qlT.t[:, cc, t0:t0 + m]) for cc in range(4)],
                     outs=[p2], ins=[wq[s_], qlT])
                k.op('dve', nc.vector.tensor_tensor, outs=[q2], ins=[p2, cs_t], out=q2.t[:, :m], in0=p2.t[:RD, :m],
                     in1=cs_t.t[:, 1, :m], op=ALU.mult)
                k.op('dve', nc.vector.tensor_tensor, outs=[q1], ins=[q1, q2], out=q1.t[:, :m], in0=q1.t[:, :m],
                     in1=q2.t[:, :m], op=ALU.add)
                k.op('act', nc.scalar.activation, outs=[QrT], ins=[q1], out=QrT.t[:, a:a + m], in_=q1.t[:, :m],
                     func=AF.Copy, scale=SC)

        def attend(h, qa, nq, tiles, diag, krbuf, kr0, tq0):
            s_ = h % 2
            groups = [tiles[i:i + 4] for i in range(0, len(tiles), 4)]
            mcur = None
            for gi, grp in enumerate(groups):
                pS_ = nx('pS', pS)
                col = 0
                plain = [(kc0, nk_) for ti, (kc0, nk_) in enumerate(grp) if gi * 4 + ti != diag]
                offs = []
                if plain:
                    kc0 = plain[0][0]
                    nk_ = sum(x[1] for x in plain)
                    k.mm(pS_.t[:nq, 0:nk_], [(QT.t[:, qa:qa + nq], KT.t[:, kc0:kc0 + nk_]),
                                              (QrT.t[:, qa:qa + nq], krbuf.t[:, kr0 + kc0:kr0 + kc0 + nk_])],
                         outs=[pS_], ins=[QT, KT, QrT, krbuf])
                    col = nk_
                if len(plain) != len(grp):
                    kc0, nk_ = grp[-1]
                    k.mm(pS_.t[:nq, col:col + nk_], [(QT.t[:, qa:qa + nq], KT.t[:, kc0:kc0 + nk_]),
                                                     (QrT.t[:, qa:qa + nq], krbuf.t[:, kr0 + kc0:kr0 + kc0 + nk_]),
                                                     (self.ident_b.t[:nq, :nq], dmask.t[:nq, :nk_])],
                         outs=[pS_], ins=[QT, KT, QrT, krbuf, self.ident_b, dmask])
                    col += nk_
                N = col
                first = gi == 0
                k.op('dve', nc.vector.reduce_max, outs=[gmax], ins=[pS_], out=gmax.t[:nq, :], in_=pS_.t[:nq, :N],
                     axis=AX.X)
                mnew = st_m[gi % 2]
                if first:
                    k.op('dve', nc.vector.tensor_copy, outs=[mnew], ins=[gmax], out=mnew.t[:nq, :], in_=gmax.t[:nq, :])
                else:
                    k.op('dve', nc.vector.tensor_tensor, outs=[mnew], ins=[gmax, mcur], out=mnew.t[:nq, :],
                         in0=gmax.t[:nq, :], in1=mcur.t[:nq, :], op=ALU.max)
                k.op('dve', nc.vector.tensor_scalar, outs=[negm], ins=[mnew], out=negm.t[:nq, :], in0=mnew.t[:nq, :],
                     scalar1=-1.0, scalar2=None, op0=ALU.mult)
                Pb_ = Pb[gi % 2]
                k.op('act', nc.scalar.activation, outs=[Pb_, rsum], ins=[pS_, negm], out=Pb_.t[:nq, :N],
                     in_=pS_.t[:nq, :N], func=AF.Exp, bias=negm.t[:nq, 0:1], scale=1.0, accum_out=rsum.t[:nq, :])
                if not first:
                    k.op('act', nc.scalar.activation, outs=[alpha], ins=[mcur, negm], out=alpha.t[:nq, :],
                         in_=mcur.t[:nq, :], func=AF.Exp, bias=negm.t[:nq, 0:1], scale=1.0)
                    k.op('dve', nc.vector.scalar_tensor_tensor, outs=[st_l], ins=[st_l, alpha, rsum],
                         out=st_l.t[:nq, :], in0=st_l.t[:nq, :], scalar=alpha.t[:nq, 0:1], in1=rsum.t[:nq, :],
                         op0=ALU.mult, op1=ALU.add)
                else:
                    k.op('dve', nc.vector.tensor_copy, outs=[st_l], ins=[rsum], out=st_l.t[:nq, :], in_=rsum.t[:nq, :])
                mcur = mnew
                pT_ = pT[gi % 2]
                PT_ = PT[gi % 2]
                col = 0
                order = plain + ([grp[-1]] if len(plain) != len(grp) else [])
                for j, (kc0, nk_) in enumerate(order):
                    k.tr(pT_.t[:nk_, j, :nq], Pb_.t[:nq, col:col + nk_], self.ident_b.t[:nq, :nq], outs=[pT_],
                         ins=[Pb_, self.ident_b])
                    col += nk_
                for j, (kc0, nk_) in enumerate(order):
                    if j % 2 == 0:
                        k.op('act', nc.scalar.copy, outs=[PT_], ins=[pT_], out=PT_.t[:nk_, j, :nq], in_=pT_.t[:nk_, j, :nq])
                    else:
                        k.op('dve', nc.vector.tensor_copy, outs=[PT_], ins=[pT_], out=PT_.t[:nk_, j, :nq],
                             in_=pT_.t[:nk_, j, :nq])
                pO_ = pO[gi % 2]
                k.mm(pO_.t[:nq, :], [(PT_.t[:nk_, j, :nq], V.t[:nk_, kc0 // 128, :]) for j, (kc0, nk_) in enumerate(order)],
                     outs=[pO_], ins=[PT_, V])
                if first:
                    k.op('dve', nc.vector.tensor_copy, outs=[Oacc], ins=[pO_], out=Oacc.t[:nq, :], in_=pO_.t[:nq, :])
                else:
                    k.op('dve', nc.vector.scalar_tensor_tensor, outs=[Oacc], ins=[Oacc, alpha, pO_],
                         out=Oacc.t[:nq, :], in0=Oacc.t[:nq, :], scalar=alpha.t[:nq, 0:1], in1=pO_.t[:nq, :],
                         op0=ALU.mult, op1=ALU.add)
            k.op('dve', nc.vector.reciprocal, outs=[rsum], ins=[st_l], out=rsum.t[:nq, :], in_=st_l.t[:nq, :])
            k.op('dve', nc.vector.tensor_scalar, outs=[On], ins=[Oacc, rsum], out=On.t[:nq, :], in0=Oacc.t[:nq, :],
                 scalar1=rsum.t[:nq, 0:1], scalar2=None, op0=ALU.mult)
            pOT = pT[1]
            k.tr(pOT.t[:, 7, :nq], On.t[:nq, :], self.ident_b.t[:nq, :nq], outs=[pOT], ins=[On, self.ident_b])
            k.op('dve', nc.vector.tensor_tensor, outs=[oaT[s_]], ins=[pOT, gaT[s_]], out=oaT[s_].t[:, tq0:tq0 + nq],
                 in0=pOT.t[:, 7, :nq], in1=gaT[s_].t[:, tq0:tq0 + nq], op=ALU.mult)

        for h in range(NH):
            s_ = h % 2
            load_head_w(h)
            k.dma('sp', 'gaT0', gaT[s_].t[:, :], self.s_gaT.t[h * 128:(h + 1) * 128, :], outs=[gaT[s_]],
                  ins=[self.s_gaT])
            build_kv(h, [(self.ckvT, 0, SEQ, 0)])
            build_q(h, 0, SEQ)
            for qt in range(SEQ // 128):
                tiles = [(j * 128, 128) for j in range(qt + 1)]
                attend(h, qt * 128, 128, tiles, qt, self.krT, 0, qt * 128)
            if h == NH - 1 or True:
                pass
            self._oa_pending = getattr(self, '_oa_pending', {})
            self._oa_pending[h] = s_
            k.dma('sp', 'oaT0', self.s_oaT.t[h * 128:(h + 1) * 128, 0:SEQ], oaT[s_].t[:, 0:SEQ], ins=[oaT[s_]],
                  outs=[self.s_oaT])
        for sb in range(2):
            for a in range(0, PAST, 128):
                st_ = nx('stg', stg)
                k.dma('sp', f'stg{c["stg"] % 2}', st_.t[:, :], self.cache_ckv[sb, a:a + 128, :], outs=[st_])
                p = nx('pM', pM)
                for cc in range(4):
                    k.tr(p.t[:, cc * 128:(cc + 1) * 128], st_.t[:, cc * 128:(cc + 1) * 128], self.ident_f.t[:, :],
                         outs=[p], ins=[st_, self.ident_f])
                k.op('act', nc.scalar.copy, outs=[pastT], ins=[p], out=pastT.t[:, :, a:a + 128],
                     in_=p.t[:, :].rearrange("p (c t) -> p c t", t=128))
                st2 = nx('stg', stg)
                k.dma('sp', f'stg{c["stg"] % 2}', st2.t[:, :RD], self.cache_kr[sb, a:a + 128, :], outs=[st2])
                p = nx('pM', pM)
                k.tr(p.t[:RD, :128], st2.t[:, :RD], self.ident_f.t[:, :], outs=[p], ins=[st2, self.ident_f])
                k.op('dve', nc.vector.tensor_copy, outs=[krpT], ins=[p], out=krpT.t[:, a:a + 128], in_=p.t[:RD, :128])
            tq = SEQ + 16 * sb
            k.op('dve', nc.vector.tensor_copy, outs=[pastT], ins=[self.ckvT], out=pastT.t[:, :, PAST:PAST + 16],
                 in_=self.ckvT.t[:, :, tq:tq + 16])
            k.op('dve', nc.vector.tensor_copy, outs=[krpT], ins=[self.krT], out=krpT.t[:, PAST:PAST + 16],
                 in_=self.krT.t[:, tq:tq + 16])
            for h in range(NH):
                s_ = h % 2
                load_head_w(h)
                k.dma('sp', 'gaT0', gaT[s_].t[:, :16], self.s_gaT.t[h * 128:(h + 1) * 128, tq:tq + 16],
                      outs=[gaT[s_]], ins=[self.s_gaT])
                build_kv(h, [(pastT, 0, PAST + 16, 0)])
                build_q(h, tq, 16)
                tiles = [(j * 128, 128) for j in range(PAST // 128)] + [(PAST, 16)]
                attend(h, 0, 16, tiles, None, krpT, 0, 0)
                k.dma('sp', 'oaT0', self.s_oaT.t[h * 128:(h + 1) * 128, tq:tq + 16], oaT[s_].t[:, 0:16],
                      ins=[oaT[s_]], outs=[self.s_oaT])

    def phase_c(self):
        nc, k = self.nc, self.k
        T, SEQ = self.T, self.SEQ
        HB = 8
        gpar = k.sb("gpar", [128, 33], F32)
        k.dma('sp', 'gpar', gpar.t[:, :], self.gpar.ap(), outs=[gpar])
        negA = k.sb("negA", [128, 16], F32)
        k.act(negA, negA.t[:, :], gpar, gpar.t[:, 0:16], AF.Exp)
        k.ts(negA, negA.t[:, :], negA, negA.t[:, :], None, -1.0, ALU.mult)
        gm = [k.sb(f"gm{v}", [128, 514], F32) for v in range(2)]
        for v in range(2):
            k.dma('sp', f'gm{v}', gm[v].t[:, :], self.gmask[v], outs=[gm[v]])
        ones_f = k.sb("ones_f", [128, 128], F32)
        k.op('dve', nc.vector.memset, outs=[ones_f], ap=ones_f.t[:, :], constant=1.0)
        gmb = [k.sb(f"gmb{v}", [128, 514], BF16) for v in range(2)]
        for v in range(2):
            k.cp('dve', gmb[v], gmb[v].t[:, :], gm[v], gm[v].t[:, :])
        g3 = k.sb("g3", [128, 3, 16], BF16)
        b2s = k.sb("b2s", [128, 2, 16], BF16)
        g3f = k.sb("g3f", [128, 3, 16], F32)
        b2f = k.sb("b2f", [128, 2, 16], F32)
        res16 = k.sb("res16", [128, 16], F32)
        G48 = k.sb("G48", [128, 48], F32)

        def split3(src, n, dst_b, dst_f, parts):
            cur = src
            for p in range(parts):
                k.cp('dve', dst_b, dst_b.t[:n, p, :], cur, cur.t[:n, :])
                k.cp('dve', dst_f, dst_f.t[:n, p, :], dst_b, dst_b.t[:n, p, :])
                if p < parts - 1:
                    k.tt(res16, res16.t[:n, :], cur, cur.t[:n, :], dst_f, dst_f.t[:n, p, :], ALU.subtract)
                    cur = res16
        Sf = [k.sb(f"Sf{i}", [128, NH, 128], F32) for i in range(3)]
        Sb = [k.sb(f"Sb{i}", [128, NH, 128], BF16) for i in range(3)]
        k.op('dve', nc.vector.memset, outs=[Sf[0]], ap=Sf[0].t[:, :, :], constant=0.0)
        k.op('dve', nc.vector.memset, outs=[Sb[0]], ap=Sb[0].t[:, :, :], constant=0.0)
        for i in (1, 2):
            k.dma('sp', f'Sf{i}', Sf[i].t[:, :, :], self.state_gdn[i - 1].rearrange("h p d -> p h d"), outs=[Sf[i]])
            k.cp('dve', Sb[i], Sb[i].t[:, :, :], Sf[i], Sf[i].t[:, :, :])
        qk_g = k.sb("qk_g", [128, HB, 2, 512], BF16)
        v_g = k.sb("v_g", [128, HB, 512], BF16)
        zg_g = k.sb("zg_g", [128, HB, 512], BF16)
        gb_g = k.sb("gb_g", [128, HB, 512], BF16)
        oa_g = k.sb("oa_g", [128, HB, 512], BF16)
        m_g = k.sb("m_g", [128, HB, 512], BF16)
        ab_t = k.sb("ab_t", [128, 32], F32)
        tmp16 = k.sb("tmp16", [128, 16], F32)
        g16 = k.sb("g16", [128, 16], F32)
        beta16 = k.sb("beta16", [128, 16], F32)
        G16 = k.sb("G16", [128, 16], F32)
        negG16 = k.sb("negG16", [128, 16], F32)
        GL16 = k.sb("GL16", [128, 16], F32)
        eG16 = k.sb("eG16", [128, 16], F32)
        be16 = k.sb("be16", [128, 16], F32)
        el16 = k.sb("el16", [128, 2, 16], F32)
        def per(name, shape, dt):
            return [k.sb(f"{name}{j}", shape, dt) for j in range(HB)]
        sq2 = per("sq2_", [128, 2, 128], BF16)
        r2 = per("r2_", [128, 2, 128], F32)
        qkn = per("qkn_", [128, 2, 128], BF16)
        kbe = per("kbe_", [128, 128], BF16)
        kdlm = per("kdlm_", [128, 2, 128], BF16)
        vb = per("vb_", [128, 128], BF16)
        rhsG = per("rhsG_", [128, 5, 128], BF16)
        argT = per("argT_", [128, 128], F32)
        decT = per("decT_", [128, 128], F32)
        eGbc = per("eGbc_", [128, 128], F32)
        bs = per("bs_", [128, 128], F32)
        t1 = per("t1_", [128, 128], F32)
        Nb = [per(f"Nb{q}_", [128, 128], BF16) for q in range(2)]
        Lb = [per(f"Lb{q}_", [128, 128], BF16) for q in range(2)]
        Pm = per("Pm_", [128, 128], BF16)
        AT = per("AT_", [128, 2, 128], BF16)
        u_f = per("u_f_", [128, 128], F32)
        wTm = per("wTm_", [128, 2, 128], BF16)
        qeTm = per("qeTm_", [128, 2, 128], BF16)
        vnew = per("vnew_", [128, 128], BF16)
        o_f = per("o_f_", [128, 128], F32)
        on_b = per("on_b_", [128, 128], BF16)
        oss = per("oss_", [128, 1], F32)
        ojunk = per("ojunk_", [128, 128], BF16)
        banks = [k.ps(f"pc{i}", [128, 4, 128], F32) for i in range(6)]
        bankb = [k.ps(f"pcb{i}", [128, 8, 128], BF16) for i in range(2)]
        bc = [0, 0]

        def pbank(j):
            b = banks[(bc[0] * 2 + j // 4) % 6]
            return b, b.t[:, j % 4, :]

        def step():
            bc[0] += 1

        def stepb():
            bc[1] += 1
            return bankb[bc[1] % 2]

        def run_tile(h0, t0, n, L, var, sidx, goff):
            TRI = gm[var].t[:n, 0:n]
            NEGU = gm[var].t[:n, 128:128 + n]
            SU = gm[var].t[:n, 256:256 + n]
            cm = gm[var]
            sl = slice(goff, goff + n)
            k.dma('sp', 'ab_t', ab_t.t[:n, :], self.s_ab.t[t0:t0 + n, :], outs=[ab_t], ins=[self.s_ab])
            k.tt(tmp16, tmp16.t[:n, :], ab_t, ab_t.t[:n, 0:16], gpar, gpar.t[:n, 16:32], ALU.add)
            k.act(tmp16, tmp16.t[:n, :], tmp16, tmp16.t[:n, :], AF.Exp)
            k.act(tmp16, tmp16.t[:n, :], tmp16, tmp16.t[:n, :], AF.Ln, bias=(ones_f, ones_f.t[:n, 0:1]))
            k.tt(g16, g16.t[:n, :], tmp16, tmp16.t[:n, :], negA, negA.t[:n, :], ALU.mult)
            k.act(beta16, beta16.t[:n, :], ab_t, ab_t.t[:n, 16:32], AF.Sigmoid)
            split3(g16, n, g3, g3f, 3)
            split3(beta16, n, b2s, b2f, 2)
            step()
            b_, p_ = pbank(0)
            k.mm(p_[:n, 0:48], [(gmb[var].t[:n, 0:n], g3.t[:n, :, :])], outs=[b_], ins=[gmb[var], g3])
            k.cp('dve', G48, G48.t[:n, :], b_, p_[:n, 0:48])
            k.tt(G16, G16.t[:n, :], G48, G48.t[:n, 0:16], G48, G48.t[:n, 16:32], ALU.add)
            k.tt(G16, G16.t[:n, :], G16, G16.t[:n, :], G48, G48.t[:n, 32:48], ALU.add)
            k.ts(negG16, negG16.t[:n, :], G16, G16.t[:n, :], None, -1.0, ALU.mult)
            b2, p2 = pbank(1)
            k.mm(p2[:n, 0:48], [(gmb[var].t[:n, 386:386 + n], g3.t[:n, :, :])], outs=[b2], ins=[gmb[var], g3])
            k.cp('dve', G48, G48.t[:n, :], b2, p2[:n, 0:48])
            k.tt(GL16, GL16.t[:n, :], G48, G48.t[:n, 0:16], G48, G48.t[:n, 16:32], ALU.add)
            k.tt(GL16, GL16.t[:n, :], GL16, GL16.t[:n, :], G48, G48.t[:n, 32:48], ALU.add)
            k.act(eG16, eG16.t[:n, :], G16, G16.t[:n, :], AF.Exp)
            k.tt(be16, be16.t[:n, :], eG16, eG16.t[:n, :], beta16, beta16.t[:n, :], ALU.mult)
            k.tt(tmp16, tmp16.t[:n, :], GL16, GL16.t[:n, :], G16, G16.t[:n, :], ALU.subtract)
            k.act(tmp16, tmp16.t[:n, :], tmp16, tmp16.t[:n, :], AF.Exp)
            for c_ in range(2):
                k.ts(el16, el16.t[:n, c_, :], tmp16, tmp16.t[:n, :], cm, cm.t[:n, 384 + c_:385 + c_], ALU.mult)
            if CSTOP <= 1:
                return
            hs = list(range(HB))
            step()
            for j in hs:
                k.act(sq2[j], sq2[j].t[:, :, :n], qk_g, qk_g.t[:, j, :, sl], AF.Square)
            if CSTOP <= 1.2:
                return
            for q in range(2):
                step()
                for j in hs:
                    b_, p_ = pbank(j)
                    k.mm(p_[:, :n], [(self.ones_b.t[:, :], sq2[j].t[:, q, :n])], outs=[b_], ins=[self.ones_b, sq2[j]])
                    k.ts(r2[j], r2[j].t[:, q, :n], b_, p_[:, :n], None, EPS, ALU.add)
            if CSTOP <= 1.4:
                return
            for j in hs:
                k.act(r2[j], r2[j].t[:, :, :n], r2[j], r2[j].t[:, :, :n], AF.Sqrt)
            if CSTOP <= 1.5:
                return
            for j in hs:
                k.op('dve', nc.vector.reciprocal, outs=[r2[j]], ins=[r2[j]], out=r2[j].t[:, :, :n], in_=r2[j].t[:, :, :n])
            for j in hs:
                k.ts(r2[j], r2[j].t[:, 0, :n], r2[j], r2[j].t[:, 0, :n], None, float(128.0 ** -0.5), ALU.mult)
            if CSTOP <= 1.6:
                return
            for j in hs:
                k.tt(qkn[j], qkn[j].t[:, :, :n], qk_g, qk_g.t[:, j, :, sl], r2[j], r2[j].t[:, :, :n], ALU.mult)
            if CSTOP <= 2:
                return
            pb_ = stepb()
            for j in hs:
                k.tr(pb_.t[:n, j, :], qkn[j].t[:, 1, :n], self.ident_b.t[:, :], outs=[pb_], ins=[qkn[j], self.ident_b])
            if CSTOP <= 2.2:
                return
            for j in hs:
                h = h0 + j
                k.ts(kbe[j], kbe[j].t[:n, :], pb_, pb_.t[:n, j, :], be16, be16.t[:n, h:h + 1], ALU.mult)
            if CSTOP <= 2.4:
                return
            for j in hs:
                h = h0 + j
                for c_ in range(2):
                    k.ts(kdlm[j], kdlm[j].t[:n, c_, :], pb_, pb_.t[:n, j, :], el16, el16.t[:n, c_, h:h + 1], ALU.mult)
            if CSTOP <= 2.6:
                return
            pb_ = stepb()
            for j in hs:
                k.tr(pb_.t[:n, j, :], v_g.t[:, j, sl], self.ident_b.t[:, :], outs=[pb_], ins=[v_g, self.ident_b])
            for j in hs:
                h = h0 + j
                k.ts(vb[j], vb[j].t[:n, :], pb_, pb_.t[:n, j, :], beta16, beta16.t[:n, h:h + 1], ALU.mult)
            if CSTOP <= 3:
                return
            for j in hs:
                h = h0 + j
                for p in range(3):
                    k.ts(rhsG[j], rhsG[j].t[:n, p, 0:n], gm[var], TRI, g3f, g3f.t[:n, p, h:h + 1], ALU.mult)
                for p in range(2):
                    k.ts(rhsG[j], rhsG[j].t[:n, 3 + p, 0:n], self.ident_f, self.ident_f.t[:n, :n], b2f,
                         b2f.t[:n, p, h:h + 1], ALU.mult)
            step()
            gb_slots = []
            for j in hs:
                b_, p_ = pbank(j)
                k.mm(p_[:, :n], [(self.ones_b.t[:n, :], rhsG[j].t[:n, p, 0:n]) for p in range(3)], outs=[b_],
                     ins=[self.ones_b, rhsG[j]])
                gb_slots.append((b_, p_))
            for j in hs:
                h = h0 + j
                b_, p_ = gb_slots[j]
                k.act(eGbc[j], eGbc[j].t[:, :n], b_, p_[:, :n], AF.Exp)
                k.stt(argT[j], argT[j].t[:n, :n], b_, p_[:n, :n], negG16, negG16.t[:n, h:h + 1], gm[var], NEGU,
                      ALU.add, ALU.add)
                k.act(decT[j], decT[j].t[:n, :n], argT[j], argT[j].t[:n, :n], AF.Exp)
            step()
            for j in hs:
                b_, p_ = pbank(j)
                k.mm(p_[:n, :n], [(self.ones_b.t[:n, :n], rhsG[j].t[:n, 3 + p, 0:n]) for p in range(2)], outs=[b_],
                     ins=[self.ones_b, rhsG[j]])
                k.tt(bs[j], bs[j].t[:n, :n], b_, p_[:n, :n], gm[var], SU, ALU.mult)
            if CSTOP <= 4:
                return
            step()
            for j in hs:
                b_, p_ = pbank(j)
                k.mm(p_[:n, :n], [(qkn[j].t[:, 1, :n], qkn[j].t[:, 1, :n])], outs=[b_], ins=[qkn[j]])
                k.tt(t1[j], t1[j].t[:n, :n], b_, p_[:n, :n], decT[j], decT[j].t[:n, :n], ALU.mult)
                k.tt(Nb[0][j], Nb[0][j].t[:n, :n], t1[j], t1[j].t[:n, :n], bs[j], bs[j].t[:n, :n], ALU.mult)
            step()
            for j in hs:
                b_, p_ = pbank(j)
                k.mm(p_[:n, :n], [(qkn[j].t[:, 1, :n], qkn[j].t[:, 0, :n])], outs=[b_], ins=[qkn[j]])
                k.op('dve', nc.vector.memset, outs=[AT[j]], ap=AT[j].t[:n, :, :n], constant=0.0)
                for c_ in range(2):
                    cs = slice(c_ * L, (c_ + 1) * L)
                    k.tt(AT[j], AT[j].t[:n, c_, cs], b_, p_[:n, cs], decT[j], decT[j].t[:n, cs], ALU.mult)
            if CSTOP <= 5:
                return
            pb_ = stepb()
            for j in hs:
                k.tr(pb_.t[:n, j, :n], Nb[0][j].t[:n, :n], self.ident_b.t[:n, :n], outs=[pb_], ins=[Nb[0][j], self.ident_b])
            for j in hs:
                k.cp('act', Lb[0][j], Lb[0][j].t[:n, :n], pb_, pb_.t[:n, j, :n])
                k.tt(Pm[j], Pm[j].t[:n, :n], self.ident_f, self.ident_f.t[:n, :n], Nb[0][j], Nb[0][j].t[:n, :n],
                     ALU.subtract)
            nlev = int(np.log2(L)) - 1
            cur = 0
            for lev in range(1, nlev + 1):
                nx_ = 1 - cur
                lastl = lev == nlev
                step()
                for j in hs:
                    b_, p_ = pbank(j)
                    k.mm(p_[:n, :n], [(Nb[cur][j].t[:n, :n], Lb[cur][j].t[:n, :n])], outs=[b_], ins=[Nb[cur][j], Lb[cur][j]])
                    k.cp('act', Lb[nx_][j], Lb[nx_][j].t[:n, :n], b_, p_[:n, :n])
                if not lastl:
                    step()
                    for j in hs:
                        b_, p_ = pbank(j)
                        k.mm(p_[:n, :n], [(Lb[cur][j].t[:n, :n], Nb[cur][j].t[:n, :n])], outs=[b_],
                             ins=[Nb[cur][j], Lb[cur][j]])
                        k.cp('act', Nb[nx_][j], Nb[nx_][j].t[:n, :n], b_, p_[:n, :n])
                step()
                for j in hs:
                    b_, p_ = pbank(j)
                    k.mm(p_[:n, :n], [(Lb[nx_][j].t[:n, :n], Pm[j].t[:n, :n])], outs=[b_], ins=[Lb[nx_][j], Pm[j]])
                    k.tt(Pm[j], Pm[j].t[:n, :n], Pm[j], Pm[j].t[:n, :n], b_, p_[:n, :n], ALU.add)
                cur = nx_
            if CSTOP <= 6:
                return
            step()
            for j in hs:
                b_, p_ = pbank(j)
                k.mm(p_[:n, :], [(Pm[j].t[:n, :n], vb[j].t[:n, :])], outs=[b_], ins=[Pm[j], vb[j]])
                k.cp('act', u_f[j], u_f[j].t[:n, :], b_, p_[:n, :])
            step()
            for j in hs:
                b_, p_ = pbank(j)
                k.mm(p_[:, :n], [(kbe[j].t[:n, :], Pm[j].t[:n, :n])], outs=[b_], ins=[Pm[j], kbe[j]])
                k.op('dve', nc.vector.memset, outs=[wTm[j]], ap=wTm[j].t[:, :, :n], constant=0.0)
                k.op('dve', nc.vector.memset, outs=[qeTm[j]], ap=qeTm[j].t[:, :, :n], constant=0.0)
                for c_ in range(2):
                    cs = slice(c_ * L, (c_ + 1) * L)
                    k.cp('act', wTm[j], wTm[j].t[:, c_, cs], b_, p_[:, cs])
                    k.tt(qeTm[j], qeTm[j].t[:, c_, cs], qkn[j], qkn[j].t[:, 0, cs], eGbc[j], eGbc[j].t[:, cs], ALU.mult)
            if CSTOP <= 7:
                return
            step()
            o_slots = [pbank(j) for j in hs]
            for c_ in range(2):
                Sfc, Sbc = Sf[sidx[c_]], Sb[sidx[c_]]
                step()
                ws = [pbank(j) for j in hs]
                for j in hs:
                    h = h0 + j
                    b_, p_ = ws[j]
                    k.mm(p_[:n, :], [(wTm[j].t[:, c_, :n], Sbc.t[:, h, :])], outs=[b_], ins=[wTm[j], Sbc])
                for j in hs:
                    b_, p_ = ws[j]
                    k.tt(vnew[j], vnew[j].t[:n, :], u_f[j], u_f[j].t[:n, :], b_, p_[:n, :], ALU.subtract)
                for j in hs:
                    h = h0 + j
                    b_, p_ = o_slots[j]
                    k.nc.tensor.matmul(p_[:n, :], lhsT=qeTm[j].t[:, c_, :n], rhs=Sbc.t[:, h, :], start=(c_ == 0), stop=False) if False else None
                step()
                ds = [pbank(j) for j in hs]
                for j in hs:
                    h = h0 + j
                    b_, p_ = ds[j]
                    k.mm(p_[:, :], [(kdlm[j].t[:n, c_, :], vnew[j].t[:n, :])], outs=[b_], ins=[kdlm[j], vnew[j]])
                step()
                oc = [pbank(j) for j in hs]
                for j in hs:
                    h = h0 + j
                    b_, p_ = oc[j]
                    k.mm(p_[:n, :], [(qeTm[j].t[:, c_, :n], Sbc.t[:, h, :]), (AT[j].t[:n, c_, :n], vnew[j].t[:n, :])],
                         outs=[b_], ins=[qeTm[j], Sbc, AT[j], vnew[j]])
                for j in hs:
                    b_, p_ = oc[j]
                    if c_ == 0:
                        k.cp('act', o_f[j], o_f[j].t[:n, :], b_, p_[:n, :])
                    else:
                        k.tt(o_f[j], o_f[j].t[:n, :], o_f[j], o_f[j].t[:n, :], b_, p_[:n, :], ALU.add)
                for j in hs:
                    h = h0 + j
                    b_, p_ = ds[j]
                    col = (c_ + 1) * L - 1
                    k.stt(Sfc, Sfc.t[:, h, :], Sfc, Sfc.t[:, h, :], eGbc[j], eGbc[j].t[:, col:col + 1], b_, p_[:, :],
                          ALU.mult, ALU.add)
                    k.cp('act', Sbc, Sbc.t[:, h, :], Sfc, Sfc.t[:, h, :])
            if CSTOP <= 8:
                return
            for j in hs:
                k.act(ojunk[j], ojunk[j].t[:n, :], o_f[j], o_f[j].t[:n, :], AF.Square, accum=(oss[j], oss[j].t[:n, :]))
            for j in hs:
                k.ts(oss[j], oss[j].t[:n, :], oss[j], oss[j].t[:n, :], None, 1.0 / 128.0, ALU.mult, sc2=EPS, op2=ALU.add)
            for j in hs:
                k.act(oss[j], oss[j].t[:n, :], oss[j], oss[j].t[:n, :], AF.Sqrt)
            for j in hs:
                k.op('dve', nc.vector.reciprocal, outs=[oss[j]], ins=[oss[j]], out=oss[j].t[:n, :], in_=oss[j].t[:n, :])
            for j in hs:
                k.ts(on_b[j], on_b[j].t[:n, :], o_f[j], o_f[j].t[:n, :], oss[j], oss[j].t[:n, 0:1], ALU.mult)
            pb_ = stepb()
            for j in hs:
                k.tr(pb_.t[:, j, :n], on_b[j].t[:n, :], self.ident_b.t[:n, :n], outs=[pb_], ins=[on_b[j], self.ident_b])
            for j in hs:
                k.stt(m_g, m_g.t[:, j, sl], pb_, pb_.t[:, j, :n], gpar, gpar.t[:, 32:33], zg_g, zg_g.t[:, j, sl],
                      ALU.mult, ALU.mult)
                k.tt(m_g, m_g.t[:, j, sl], m_g, m_g.t[:, j, sl], oa_g, oa_g.t[:, j, sl], ALU.add)

        groups = [(t, min(512, SEQ - t), 'p') for t in range(0, SEQ, 512)] + [(SEQ, NS, 's')]
        for h0 in range(0, NH, HB):
            for (g0, gn, kind) in groups:
                cs_ = slice(g0, g0 + gn)
                def rows(base):
                    return self.s_qkvT.t[base + h0 * 128: base + (h0 + HB) * 128, cs_].rearrange("(j p) t -> p j t", p=128)
                k.dma_group('sp', 'qk_g', [(qk_g.t[:, :, 0, :gn], rows(0)), (qk_g.t[:, :, 1, :gn], rows(2048))],
                            outs=[qk_g], ins=[self.s_qkvT])
                k.dma('sp', 'v_g', v_g.t[:, :, :gn], rows(4096), outs=[v_g], ins=[self.s_qkvT])
                def rows2(t_):
                    return t_.t[h0 * 128:(h0 + HB) * 128, cs_].rearrange("(j p) t -> p j t", p=128)
                k.dma('sp', 'zg_g', zg_g.t[:, :, :gn], rows2(self.s_zT), outs=[zg_g], ins=[self.s_zT])
                k.dma('sp', 'gb_g', gb_g.t[:, :, :gn], rows2(self.s_gbT), outs=[gb_g], ins=[self.s_gbT])
                k.dma('sp', 'oa_g', oa_g.t[:, :, :gn], rows2(self.s_oaT), outs=[oa_g], ins=[self.s_oaT])
                k.tt(zg_g, zg_g.t[:, :, :gn], zg_g, zg_g.t[:, :, :gn], gb_g, gb_g.t[:, :, :gn], ALU.mult)
                if kind == 'p':
                    for a in range(0, gn, 128):
                        run_tile(h0, g0 + a, min(128, gn - a), 64, 0, (0, 0), a)
                else:
                    run_tile(h0, g0, NS, 16, 1, (1, 2), 0)
                k.dma('sp', 'm_g', rows2(self.s_mT), m_g.t[:, :, :gn], ins=[m_g], outs=[self.s_mT])
        for i in range(3):
            self.out_toks.append(k.dma('sp', f'Sf{i}', self.o_gdn[i].rearrange("h p d -> p h d"), Sf[i].t[:, :, :],
                                       ins=[Sf[i]]))

    def phase_d(self):
        nc, k = self.nc, self.k
        T, NTL = self.T, self.NT
        CAP, NE = self.CAP, self.NE
        self.key = [k.sb(f"key{i}", [128, NTL], I32, persist=True) for i in range(2)]
        self.wgt = [k.sb(f"wgt{i}", [128, NTL], F32, persist=True) for i in range(2)]
        wo = k.sb("wo", [128, KC, D], BF16)
        wo_v = self.w_out.ap().rearrange("(kc p) c -> p kc c", p=128)
        k.dma_group('pool', 'wo', [(wo.t[:, q * 4:(q + 1) * 4, :], wo_v[:, q * 4:(q + 1) * 4, :]) for q in range(4)],
                    outs=[wo])
        gffn = k.sb("gffn", [128, D], F32)
        k.dma('sp', 'gffn', gffn.t[:, :], self.ffn_g.ap(), outs=[gffn])
        rb = k.sb("rb", [128, 72], F32)
        k.dma('sp', 'rb', rb.t[:, :], self.rbias.ap(), outs=[rb])
        mc = k.sb("mc", [128, 192], F32)
        k.dma('sp', 'mc', mc.t[:, :], self.mconst.ap(), outs=[mc])
        sut = k.sb("sut", [128, 128], BF16)
        k.cp('dve', sut, sut.t[:, :], mc, mc.t[:, 64:192])
        wrf = k.sb("wrf", [128, KC, 72], F32)
        k.dma('sp', 'wrf', wrf.t[:, :, :], self.w_rt.ap().rearrange("(kc p) c -> p kc c", p=128), outs=[wrf])
        wr_hi = k.sb("wr_hi", [128, KC, 72], BF16)
        wr_mid = k.sb("wr_mid", [128, KC, 72], BF16)
        wr_t = k.sb("wr_t", [128, KC, 72], F32)
        k.cp('dve', wr_hi, wr_hi.t[:, :, :], wrf, wrf.t[:, :, :])
        k.cp('dve', wr_t, wr_t.t[:, :, :], wr_hi, wr_hi.t[:, :, :])
        k.tt(wr_t, wr_t.t[:, :, :], wrf, wrf.t[:, :, :], wr_t, wr_t.t[:, :, :], ALU.subtract)
        k.cp('dve', wr_mid, wr_mid.t[:, :, :], wr_t, wr_t.t[:, :, :])
        carry = k.sb("carry", [128, 64], F32)
        k.op('dve', nc.vector.memset, outs=[carry], ap=carry.t[:, :], constant=0.0)
        mT = [k.sb(f"mT{i}", [128, KC, 128], BF16) for i in range(2)]
        xt = [k.sb(f"xt{i}", [128, D], F32) for i in range(2)]
        x1 = k.sb("x1", [128, D], F32)
        h2 = k.sb("h2", [128, D], F32)
        hhi = k.sb("hhi", [128, D], BF16)
        hmid = k.sb("hmid", [128, D], BF16)
        hTh = k.sb("hTh", [128, KC, 128], BF16)
        hTm = k.sb("hTm", [128, KC, 128], BF16)
        sc1 = [k.sb(f"dsc{i}", [128, 1], F32) for i in range(8)]
        lg = k.sb("lg", [128, 72], F32)
        e8 = k.sb("e8", [128, 8], F32)
        gm8 = k.sb("gm8", [128, 8], F32)
        lem = k.sb("lem", [128, 64], F32)
        m12 = [k.sb(f"m12_{i}", [128, 64], F32) for i in range(2)]
        Eb = k.sb("Eb", [128, 64], BF16)
        cntf = k.sb("cntf", [128, 64], F32)
        tmp64 = k.sb("tmp64", [128, 64], F32)
        keyf = k.sb("keyf", [128, 1], F32)
        pw = [k.ps(f"pw{i}", [128, 512], F32) for i in range(4)]
        ptb = [k.ps(f"ptb{i}", [128, 8, 128], BF16) for i in range(2)]
        pr = k.ps("pr", [128, 512], F32)
        pc = k.ps("pcnt", [128, 512], F32)
        mT_v = self.s_mT.t
        for ti in range(NTL):
            t0 = ti * 128
            n = min(128, T - t0)
            s_ = ti % 2
            k.dma('sp', f'mT{s_}', mT[s_].t[:, :, :n], mT_v[:, t0:t0 + n].rearrange("(c p) t -> p c t", p=128),
                  outs=[mT[s_]], ins=[self.s_mT])
            k.dma('sp', f'xt{s_}', xt[s_].t[:n, :], self.x_all[t0:t0 + n, :], outs=[xt[s_]])
            for cg in range(4):
                p = pw[cg]
                k.mm(p.t[:n, :], [(mT[s_].t[:, kc, :n], wo.t[:, kc, cg * 512:(cg + 1) * 512]) for kc in range(KC)],
                     outs=[p], ins=[mT[s_], wo])
                k.tt(x1, x1.t[:n, cg * 512:(cg + 1) * 512], p, p.t[:n, :], xt[s_], xt[s_].t[:n, cg * 512:(cg + 1) * 512],
                     ALU.add)
            k.dma('sp', 'x1', self.s_x1.t[t0:t0 + n, :], x1.t[:n, :], ins=[x1], outs=[self.s_x1])
            ss, rs = sc1[0], sc1[1]
            k.act(hmid, hmid.t[:n, :], x1, x1.t[:n, :], AF.Square, accum=(ss, ss.t[:n, :]))
            k.ts(rs, rs.t[:n, :], ss, ss.t[:n, :], None, 1.0 / D, ALU.mult, sc2=EPS, op2=ALU.add)
            k.act(rs, rs.t[:n, :], rs, rs.t[:n, :], AF.Sqrt)
            k.op('dve', nc.vector.reciprocal, outs=[rs], ins=[rs], out=rs.t[:n, :], in_=rs.t[:n, :])
            k.stt(h2, h2.t[:n, :], x1, x1.t[:n, :], rs, rs.t[:n, 0:1], gffn, gffn.t[:n, :], ALU.mult, ALU.mult)
            k.cp('act', hhi, hhi.t[:n, :], h2, h2.t[:n, :])
            k.tt(h2, h2.t[:n, :], h2, h2.t[:n, :], hhi, hhi.t[:n, :], ALU.subtract)
            k.cp('act', hmid, hmid.t[:n, :], h2, h2.t[:n, :])
            for (src, dst) in ((hhi, hTh), (hmid, hTm)):
                for q in range(2):
                    pb_ = ptb[q]
                    for j in range(8):
                        kc = q * 8 + j
                        k.tr(pb_.t[:, j, :n], src.t[:n, kc * 128:(kc + 1) * 128], self.ident_b.t[:n, :n], outs=[pb_],
                             ins=[src, self.ident_b])
                    k.cp('dve', dst, dst.t[:, q * 8:(q + 1) * 8, :n], pb_, pb_.t[:, :, :n])
            ops = []
            for kc in range(KC):
                ops += [(hTh.t[:, kc, :n], wr_hi.t[:, kc, :]), (hTh.t[:, kc, :n], wr_mid.t[:, kc, :]),
                        (hTm.t[:, kc, :n], wr_hi.t[:, kc, :])]
            k.mm(pr.t[:n, 0:72], ops, outs=[pr], ins=[hTh, hTm, wr_hi, wr_mid])
            k.tt(lg, lg.t[:n, :], pr, pr.t[:n, 0:72], rb, rb.t[:n, :], ALU.add)
            gmx, ngm, es, pg = sc1[2], sc1[3], sc1[4], sc1[5]
            k.op('dve', nc.vector.reduce_max, outs=[gmx], ins=[lg], out=gmx.t[:n, :], in_=lg.t[:n, 0:8], axis=AX.X)
            k.ts(ngm, ngm.t[:n, :], gmx, gmx.t[:n, :], None, -1.0, ALU.mult)
            k.act(e8, e8.t[:n, :], lg, lg.t[:n, 0:8], AF.Exp, bias=(ngm, ngm.t[:n, 0:1]), accum=(es, es.t[:n, :]))
            k.op('dve', nc.vector.reciprocal, outs=[pg], ins=[es], out=pg.t[:n, :], in_=es.t[:n, :])
            k.ts(gm8, gm8.t[:n, :], lg, lg.t[:n, 0:8], gmx, gmx.t[:n, 0:1], ALU.is_equal)
            k.ts(gm8, gm8.t[:n, :], gm8, gm8.t[:n, :], None, -1.0, ALU.add, sc2=30000.0, op2=ALU.mult)
            for g in range(8):
                k.ts(lem, lem.t[:n, g * 8:(g + 1) * 8], lg, lg.t[:n, 8 + g * 8:16 + g * 8], gm8, gm8.t[:n, g:g + 1], ALU.add)
            v1, v2 = sc1[6], sc1[7]
            k.op('dve', nc.vector.reduce_max, outs=[v1], ins=[lem], out=v1.t[:n, :], in_=lem.t[:n, :], axis=AX.X)
            k.ts(m12[0], m12[0].t[:n, :], lem, lem.t[:n, :], v1, v1.t[:n, 0:1], ALU.is_equal)
            k.stt(lem, lem.t[:n, :], m12[0], m12[0].t[:n, :], None, -60000.0, lem, lem.t[:n, :], ALU.mult, ALU.add) \
                if False else k.op('dve', nc.vector.scalar_tensor_tensor, outs=[lem], ins=[m12[0], lem], out=lem.t[:n, :],
                                   in0=m12[0].t[:n, :], scalar=-60000.0, in1=lem.t[:n, :], op0=ALU.mult, op1=ALU.add)
            k.op('dve', nc.vector.reduce_max, outs=[v2], ins=[lem], out=v2.t[:n, :], in_=lem.t[:n, :], axis=AX.X)
            k.ts(m12[1], m12[1].t[:n, :], lem, lem.t[:n, :], v2, v2.t[:n, 0:1], ALU.is_equal)
            tt_, den = sc1[0], sc1[1]
            k.tt(tt_, tt_.t[:n, :], v2, v2.t[:n, :], v1, v1.t[:n, :], ALU.subtract)
            k.act(tt_, tt_.t[:n, :], tt_, tt_.t[:n, :], AF.Exp)
            k.ts(den, den.t[:n, :], tt_, tt_.t[:n, :], None, 1.0, ALU.add)
            k.op('dve', nc.vector.reciprocal, outs=[den], ins=[den], out=den.t[:n, :], in_=den.t[:n, :])
            k.tt(self.wgt[0], self.wgt[0].t[:n, ti:ti + 1], pg, pg.t[:n, :], den, den.t[:n, :], ALU.mult)
            k.tt(self.wgt[1], self.wgt[1].t[:n, ti:ti + 1], self.wgt[0], self.wgt[0].t[:n, ti:ti + 1], tt_, tt_.t[:n, :],
                 ALU.mult)
            k.tt(tmp64, tmp64.t[:n, :], m12[0], m12[0].t[:n, :], m12[1], m12[1].t[:n, :], ALU.add)
            k.cp('dve', Eb, Eb.t[:n, :], tmp64, tmp64.t[:n, :])
            k.mm(pc.t[:n, 0:64], [(sut.t[:n, :n], Eb.t[:n, :])], outs=[pc], ins=[sut, Eb])
            k.tt(cntf, cntf.t[:n, :], pc, pc.t[:n, 0:64], carry, carry.t[:n, :], ALU.add)
            k.mm(pc.t[:, 64:128], [(self.ones_b.t[:n, :], Eb.t[:n, :])], outs=[pc], ins=[self.ones_b, Eb])
            k.tt(carry, carry.t[:, :], carry, carry.t[:, :], pc, pc.t[:, 64:128], ALU.add)
            for kk in range(2):
                rk, ov = sc1[2], sc1[3]
                k.tt(tmp64, tmp64.t[:n, :], cntf, cntf.t[:n, :], m12[kk], m12[kk].t[:n, :], ALU.mult)
                k.op('dve', nc.vector.reduce_sum, outs=[rk], ins=[tmp64], out=rk.t[:n, :], in_=tmp64.t[:n, :], axis=AX.X)
                k.tt(tmp64, tmp64.t[:n, :], mc, mc.t[:n, 0:64], m12[kk], m12[kk].t[:n, :], ALU.mult)
                k.op('dve', nc.vector.reduce_sum, outs=[keyf], ins=[tmp64], out=keyf.t[:n, :], in_=tmp64.t[:n, :], axis=AX.X)
                k.tt(keyf, keyf.t[:n, :], keyf, keyf.t[:n, :], rk, rk.t[:n, :], ALU.add)
                k.ts(ov, ov.t[:n, :], rk, rk.t[:n, :], None, float(CAP), ALU.is_ge, sc2=float(4 * self.NROW), op2=ALU.mult)
                k.tt(keyf, keyf.t[:n, :], keyf, keyf.t[:n, :], ov, ov.t[:n, :], ALU.add)
                k.cp('dve', self.key[kk], self.key[kk].t[:n, ti:ti + 1], keyf, keyf.t[:n, :])
                k.idma(f'sc{kk}', self.s_xs.t[:, :], self.key[kk].t[:n, ti:ti + 1], hhi.t[:n, :], None, self.NROW,
                       outs=[self.s_xs], ins=[hhi, self.key[kk]])

    def phase_e(self):
        nc, k = self.nc, self.k
        CAP, NE = self.CAP, self.NE
        NST = CAP // 128
        wg = [k.sb(f"wg{i}", [128, KC, 512], BF16) for i in range(2)]
        wu = [k.sb(f"wu{i}", [128, KC, 512], BF16) for i in range(2)]
        wd = [k.sb(f"wd{i}", [128, 4, D], BF16) for i in range(2)]
        xs_ = [k.sb(f"xse{i}", [128, D], BF16) for i in range(2)]
        xT = k.sb("xTe", [128, KC, CAP], BF16)
        sg = k.sb("sg", [128, CAP], BF16)
        hT = k.sb("hTe", [128, 4, CAP], BF16)
        eo = [k.sb(f"eo{i}", [128, D], F32) for i in range(2)]
        ptb = [k.ps(f"pte{i}", [128, 8, 128], BF16) for i in range(2)]
        pg_ = [k.ps(f"pge{i}", [128, 512], F32) for i in range(2)]
        pu_ = [k.ps(f"pue{i}", [128, 512], F32) for i in range(2)]
        pd_ = [k.ps(f"pde{i}", [128, 512], F32) for i in range(2)]
        cnt = [0]
        for e in range(NE):
            s_ = e % 2
            gv = self.w_gate[e].rearrange("(kc p) c -> p kc c", p=128)
            uv = self.w_up[e].rearrange("(kc p) c -> p kc c", p=128)
            dv = self.w_down[e].rearrange("(fc p) c -> p fc c", p=128)
            k.dma_group('pool', f'wg{s_}', [(wg[s_].t[:, q * 8:(q + 1) * 8, :], gv[:, q * 8:(q + 1) * 8, :]) for q in range(2)],
                        outs=[wg[s_]])
            k.dma_group('pool', f'wu{s_}', [(wu[s_].t[:, q * 8:(q + 1) * 8, :], uv[:, q * 8:(q + 1) * 8, :]) for q in range(2)],
                        outs=[wu[s_]])
            k.dma_group('pool', f'wd{s_}', [(wd[s_].t[:, q * 2:(q + 1) * 2, :], dv[:, q * 2:(q + 1) * 2, :]) for q in range(2)],
                        outs=[wd[s_]])
            for st in range(NST):
                r0 = e * CAP + st * 128
                x_ = xs_[st % 2]
                k.dma('sp', f'xse{st % 2}', x_.t[:, :], self.s_xs.t[r0:r0 + 128, :], outs=[x_], ins=[self.s_xs])
                for q in range(2):
                    pb_ = ptb[q]
                    for j in range(8):
                        kc = q * 8 + j
                        k.tr(pb_.t[:, j, :], x_.t[:, kc * 128:(kc + 1) * 128], self.ident_b.t[:, :], outs=[pb_],
                             ins=[x_, self.ident_b])
                    k.cp('dve' if q == 0 else 'act', xT, xT.t[:, q * 8:(q + 1) * 8, st * 128:(st + 1) * 128], pb_,
                         pb_.t[:, :, :])
            for fc in range(4):
                pg, pu = pg_[fc % 2], pu_[fc % 2]
                k.mm(pg.t[:, :CAP], [(wg[s_].t[:, kc, fc * 128:(fc + 1) * 128], xT.t[:, kc, :]) for kc in range(KC)],
                     outs=[pg], ins=[wg[s_], xT])
                k.mm(pu.t[:, :CAP], [(wu[s_].t[:, kc, fc * 128:(fc + 1) * 128], xT.t[:, kc, :]) for kc in range(KC)],
                     outs=[pu], ins=[wu[s_], xT])
                k.act(sg, sg.t[:, :], pg, pg.t[:, :CAP], AF.Silu)
                k.tt(hT, hT.t[:, fc, :], sg, sg.t[:, :], pu, pu.t[:, :CAP], ALU.mult)
            for st in range(NST):
                cnt[0] += 1
                o_ = eo[cnt[0] % 2]
                for cg in range(4):
                    pd = pd_[cg % 2]
                    k.mm(pd.t[:, :], [(hT.t[:, fc, st * 128:(st + 1) * 128], wd[s_].t[:, fc, cg * 512:(cg + 1) * 512])
                                      for fc in range(4)], outs=[pd], ins=[hT, wd[s_]])
                    k.cp('act' if cg % 2 else 'dve', o_, o_.t[:, cg * 512:(cg + 1) * 512], pd, pd.t[:, :])
                r0 = e * CAP + st * 128
                k.dma('sp', f'eo{cnt[0] % 2}', self.s_eo.t[r0:r0 + 128, :], o_.t[:, :], ins=[o_], outs=[self.s_eo])

    def phase_f(self):
        nc, k = self.nc, self.k
        T, NTL = self.T, self.NT
        gfin = k.sb("gfin", [128, D], F32)
        k.dma('sp', 'gfin', gfin.t[:, :], self.fin_g.ap(), outs=[gfin])
        g1 = [k.sb(f"g1_{i}", [128, D], F32) for i in range(2)]
        g2 = [k.sb(f"g2_{i}", [128, D], F32) for i in range(2)]
        x1 = [k.sb(f"x1f{i}", [128, D], F32) for i in range(2)]
        jk = k.sb("jkf", [128, D], BF16)
        ss = k.sb("ssf", [128, 1], F32)
        rs = k.sb("rsf", [128, 1], F32)
        for ti in range(NTL):
            t0 = ti * 128
            n = min(128, T - t0)
            s_ = ti % 2
            k.op('dve', nc.vector.memset, outs=[g1[s_]], ap=g1[s_].t[:n, :], constant=0.0)
            k.op('dve', nc.vector.memset, outs=[g2[s_]], ap=g2[s_].t[:n, :], constant=0.0)
            k.idma(f'g1_{s_}', g1[s_].t[:n, :], None, self.s_eo.t[:, :], self.key[0].t[:n, ti:ti + 1], self.NROW,
                   outs=[g1[s_]], ins=[self.s_eo, self.key[0]])
            k.idma(f'g2_{s_}', g2[s_].t[:n, :], None, self.s_eo.t[:, :], self.key[1].t[:n, ti:ti + 1], self.NROW,
                   outs=[g2[s_]], ins=[self.s_eo, self.key[1]])
            k.dma('sp', f'x1f{s_}', x1[s_].t[:n, :], self.s_x1.t[t0:t0 + n, :], outs=[x1[s_]], ins=[self.s_x1])
            k.stt(x1[s_], x1[s_].t[:n, :], g1[s_], g1[s_].t[:n, :], self.wgt[0], self.wgt[0].t[:n, ti:ti + 1], x1[s_],
                  x1[s_].t[:n, :], ALU.mult, ALU.add)
            k.stt(x1[s_], x1[s_].t[:n, :], g2[s_], g2[s_].t[:n, :], self.wgt[1], self.wgt[1].t[:n, ti:ti + 1], x1[s_],
                  x1[s_].t[:n, :], ALU.mult, ALU.add)
            k.act(jk, jk.t[:n, :], x1[s_], x1[s_].t[:n, :], AF.Square, accum=(ss, ss.t[:n, :]))
            k.ts(rs, rs.t[:n, :], ss, ss.t[:n, :], None, 1.0 / D, ALU.mult, sc2=EPS, op2=ALU.add)
            k.act(rs, rs.t[:n, :], rs, rs.t[:n, :], AF.Sqrt)
            k.op('dve', nc.vector.reciprocal, outs=[rs], ins=[rs], out=rs.t[:n, :], in_=rs.t[:n, :])
            k.stt(g1[s_], g1[s_].t[:n, :], x1[s_], x1[s_].t[:n, :], rs, rs.t[:n, 0:1], gfin, gfin.t[:n, :], ALU.mult,
                  ALU.mult)
            self.out_toks.append(k.dma('sp', f'yo{s_}', self.o_y[t0:t0 + n, :], g1[s_].t[:n, :], ins=[g1[s_]]))


def host_prep(inp, c, SEQ, PAST, NG=8):
    b = c % 4
    f = np.float32
    m = {}
    m["x_all"] = np.ascontiguousarray(np.concatenate(
        [inp["x_prompt"][b, :SEQ], inp["x_sample"][2 * c], inp["x_sample"][2 * c + 1]], axis=0), dtype=f)
    m["w_in"] = np.ascontiguousarray(inp["w_in"][0], dtype=f)
    m["attn_g"] = np.ascontiguousarray(inp["attn_norm_g"][0].reshape(KC, 128).T, dtype=f)
    m["qn_g"] = np.ascontiguousarray(inp["q_norm_g"][0].reshape(4, 128).T, dtype=f)
    m["kvn_g"] = np.ascontiguousarray(inp["kv_norm_g"][0].reshape(4, 128).T, dtype=f)
    m["ident"] = np.eye(128, dtype=f)
    pos = np.concatenate([np.arange(SEQ), PAST + np.arange(16), PAST + np.arange(16)])
    m["cosT"], m["sinT"] = rope_tables(pos)
    m["conv_w"] = np.ascontiguousarray(inp["conv_w"][0].reshape(4, 48, 128).transpose(2, 1, 0), dtype=f)
    m["conv0"] = np.ascontiguousarray(
        inp["state_conv"][0, 2 * c:2 * c + 2].reshape(2, 3, 48, 128).transpose(3, 2, 0, 1), dtype=f)
    m["w_uq"] = np.ascontiguousarray(inp["w_uq"][0].reshape(QL, NH * 192), dtype=f)
    m["w_uk"] = np.ascontiguousarray(inp["w_uk"][0].reshape(KVL, NH * 128), dtype=f)
    m["w_uv"] = np.ascontiguousarray(inp["w_uv"][0].reshape(KVL, NH * 128), dtype=f)
    m["cache_ckv"] = np.ascontiguousarray(inp["cache_ckv"][0, 2 * c:2 * c + 2, :PAST], dtype=f)
    m["cache_kr"] = np.ascontiguousarray(inp["cache_k_rope"][0, 2 * c:2 * c + 2, :PAST], dtype=f)
    m["gpar"] = np.ascontiguousarray(np.concatenate([
        np.broadcast_to(inp["a_log"][0][None, :], (128, 16)), np.broadcast_to(inp["dt_bias"][0][None, :], (128, 16)),
        inp["gdn_norm_g"][0].reshape(128, 1)], axis=1), dtype=f)
    gmk = np.zeros((2, 128, 514), f)
    for v, (n, L) in enumerate(((128, 64), (32, 16))):
        idx = np.arange(n)
        same = (idx[:, None] // L) == (idx[None, :] // L)
        tri = same & (idx[:, None] <= idx[None, :])
        gmk[v, :n, 0:n] = tri
        gmk[v, :, 128:256] = -30000.0
        gmk[v, :n, 128:128 + n] = np.where(tri, 0.0, -30000.0)
        gmk[v, :n, 256:256 + n] = same & (idx[:, None] < idx[None, :])
        gmk[v, :n, 384] = (idx // L) == 0
        gmk[v, :n, 385] = (idx // L) == 1
        gmk[v, :n, 386:386 + n] = same
    m["gmask"] = gmk
    m["state_gdn"] = np.ascontiguousarray(inp["state_gdn"][0, 2 * c:2 * c + 2], dtype=f)
    NE = NG * 8
    m["w_out"] = np.ascontiguousarray(inp["w_out"][0], dtype=f)
    m["ffn_g"] = np.ascontiguousarray(np.broadcast_to(inp["ffn_norm_g"][0][None, :], (128, D)), dtype=f)
    m["fin_g"] = np.ascontiguousarray(np.broadcast_to(inp["final_norm_g"][None, :], (128, D)), dtype=f)
    wr = inp["w_router"][0][:NG].transpose(1, 0, 2).reshape(D, NG * 8)
    wrt = np.zeros((D, 72), f)
    wrt[:, :NG] = inp["w_group"][0][:, :NG]
    wrt[:, 8:8 + NG * 8] = wr
    m["w_rt"] = wrt
    rbv = np.full((72,), -30000.0, f)
    rbv[:NG] = inp["b_group"][0][:NG]
    rbv[8:8 + NG * 8] = inp["b_router"][0][:NG].reshape(-1)
    m["rbias"] = np.ascontiguousarray(np.broadcast_to(rbv[None, :], (128, 72)), dtype=f)
    mcst = np.zeros((128, 192), f)
    mcst[:, :64] = (np.arange(64) * 256)[None, :]
    ii = np.arange(128)
    mcst[:, 64:] = (ii[:, None] < ii[None, :])
    m["mconst"] = mcst
    m["w_gate"] = inp["w_gate"][0][:NE]
    m["w_up"] = inp["w_up"][0][:NE]
    m["w_down"] = inp["w_down"][0][:NE]
    dm = np.zeros((128, 128), f)
    dm[:64, 64:] = -30000.0
    m["dmask"] = dm
    return m


SEQ_FULL, PAST_FULL = 4096, 4096
_PROG = {}


def kernel(**inputs):
    inp = {k_: np.asarray(v) for k_, v in inputs.items()}
    phases = "ABCD"
    if phases not in _PROG:
        _PROG[phases] = Prog(SEQ_FULL, PAST_FULL, phases=phases)
    P = _PROG[phases]
    maps = []
    for c in range(8):
        m = host_prep(inp, c, SEQ_FULL, PAST_FULL)
        maps.append({k_: v for k_, v in m.items() if k_ in P.inp})
    res = run_bass_kernel_spmd(P.nc, maps, core_ids=list(range(8))).results
    S = SEQ_FULL
    f = np.float32
    y_prompt = np.stack([res[b]["o_y"][:S] for b in range(4)])
    y_sample = np.stack([res[c]["o_y"][S + 16 * i:S + 16 * (i + 1)] for c in range(8) for i in range(2)])
    ckv_p = np.stack([res[b]["o_ckv"][:S] for b in range(4)])[None]
    kr_p = np.stack([res[b]["o_kr"][:S] for b in range(4)])[None]
    gdn_p = np.stack([res[b]["o_gdn"][0] for b in range(4)])[None]
    conv_p = np.stack([res[b]["o_conv"][0] for b in range(4)])[None]
    ckv_s = np.stack([res[c]["o_ckv"][S + 16 * i:S + 16 * (i + 1)] for c in range(8) for i in range(2)])[None]
    kr_s = np.stack([res[c]["o_kr"][S + 16 * i:S + 16 * (i + 1)] for c in range(8) for i in range(2)])[None]
    gdn_s = np.stack([res[c]["o_gdn"][1 + i] for c in range(8) for i in range(2)])[None]
    conv_s = np.stack([res[c]["o_conv"][1 + i] for c in range(8) for i in range(2)])[None]
    return tuple(np.ascontiguousarray(a, dtype=f) for a in
                 (y_prompt, y_sample, ckv_p, kr_p, gdn_p, conv_p, ckv_s, kr_s, gdn_s, conv_s))
```

```python
import numpy as np
from contextlib import ExitStack
import ml_dtypes
import concourse.bass as bass
import concourse.mybir as mybir
from concourse.bass_utils import run_bass_kernel_spmd

F32 = mybir.dt.float32
BF16 = mybir.dt.bfloat16
I32 = mybir.dt.int32
AF = mybir.ActivationFunctionType
ALU = mybir.AluOpType
AX = mybir.AxisListType

D = 2048
KC = D // 128
EPS = 1e-6
NH = 16
QL = 512
KVL = 512
RD = 64
GQKV = 6144
IN_TOTAL = 13408
C_QL, C_KV, C_KR, C_QKV, C_A, C_B, C_Z, C_GA, C_GB = 0, 512, 1024, 1088, 7232, 7248, 7264, 9312, 11360
NS = 32
import os
CSTOP = float(os.environ.get('CSTOP', '99'))


class Buf:
    def __init__(self, t, psum=False):
        self.t = t
        self.w = None
        self.r = []
        self.psum = psum


class Ctx:
    def __init__(self, nc):
        self.nc = nc
        self.E = dict(pe=nc.tensor, act=nc.scalar, dve=nc.vector, pool=nc.gpsimd, sp=nc.sync)
        self.sems = {}
        self.cnt = {}
        self.seen = {}
        for e in self.E:
            self.newsem(e)
        self.n_inst = 0
        self.gstack = ExitStack()
        self.pstack = ExitStack()
        self.mstack = ExitStack()

    def newsem(self, name):
        self.sems[name] = self.nc.alloc_semaphore(name)
        self.cnt[name] = 0

    def sb(self, name, shape, dt, persist=False):
        st = self.gstack if persist is True else (self.mstack if persist == 'mid' else self.pstack)
        return Buf(st.enter_context(self.nc.sbuf_tensor("sb_" + name, list(shape), dt)))

    def ps(self, name, shape, dt=F32):
        return Buf(self.pstack.enter_context(self.nc.psum_tensor("ps_" + name, list(shape), dt)), psum=True)

    def barrier(self):
        toks = [(n, v) for n, v in self.cnt.items() if v > 0]
        for e in self.E:
            self.wait(e, toks)

    def end_phase(self):
        self.barrier()
        self.pstack.close()
        self.pstack = ExitStack()

    def dram(self, name, shape, dt):
        if getattr(self, 'debug', False):
            return Buf(self.nc.dram_tensor(name, list(shape), dt, kind="ExternalOutput"))
        return Buf(self.nc.dram_tensor(name, list(shape), dt))

    def wait(self, eng, toks):
        for t in toks:
            if t is None:
                continue
            name, val = t
            if self.seen.get((eng, name), 0) >= val:
                continue
            self.E[eng].wait_ge(self.sems[name], val)
            self.seen[(eng, name)] = val

    def _deps(self, eng, outs, ins, extra=()):
        toks = list(extra)
        for b in ins:
            toks.append(b.w)
            if b.psum:
                toks.extend(t for t in b.r if t[0] != eng)
        for b in outs:
            toks.append(b.w)
            toks.extend(b.r)
        self.wait(eng, toks)

    def _mark(self, tok, outs, ins):
        for b in ins:
            b.r.append(tok)
            if len(b.r) > 24:
                b.r = b.r[-24:] if False else self._compact(b.r)
        for b in outs:
            b.w = tok
            b.r = []

    @staticmethod
    def _compact(r):
        best = {}
        for n, v in r:
            if best.get(n, 0) < v:
                best[n] = v
        return list(best.items())

    def op(self, eng, fn, outs=(), ins=(), deps=(), **kw):
        self._deps(eng, outs, ins, deps)
        inst = fn(**kw)
        self.cnt[eng] += 1
        inst.then_inc(self.sems[eng], 1)
        tok = (eng, self.cnt[eng])
        self._mark(tok, outs, ins)
        self.n_inst += 1
        return tok

    def mm(self, out, ops, outs, ins, transpose=False):
        self._deps('pe', outs, ins)
        n = len(ops)
        inst = None
        for i, (l, r) in enumerate(ops):
            inst = self.nc.tensor.matmul(out, lhsT=l, rhs=r, start=(i == 0), stop=(i == n - 1))
            self.n_inst += 1
        self.cnt['pe'] += 1
        inst.then_inc(self.sems['pe'], 1)
        tok = ('pe', self.cnt['pe'])
        self._mark(tok, outs, ins)
        return tok

    def tr(self, out, in_, ident, outs, ins):
        self._deps('pe', outs, ins)
        inst = self.nc.tensor.transpose(out=out, in_=in_, identity=ident)
        self.cnt['pe'] += 1
        inst.then_inc(self.sems['pe'], 1)
        tok = ('pe', self.cnt['pe'])
        self._mark(tok, outs, ins)
        self.n_inst += 1
        return tok

    def dma(self, eng, sem, out, in_, outs=(), ins=(), deps=(), **kw):
        if sem not in self.sems:
            self.newsem(sem)
        self._deps(eng, outs, ins, deps)
        inst = self.E[eng].dma_start(out=out, in_=in_, **kw)
        self.cnt[sem] += 16
        inst.then_inc(self.sems[sem], 16)
        tok = (sem, self.cnt[sem])
        self._mark(tok, outs, ins)
        self.n_inst += 1
        return tok

    def tt(self, ob, o, ab, a, bb, b, op, eng='dve'):
        fn = self.nc.vector.tensor_tensor if eng == 'dve' else self.nc.gpsimd.tensor_tensor
        return self.op(eng, fn, outs=[ob], ins=[ab, bb], out=o, in0=a, in1=b, op=op)

    def ts(self, ob, o, ab, a, sbuf, sc, op, sc2=None, op2=None, eng='dve'):
        fn = self.nc.vector.tensor_scalar if eng == 'dve' else self.nc.gpsimd.tensor_scalar
        ins = [ab] + ([sbuf] if sbuf is not None else [])
        if op2 is None:
            return self.op(eng, fn, outs=[ob], ins=ins, out=o, in0=a, scalar1=sc, scalar2=None, op0=op)
        return self.op(eng, fn, outs=[ob], ins=ins, out=o, in0=a, scalar1=sc, scalar2=sc2, op0=op, op1=op2)

    def stt(self, ob, o, ab, a, sbuf, sc, bb, b, op0, op1):
        return self.op('dve', self.nc.vector.scalar_tensor_tensor, outs=[ob], ins=[ab, sbuf, bb], out=o, in0=a,
                       scalar=sc, in1=b, op0=op0, op1=op1)

    def act(self, ob, o, ib, i, func, bias=None, scale=1.0, accum=None):
        ins = [ib]
        kw = dict(out=o, in_=i, func=func, scale=scale)
        outs = [ob]
        if bias is not None:
            ins.append(bias[0])
            kw['bias'] = bias[1]
        if accum is not None:
            outs.append(accum[0])
            kw['accum_out'] = accum[1]
        return self.op('act', self.nc.scalar.activation, outs=outs, ins=ins, **kw)

    def cp(self, eng, ob, o, ib, i):
        if eng == 'act':
            return self.op('act', self.nc.scalar.copy, outs=[ob], ins=[ib], out=o, in_=i)
        fn = self.nc.vector.tensor_copy if eng == 'dve' else self.nc.gpsimd.tensor_copy
        return self.op(eng, fn, outs=[ob], ins=[ib], out=o, in_=i)

    def idma(self, sem, out, out_off, in_, in_off, nrows, outs=(), ins=()):
        if sem not in self.sems:
            self.newsem(sem)
        self._deps('pool', outs, ins)
        oo = bass.IndirectOffsetOnAxis(ap=out_off, axis=0) if out_off is not None else None
        io = bass.IndirectOffsetOnAxis(ap=in_off, axis=0) if in_off is not None else None
        if not hasattr(self, 'breg'):
            self.breg = {}
        if nrows not in self.breg:
            r = self.nc.gpsimd.alloc_register()
            self.nc.gpsimd.reg_mov(r, nrows - 1)
            self.breg[nrows] = r
        inst = self.nc.gpsimd.indirect_dma_start(out=out, out_offset=oo, in_=in_, in_offset=io,
                                                 bounds_check=self.breg[nrows], oob_is_err=False)
        self.cnt[sem] += 16
        inst.then_inc(self.sems[sem], 16)
        tok = (sem, self.cnt[sem])
        self._mark(tok, outs, ins)
        self.n_inst += 1
        return tok

    def dma_group(self, eng, sem, pairs, outs=(), ins=(), deps=(), **kw):
        if sem not in self.sems:
            self.newsem(sem)
        self._deps(eng, outs, ins, deps)
        for o, i in pairs:
            inst = self.E[eng].dma_start(out=o, in_=i, **kw)
            self.cnt[sem] += 16
            inst.then_inc(self.sems[sem], 16)
            self.n_inst += 1
        tok = (sem, self.cnt[sem])
        self._mark(tok, outs, ins)
        return tok


def rope_tables(pos):
    half = RD // 2
    inv = (10000.0 ** (-np.arange(half, dtype=np.float32) / half)).astype(np.float32)
    ang = pos.astype(np.float32)[:, None] * inv[None, :]
    cos = np.cos(ang).astype(np.float32)
    sin = np.sin(ang).astype(np.float32)
    cosT = np.concatenate([cos, cos], axis=1).T.copy()
    sinT = np.concatenate([sin, sin], axis=1).T.copy()
    return cosT, sinT


class Prog:
    def __init__(self, SEQ, PAST, NG=8, phases="ABCDEF", debug=False):
        self.SEQ, self.PAST, self.NG = SEQ, PAST, NG
        self.debug = debug
        self.T = SEQ + NS
        self.NT = (self.T + 127) // 128
        self.phases = phases
        nc = bass.Bass("TRN2", target_bir_lowering=False)
        self.nc = nc
        self.k = Ctx(nc)
        self.k.debug = debug
        self.inp = {}
        self.out = {}
        self.build()

    def din(self, name, shape, dt=F32):
        t = self.nc.dram_tensor(name, list(shape), dt, kind="ExternalInput")
        self.inp[name] = t
        return t

    def dout(self, name, shape, dt=F32):
        t = self.nc.dram_tensor(name, list(shape), dt, kind="ExternalOutput")
        self.out[name] = t
        return t

    def build(self):
        nc, k = self.nc, self.k
        T, SEQ = self.T, self.SEQ
        self.x_all = self.din("x_all", [T, D])
        self.w_in = self.din("w_in", [D, IN_TOTAL])
        self.attn_g = self.din("attn_g", [128, KC])
        self.qn_g = self.din("qn_g", [128, 4])
        self.kvn_g = self.din("kvn_g", [128, 4])
        self.ident_in = self.din("ident", [128, 128])
        self.cosT = self.din("cosT", [RD, T])
        self.sinT = self.din("sinT", [RD, T])
        self.conv_w = self.din("conv_w", [128, 48, 4])
        self.conv0 = self.din("conv0", [128, 48, 2, 3])
        self.w_uq = self.din("w_uq", [QL, NH * 192])
        self.w_uk = self.din("w_uk", [KVL, NH * 128])
        self.w_uv = self.din("w_uv", [KVL, NH * 128])
        self.cache_ckv = self.din("cache_ckv", [2, self.PAST, KVL])
        self.cache_kr = self.din("cache_kr", [2, self.PAST, RD])
        self.dmask_in = self.din("dmask", [128, 128])
        self.gpar = self.din("gpar", [128, 33])
        self.gmask = self.din("gmask", [2, 128, 514])
        self.state_gdn = self.din("state_gdn", [2, NH, 128, 128])
        NE = self.NG * 8
        self.NE = NE
        self.CAP = 256
        self.NROW = NE * self.CAP + 128
        self.w_out = self.din("w_out", [D, D])
        self.ffn_g = self.din("ffn_g", [128, D])
        self.fin_g = self.din("fin_g", [128, D])
        self.w_rt = self.din("w_rt", [D, 8 + 64])
        self.rbias = self.din("rbias", [128, 72])
        self.mconst = self.din("mconst", [128, 64 + 128])
        self.w_gate = self.din("w_gate", [NE, D, 512])
        self.w_up = self.din("w_up", [NE, D, 512])
        self.w_down = self.din("w_down", [NE, 512, D])
        self.o_y = self.dout("o_y", [T, D])
        self.o_ckv = self.dout("o_ckv", [T, KVL])
        self.o_kr = self.dout("o_kr", [T, RD])
        self.o_conv = self.dout("o_conv", [3, 3, GQKV])
        self.o_gdn = self.dout("o_gdn", [3, NH, 128, 128])
        self.s_qlT = k.dram("s_qlT", [QL, T], BF16)
        self.s_qkvT = k.dram("s_qkvT", [GQKV, T], BF16)
        self.s_zT = k.dram("s_zT", [D, T], BF16)
        self.s_gaT = k.dram("s_gaT", [D, T], BF16)
        self.s_gbT = k.dram("s_gbT", [D, T], BF16)
        self.s_ab = k.dram("s_ab", [T, 32], F32)
        self.s_oaT = k.dram("s_oaT", [D, T], BF16)
        self.s_mT = k.dram("s_mT", [D, T], BF16)
        self.s_x1 = k.dram("s_x1", [T, D], F32)
        self.s_xs = k.dram("s_xs", [self.NROW, D], BF16)
        self.s_eo = k.dram("s_eo", [self.NROW, D], F32)
        self.ident_f = k.sb("ident_f", [128, 128], F32, persist=True)
        self.ident_b = k.sb("ident_b", [128, 128], BF16, persist=True)
        self.ones_b = k.sb("ones_b", [128, 128], BF16, persist=True)
        self.ckvT = k.sb("ckvT", [128, 4, T], BF16, persist='mid')
        self.krT = k.sb("krT", [RD, T], BF16, persist='mid')
        k.dma('sp', 'c_ident', self.ident_f.t[:, :], self.ident_in.ap(), outs=[self.ident_f])
        k.op('dve', nc.vector.tensor_copy, outs=[self.ident_b], ins=[self.ident_f],
             out=self.ident_b.t[:, :], in_=self.ident_f.t[:, :])
        k.op('dve', nc.vector.memset, outs=[self.ones_b], ap=self.ones_b.t[:, :], constant=1.0)
        self.out_toks = []
        if "A" in self.phases:
            self.phase_a()
            k.end_phase()
        if "B" in self.phases:
            self.phase_b()
            k.end_phase()
        k.mstack.close()
        if "C" in self.phases:
            self.phase_c()
            k.end_phase()
        if "D" in self.phases:
            self.phase_d()
            k.end_phase()
            self.phase_e()
            k.end_phase()
            self.phase_f()
            k.end_phase()
        k.wait('sp', self.out_toks)

    def token_groups(self):
        g = []
        for s in range(0, self.SEQ, 512):
            g.append((s, min(512, self.SEQ - s)))
        g.append((self.SEQ, NS))
        return g

    def phase_a(self):
        nc, k = self.nc, self.k
        T, SEQ = self.T, self.SEQ
        ag = k.sb("ag", [128, KC], F32)
        qg = k.sb("qg", [128, 4], F32)
        kvg = k.sb("kvg", [128, 4], F32)
        cw = k.sb("cw", [128, 48, 4], F32)
        c0 = k.sb("c0", [128, 48, 2, 3], F32)
        histP = k.sb("histP", [128, 48, 3], F32)
        k.dma('sp', 'c_ag', ag.t[:, :], self.attn_g.ap(), outs=[ag])
        k.dma('sp', 'c_qg', qg.t[:, :], self.qn_g.ap(), outs=[qg])
        k.dma('sp', 'c_kvg', kvg.t[:, :], self.kvn_g.ap(), outs=[kvg])
        k.dma('sp', 'c_cw', cw.t[:, :, :], self.conv_w.ap(), outs=[cw])
        k.dma('sp', 'c_c0', c0.t[:, :, :, :], self.conv0.ap(), outs=[c0])
        k.op('dve', nc.vector.memset, outs=[histP], ap=histP.t[:, :, :], constant=0.0)
        for g_ in (qg, kvg):
            k.op('dve', nc.vector.tensor_scalar, outs=[g_], ins=[g_], out=g_.t[:, :], in0=g_.t[:, :],
                 scalar1=float(np.sqrt(512.0)), scalar2=None, op0=ALU.mult)

        HT = max(128, (SEQ // 2 + 511) // 512 * 512) if SEQ > 512 else SEQ
        halves = [(0, HT)] if HT >= SEQ else [(0, HT), (HT, SEQ)]
        TH = max(h[1] - h[0] for h in halves) + NS
        xnT = k.sb("xnT", [128, KC, TH], BF16)
        xs = [k.sb(f"xs{i}", [128, D], F32) for i in range(2)]
        xb = [k.sb(f"xb{i}", [128, D], BF16) for i in range(2)]
        ssq = [k.sb(f"ssq{i}", [128, 1], F32) for i in range(2)]
        rstd = [k.sb(f"rstd{i}", [128, 1], F32) for i in range(2)]
        ptr = [k.ps(f"ptr{i}", [128, 4, 128], BF16) for i in range(2)]
        pb = [k.ps(f"pb{i}", [128, 512], F32) for i in range(6)]
        self.pb, self.ptr = pb, ptr
        wb = [k.sb(f"wb{i}", [128, KC, 512], BF16) for i in range(2)]
        ckvT, krT = self.ckvT, self.krT
        cst = [k.sb(f"cst{i}", [128, 515], F32) for i in range(2)]
        acc = [k.sb(f"cacc{i}", [128, 512], F32) for i in range(2)]
        ob = [k.sb(f"ob{i}", [128, 512], BF16) for i in range(4)]
        of = [k.sb(f"of{i}", [128, 512], F32) for i in range(4)]
        sq = [k.sb(f"sq{i}", [128, 512], BF16) for i in range(4)]
        rbc = k.sb("rbc", [128, 512], F32)
        cs_t = k.sb("cs_t", [RD, 2, 512], F32)
        otr = [k.sb(f"otr{i}", [128, 512], F32) for i in range(2)]
        wkr = k.sb("wkr", [128, KC, 128], BF16)
        wabt = k.sb("wabt", [128, KC, 32], BF16)
        cnt = dict(ob=0, of=0, cst=0, pb=0, otr=0, sq=0)

        def nxt(name, lst):
            cnt[name] += 1
            return lst[cnt[name] % len(lst)]

        blocks = [(C_QL, 512, 'ql'), (C_KV, 512, 'kv'), (C_KR, 64, 'kr')]
        blocks += [(C_QKV + i * 512, 512, 'qkv') for i in range(12)]
        blocks += [(C_A, 32, 'ab')]
        blocks += [(C_Z + i * 512, 512, 'z') for i in range(4)]
        blocks += [(C_GA + i * 512, 512, 'ga') for i in range(4)]
        blocks += [(C_GB + i * 512, 512, 'gb') for i in range(4)]
        w_view = self.w_in.ap().rearrange("(kc p) c -> p kc c", p=128)

        for hi, (h0, h1) in enumerate(halves):
            last = hi == len(halves) - 1
            ranges = [(h0, h1)] + ([(SEQ, SEQ + NS)] if last else [])
            loc = {}
            nloc = 0
            for (a, b) in ranges:
                for t in range(a, b, 128):
                    loc[t] = nloc
                    nloc += min(128, b - t)
            tiles = [(t, min(128, b - t)) for (a, b) in ranges for t in range(a, b, 128)]
            for ti, (t0, n) in enumerate(tiles):
                s_ = ti % 2
                l0 = loc[t0]
                k.dma('sp', f'xs{s_}', xs[s_].t[:n, :], self.x_all[t0:t0 + n, :], outs=[xs[s_]])
                k.op('act', nc.scalar.activation, outs=[xb[s_], ssq[s_]], ins=[xs[s_]],
                     out=xb[s_].t[:n, :], in_=xs[s_].t[:n, :], func=AF.Square, accum_out=ssq[s_].t[:n, :])
                k.op('dve', nc.vector.tensor_scalar, outs=[rstd[s_]], ins=[ssq[s_]],
                     out=rstd[s_].t[:n, :], in0=ssq[s_].t[:n, :], scalar1=1.0 / D, scalar2=EPS,
                     op0=ALU.mult, op1=ALU.add)
                k.op('act', nc.scalar.activation, outs=[rstd[s_]], ins=[rstd[s_]],
                     out=rstd[s_].t[:n, :], in_=rstd[s_].t[:n, :], func=AF.Sqrt)
                k.op('dve', nc.vector.reciprocal, outs=[rstd[s_]], ins=[rstd[s_]],
                     out=rstd[s_].t[:n, :], in_=rstd[s_].t[:n, :])
                k.op('dve', nc.vector.tensor_scalar, outs=[xb[s_]], ins=[xs[s_], rstd[s_]],
                     out=xb[s_].t[:n, :], in0=xs[s_].t[:n, :], scalar1=rstd[s_].t[:n, 0:1], scalar2=None,
                     op0=ALU.mult)
                for q4 in range(KC // 4):
                    p = ptr[q4 % 2]
                    for j in range(4):
                        kc = q4 * 4 + j
                        k.tr(p.t[:, j, :n], xb[s_].t[:n, kc * 128:(kc + 1) * 128], self.ident_b.t[:n, :n],
                             outs=[p], ins=[xb[s_], self.ident_b])
                    for j in range(4):
                        kc = q4 * 4 + j
                        if j % 2 == 0:
                            k.op('dve', nc.vector.tensor_scalar, outs=[xnT], ins=[p, ag],
                                 out=xnT.t[:, kc, l0:l0 + n], in0=p.t[:, j, :n], scalar1=ag.t[:, kc:kc + 1],
                                 scalar2=None, op0=ALU.mult)
                        else:
                            k.op('act', nc.scalar.activation, outs=[xnT], ins=[p, ag],
                                 out=xnT.t[:, kc, l0:l0 + n], in_=p.t[:, j, :n], func=AF.Copy,
                                 scale=ag.t[:, kc:kc + 1])
            groups = []
            for t in range(h0, h1, 512):
                groups.append((t, min(512, h1 - t), loc[t - (t - h0) % 128] + (t - h0) % 128 if False else loc[h0] + (t - h0), 'p'))
            if last:
                groups.append((SEQ, 16, loc[SEQ], 's0'))
                groups.append((SEQ + 16, 16, loc[SEQ] + 16, 's1'))

            def chan_mm(ps_, wbuf, c_lo, ncol, l0, n):
                ops = [(wbuf.t[:, kc, c_lo:c_lo + ncol], xnT.t[:, kc, l0:l0 + n]) for kc in range(KC)]
                return k.mm(ps_.t[:ncol, :n], ops, outs=[ps_], ins=[wbuf, xnT])

            for bi, (c_lo, ncol, kind) in enumerate(blocks):
                if kind == 'kr':
                    wbuf = wkr
                    k.dma('pool', 'wkr', wkr.t[:, :, 0:64], w_view[:, :, c_lo:c_lo + 64], outs=[wkr])
                    k.op('dve', nc.vector.tensor_scalar, outs=[wkr], ins=[wkr], out=wkr.t[:, :, 64:96],
                         in0=wkr.t[:, :, 32:64], scalar1=-1.0, scalar2=None, op0=ALU.mult)
                    k.op('dve', nc.vector.tensor_copy, outs=[wkr], ins=[wkr], out=wkr.t[:, :, 96:128],
                         in_=wkr.t[:, :, 0:32])
                elif kind == 'ab':
                    wbuf = wabt
                    k.dma('pool', 'wabt', wabt.t[:, :, :], w_view[:, :, c_lo:c_lo + 32], outs=[wabt])
                else:
                    wbuf = wb[bi % 2]
                    k.dma_group('pool', f'wb{bi % 2}',
                                [(wbuf.t[:, q * 4:(q + 1) * 4, :], w_view[:, q * 4:(q + 1) * 4, c_lo:c_lo + 512])
                                 for q in range(4)], outs=[wbuf])
                if kind in ('ql', 'kv'):
                    gain = qg if kind == 'ql' else kvg
                    for (t0, n, l0, sk) in groups:
                        pss = [pb[j] for j in range(4)]
                        for j in range(4):
                            chan_mm(pss[j], wbuf, j * 128, 128, l0, n)
                        ssb = pb[4]
                        for j in range(4):
                            k.op('act', nc.scalar.activation, outs=[sq[j]], ins=[pss[j]], out=sq[j].t[:, :n],
                                 in_=pss[j].t[:, :n], func=AF.Square)
                        k.mm(ssb.t[:, :n], [(self.ones_b.t[:, :], sq[j].t[:, :n]) for j in range(4)],
                             outs=[ssb], ins=[self.ones_b] + sq)
                        k.op('dve', nc.vector.tensor_scalar, outs=[rbc], ins=[ssb], out=rbc.t[:, :n],
                             in0=ssb.t[:, :n], scalar1=512.0 * EPS, scalar2=None, op0=ALU.add)
                        k.op('act', nc.scalar.activation, outs=[rbc], ins=[rbc], out=rbc.t[:, :n],
                             in_=rbc.t[:, :n], func=AF.Sqrt)
                        k.op('dve', nc.vector.reciprocal, outs=[rbc], ins=[rbc], out=rbc.t[:, :n], in_=rbc.t[:, :n])
                        for j in range(4):
                            if kind == 'ql':
                                o_ = nxt('ob', ob)
                                k.op('dve', nc.vector.scalar_tensor_tensor, outs=[o_], ins=[pss[j], gain, rbc],
                                     out=o_.t[:, :n], in0=pss[j].t[:, :n], scalar=gain.t[:, j:j + 1],
                                     in1=rbc.t[:, :n], op0=ALU.mult, op1=ALU.mult)
                                k.dma('sp', f'ob{cnt["ob"] % 4}', self.s_qlT.t[j * 128:(j + 1) * 128, t0:t0 + n],
                                      o_.t[:, :n], ins=[o_], outs=[self.s_qlT])
                            else:
                                k.op('dve', nc.vector.scalar_tensor_tensor, outs=[of[j]], ins=[pss[j], gain, rbc],
                                     out=of[j].t[:, :n], in0=pss[j].t[:, :n], scalar=gain.t[:, j:j + 1],
                                     in1=rbc.t[:, :n], op0=ALU.mult, op1=ALU.mult)
                                k.op('act', nc.scalar.copy, outs=[ckvT], ins=[of[j]], out=ckvT.t[:, j, t0:t0 + n],
                                     in_=of[j].t[:, :n])
                        if kind == 'kv':
                            for a in range(0, n, 128):
                                m = min(128, n - a)
                                pt = pb[5]
                                for j in range(4):
                                    k.tr(pt.t[:m, j * 128:(j + 1) * 128], of[j].t[:, a:a + m], self.ident_f.t[:, :],
                                         outs=[pt], ins=[of[j], self.ident_f])
                                o_ = nxt('otr', otr)
                                k.op('act', nc.scalar.copy, outs=[o_], ins=[pt], out=o_.t[:m, :], in_=pt.t[:m, :])
                                self.out_toks.append(k.dma('sp', f'otr{cnt["otr"] % 2}',
                                                           self.o_ckv[t0 + a:t0 + a + m, :], o_.t[:m, :], ins=[o_]))
                elif kind == 'kr':
                    for (t0, n, l0, sk) in groups:
                        p1, p2 = pb[4], pb[5]
                        chan_mm(p1, wbuf, 0, 64, l0, n)
                        chan_mm(p2, wbuf, 64, 64, l0, n)
                        k.dma_group('sp', 'cs_t', [(cs_t.t[:, 0, :n], self.cosT[:, t0:t0 + n]),
                                                   (cs_t.t[:, 1, :n], self.sinT[:, t0:t0 + n])], outs=[cs_t])
                        o1 = nxt('of', of)
                        o2 = nxt('of', of)
                        k.op('dve', nc.vector.tensor_tensor, outs=[o1], ins=[p1, cs_t], out=o1.t[:RD, :n],
                             in0=p1.t[:RD, :n], in1=cs_t.t[:, 0, :n], op=ALU.mult)
                        k.op('dve', nc.vector.tensor_tensor, outs=[o2], ins=[p2, cs_t], out=o2.t[:RD, :n],
                             in0=p2.t[:RD, :n], in1=cs_t.t[:, 1, :n], op=ALU.mult)
                        k.op('dve', nc.vector.tensor_tensor, outs=[o1], ins=[o1, o2], out=o1.t[:RD, :n],
                             in0=o1.t[:RD, :n], in1=o2.t[:RD, :n], op=ALU.add)
                        k.op('act', nc.scalar.copy, outs=[krT], ins=[o1], out=krT.t[:, t0:t0 + n], in_=o1.t[:RD, :n])
                        for a in range(0, n, 128):
                            m = min(128, n - a)
                            pt = pb[3]
                            k.tr(pt.t[:m, :RD], o1.t[:RD, a:a + m], self.ident_f.t[:RD, :RD], outs=[pt],
                                 ins=[o1, self.ident_f])
                            o_ = nxt('otr', otr)
                            k.op('act', nc.scalar.copy, outs=[o_], ins=[pt], out=o_.t[:m, :RD], in_=pt.t[:m, :RD])
                            self.out_toks.append(k.dma('sp', f'otr{cnt["otr"] % 2}', self.o_kr[t0 + a:t0 + a + m, :],
                                                       o_.t[:m, :RD], ins=[o_]))
                elif kind == 'ab':
                    for (a, b) in ranges:
                        for t0 in range(a, b, 128):
                            n = min(128, b - t0)
                            l0 = loc[t0]
                            pt = pb[5]
                            ops = [(xnT.t[:, kc, l0:l0 + n], wbuf.t[:, kc, :]) for kc in range(KC)]
                            k.mm(pt.t[:n, :32], ops, outs=[pt], ins=[wbuf, xnT])
                            o_ = nxt('otr', otr)
                            k.op('act', nc.scalar.copy, outs=[o_], ins=[pt], out=o_.t[:n, :32], in_=pt.t[:n, :32])
                            k.dma('sp', f'otr{cnt["otr"] % 2}', self.s_ab.t[t0:t0 + n, :], o_.t[:n, :32],
                                  ins=[o_], outs=[self.s_ab])
                else:
                    for j in range(4):
                        ch = (c_lo - {'qkv': C_QKV, 'z': C_Z, 'ga': C_GA, 'gb': C_GB}[kind]) // 128 + j
                        for (t0, n, l0, sk) in groups:
                            ps_ = nxt('pb', pb[:4])
                            chan_mm(ps_, wbuf, j * 128, 128, l0, n)
                            o_ = nxt('ob', ob)
                            osem = f'ob{cnt["ob"] % 4}'
                            if kind == 'qkv':
                                cs_ = nxt('cst', cst)
                                ac_ = acc[cnt['cst'] % 2]
                                hist = histP.t[:, ch, :] if sk == 'p' else c0.t[:, ch, int(sk[1]), :]
                                hb = histP if sk == 'p' else c0
                                k.op('act', nc.scalar.copy, outs=[cs_], ins=[ps_], out=cs_.t[:, 3:3 + n],
                                     in_=ps_.t[:, :n])
                                k.op('pool', nc.gpsimd.tensor_copy, outs=[cs_], ins=[hb], out=cs_.t[:, 0:3], in_=hist)
                                if sk == 'p':
                                    k.op('pool', nc.gpsimd.tensor_copy, outs=[histP], ins=[cs_],
                                         out=histP.t[:, ch, :], in_=cs_.t[:, n:n + 3])
                                if sk != 'p' or t0 + n == SEQ:
                                    si = 0 if sk == 'p' else 1 + int(sk[1])
                                    self.out_toks.append(k.dma(
                                        'sp', f'cst{cnt["cst"] % 2}',
                                        self.o_conv[si, :, ch * 128:(ch + 1) * 128].rearrange("j p -> p j"),
                                        cs_.t[:, n:n + 3], ins=[cs_], allow_slow_non_contiguous=True))
                                k.op('dve', nc.vector.tensor_scalar, outs=[ac_], ins=[cs_, cw], out=ac_.t[:, :n],
                                     in0=cs_.t[:, 0:n], scalar1=cw.t[:, ch, 0:1], scalar2=None, op0=ALU.mult)
                                for jj in range(1, 4):
                                    k.op('dve', nc.vector.scalar_tensor_tensor, outs=[ac_], ins=[cs_, cw, ac_],
                                         out=ac_.t[:, :n], in0=cs_.t[:, jj:jj + n], scalar=cw.t[:, ch, jj:jj + 1],
                                         in1=ac_.t[:, :n], op0=ALU.mult, op1=ALU.add)
                                k.op('act', nc.scalar.activation, outs=[o_], ins=[ac_], out=o_.t[:, :n],
                                     in_=ac_.t[:, :n], func=AF.Silu)
                                dst = self.s_qkvT
                            else:
                                func = AF.Silu if kind == 'z' else AF.Sigmoid
                                k.op('act', nc.scalar.activation, outs=[o_], ins=[ps_], out=o_.t[:, :n],
                                     in_=ps_.t[:, :n], func=func)
                                dst = {'z': self.s_zT, 'ga': self.s_gaT, 'gb': self.s_gbT}[kind]
                            k.dma('sp', osem, dst.t[ch * 128:(ch + 1) * 128, t0:t0 + n], o_.t[:, :n], ins=[o_],
                                  outs=[dst])


    def phase_b(self):
        nc, k = self.nc, self.k
        T, SEQ, PAST = self.T, self.SEQ, self.PAST
        SC = float(192.0 ** -0.5)
        NKMAX = max(SEQ, PAST + 16)
        NTK = (NKMAX + 127) // 128
        qlT = k.sb("qlT", [128, 4, T], BF16)
        k.dma_group('sp', 'qlT', [(qlT.t[:, j, :], self.s_qlT.t[j * 128:(j + 1) * 128, :]) for j in range(4)],
                    outs=[qlT], ins=[self.s_qlT])
        dmask_f = k.sb("dmask_f", [128, 128], F32)
        dmask = k.sb("dmask", [128, 128], BF16)
        k.dma('sp', 'dmask', dmask_f.t[:, :], self.dmask_in.ap(), outs=[dmask_f])
        k.op('dve', nc.vector.tensor_copy, outs=[dmask], ins=[dmask_f], out=dmask.t[:, :], in_=dmask_f.t[:, :])
        pastT = k.sb("pastT", [128, 4, PAST + 16], BF16)
        krpT = k.sb("krpT", [RD, PAST + 16], BF16)
        stg = [k.sb(f"stg{i}", [128, 512], F32) for i in range(2)]
        wq = [k.sb(f"wq{i}", [128, 4, 256], BF16) for i in range(2)]
        wk = [k.sb(f"wk{i}", [128, 4, 128], BF16) for i in range(2)]
        wv = [k.sb(f"wv{i}", [128, 4, 128], BF16) for i in range(2)]
        KT = k.sb("KT", [128, NKMAX], BF16)
        V = k.sb("V", [128, NTK, 128], BF16)
        QT = k.sb("QT", [128, SEQ], BF16)
        QrT = k.sb("QrT", [RD, SEQ], BF16)
        q1 = k.sb("q1", [RD, 512], F32)
        q2 = k.sb("q2", [RD, 512], F32)
        cs_t = k.sb("cs_tb", [RD, 2, 512], F32)
        gaT1 = k.sb("gaT0", [128, T], BF16)
        oaT1 = k.sb("oaT0", [128, T], BF16)
        gaT = [gaT1, gaT1]
        oaT = [oaT1, oaT1]
        NSL = 2
        pSs = [k.ps(f"pS{i}", [128, 512], F32) for i in range(NSL)]
        pTt = [k.ps(f"pT{i}", [128, 8, 128], BF16) for i in range(2)]
        pOs = [k.ps(f"pO{i}", [128, 4, 128], F32) for i in range(2)]
        pM = [k.ps(f"pM{i}", [128, 512], F32) for i in range(2)]

        class Slot:
            pass
        slots = []
        for i in range(NSL):
            sl = Slot()
            sl.Pb = [k.sb(f"Pb{i}_{q}", [128, 512], BF16) for q in range(2)]
            sl.PT = [k.sb(f"PT{i}_{q}", [128, 4, 128], BF16) for q in range(2)]
            sl.Oacc = k.sb(f"Oacc{i}", [128, 128], F32)
            sl.On = k.sb(f"On{i}", [128, 128], BF16)
            sl.st_m = [k.sb(f"st_m{i}_{q}", [128, 1], F32) for q in range(2)]
            sl.st_l = k.sb(f"st_l{i}", [128, 1], F32)
            sl.gmax = k.sb(f"gmax{i}", [128, 1], F32)
            sl.negm = k.sb(f"negm{i}", [128, 1], F32)
            sl.alpha = k.sb(f"alpha{i}", [128, 1], F32)
            sl.rsum = k.sb(f"rsum{i}", [128, 1], F32)
            sl.pS = pSs[i]
            sl.pT = pTt[i]
            sl.reg = [(pTt[i].t, q) for q in range(5)]
            sl.pO = pOs[i]
            sl.oi = 0
            slots.append(sl)
        c = dict(pM=0, pS=0, stg=0)

        def nx(name, lst):
            c[name] += 1
            return lst[c[name] % len(lst)]

        wq_v = self.w_uq.ap().rearrange("(cc p) x -> p cc x", p=128)
        wk_v = self.w_uk.ap().rearrange("(cc p) x -> p cc x", p=128)
        wv_v = self.w_uv.ap().rearrange("(cc p) x -> p cc x", p=128)

        def load_head_w(h):
            s_ = h % 2
            k.dma('pool', f'wq{s_}', wq[s_].t[:, :, 0:192], wq_v[:, :, h * 192:(h + 1) * 192], outs=[wq[s_]])
            k.op('dve', nc.vector.tensor_scalar, outs=[wq[s_]], ins=[wq[s_]], out=wq[s_].t[:, :, 192:224],
                 in0=wq[s_].t[:, :, 160:192], scalar1=-1.0, scalar2=None, op0=ALU.mult)
            k.op('dve', nc.vector.tensor_copy, outs=[wq[s_]], ins=[wq[s_]], out=wq[s_].t[:, :, 224:256],
                 in_=wq[s_].t[:, :, 128:160])
            k.dma('pool', f'wk{s_}', wk[s_].t[:, :, :], wk_v[:, :, h * 128:(h + 1) * 128], outs=[wk[s_]])
            k.dma('pool', f'wv{s_}', wv[s_].t[:, :, :], wv_v[:, :, h * 128:(h + 1) * 128], outs=[wv[s_]])

        def build_kv(h, segs):
            s_ = h % 2
            for (src, a0, n, d0) in segs:
                for a in range(0, n, 512):
                    m = min(512, n - a)
                    p = nx('pM', pM)
                    k.mm(p.t[:, :m], [(wk[s_].t[:, cc, :], src.t[:, cc, a0 + a:a0 + a + m]) for cc in range(4)],
                         outs=[p], ins=[wk[s_], src])
                    k.op('act', nc.scalar.copy, outs=[KT], ins=[p], out=KT.t[:, d0 + a:d0 + a + m], in_=p.t[:, :m])
                assert d0 % 128 == 0
                tl = [(a, min(128, n - a)) for a in range(0, n, 128)]
                for g0 in range(0, len(tl), 4):
                    p = nx('pM', pM)
                    grp = tl[g0:g0 + 4]
                    for j, (a, m) in enumerate(grp):
                        k.mm(p.t[:m, j * 128:(j + 1) * 128],
                             [(src.t[:, cc, a0 + a:a0 + a + m], wv[s_].t[:, cc, :]) for cc in range(4)],
                             outs=[p], ins=[wv[s_], src])
                    t_ = (d0 + grp[0][0]) // 128
                    if all(m == 128 for (_, m) in grp):
                        k.op('dve', nc.vector.tensor_copy, outs=[V], ins=[p],
                             out=V.t[:, t_:t_ + len(grp), :],
                             in_=p.t[:, :len(grp) * 128].rearrange("p (j d) -> p j d", d=128))
                    else:
                        for j, (a, m) in enumerate(grp):
                            k.op('dve', nc.vector.tensor_copy, outs=[V], ins=[p], out=V.t[:m, t_ + j, :],
                                 in_=p.t[:m, j * 128:(j + 1) * 128])

        def build_q(h, q0, nq):
            s_ = h % 2
            for a in range(0, nq, 512):
                m = min(512, nq - a)
                t0 = q0 + a
                p = nx('pM', pM)
                k.mm(p.t[:, :m], [(wq[s_].t[:, cc, 0:128], qlT.t[:, cc, t0:t0 + m]) for cc in range(4)],
                     outs=[p], ins=[wq[s_], qlT])
                k.op('act', nc.scalar.activation, outs=[QT], ins=[p], out=QT.t[:, a:a + m], in_=p.t[:, :m],
                     func=AF.Copy, scale=SC)
                p1 = nx('pM', pM)
                k.mm(p1.t[:RD, :m], [(wq[s_].t[:, cc, 128:192], qlT.t[:, cc, t0:t0 + m]) for cc in range(4)],
                     outs=[p1], ins=[wq[s_], qlT])
                k.dma_group('sp', 'cs_tb', [(cs_t.t[:, 0, :m], self.cosT[:, t0:t0 + m]),
                                            (cs_t.t[:, 1, :m], self.sinT[:, t0:t0 + m])], outs=[cs_t])
                k.op('dve', nc.vector.tensor_tensor, outs=[q1], ins=[p1, cs_t], out=q1.t[:, :m], in0=p1.t[:RD, :m],
                     in1=cs_t.t[:, 0, :m], op=ALU.mult)
                p2 = nx('pM', pM)
                k.mm(p2.t[:RD, :m], [(wq[s_].t[:, cc, 192:256], qlT.t[:, cc, t0:t0 + m]) for cc in range(4)],
                     outs=[p2], ins=[wq[s_], qlT])
                k.op('dve', nc.vector.tensor_tensor, outs=[q2], ins=[p2, cs_t], out=q2.t[:, :m], in0=p2.t[:RD, :m],
                     in1=cs_t.t[:, 1, :m], op=ALU.mult)
                k.op('dve', nc.vector.tensor_tensor, outs=[q1], ins=[q1, q2], out=q1.t[:, :m], in0=q1.t[:, :m],
                     in1=q2.t[:, :m], op=ALU.add)
                k.op('act', nc.scalar.activation, outs=[QrT], ins=[q1], out=QrT.t[:, a:a + m], in_=q1.t[:, :m],
                     func=AF.Copy, scale=SC)

        def attend(sl, h, qa, nq, tiles, diag, krbuf, kr0, tq0):
            s_ = h % 2
            groups = [tiles[i:i + 4] for i in range(0, len(tiles), 4)]
            mcur = None
            gmax, negm, alpha, rsum, st_l, Oacc, On = sl.gmax, sl.negm, sl.alpha, sl.rsum, sl.st_l, sl.Oacc, sl.On
            for gi, grp in enumerate(groups):
                pS_ = sl.pS
                col = 0
                plain = [(kc0, nk_) for ti, (kc0, nk_) in enumerate(grp) if gi * 4 + ti != diag]
                if plain:
                    kc0 = plain[0][0]
                    nk_ = sum(x[1] for x in plain)
                    k.mm(pS_.t[:nq, 0:nk_], [(QT.t[:, qa:qa + nq], KT.t[:, kc0:kc0 + nk_]),
                                              (QrT.t[:, qa:qa + nq], krbuf.t[:, kr0 + kc0:kr0 + kc0 + nk_])],
                         outs=[pS_], ins=[QT, KT, QrT, krbuf])
                    col = nk_
                if len(plain) != len(grp):
                    kc0, nk_ = grp[-1]
                    k.mm(pS_.t[:nq, col:col + nk_], [(QT.t[:, qa:qa + nq], KT.t[:, kc0:kc0 + nk_]),
                                                     (QrT.t[:, qa:qa + nq], krbuf.t[:, kr0 + kc0:kr0 + kc0 + nk_]),
                                                     (self.ident_b.t[:nq, :nq], dmask.t[:nq, :nk_])],
                         outs=[pS_], ins=[QT, KT, QrT, krbuf, self.ident_b, dmask])
                    col += nk_
                N = col
                first = gi == 0
                yield
                k.op('dve', nc.vector.reduce_max, outs=[gmax], ins=[pS_], out=gmax.t[:nq, :], in_=pS_.t[:nq, :N],
                     axis=AX.X)
                mnew = sl.st_m[gi % 2]
                if first:
                    k.op('dve', nc.vector.tensor_scalar, outs=[negm], ins=[gmax], out=negm.t[:nq, :], in0=gmax.t[:nq, :],
                         scalar1=-1.0, scalar2=None, op0=ALU.mult)
                    mnew = gmax_keep = None
                yield
                if first:
                    mnew = sl.st_m[0]
                    k.op('dve', nc.vector.tensor_copy, outs=[mnew], ins=[gmax], out=mnew.t[:nq, :], in_=gmax.t[:nq, :])
                else:
                    k.op('dve', nc.vector.tensor_tensor, outs=[mnew], ins=[gmax, mcur], out=mnew.t[:nq, :],
                         in0=gmax.t[:nq, :], in1=mcur.t[:nq, :], op=ALU.max)
                    k.op('dve', nc.vector.tensor_scalar, outs=[negm], ins=[mnew], out=negm.t[:nq, :], in0=mnew.t[:nq, :],
                         scalar1=-1.0, scalar2=None, op0=ALU.mult)
                yield
                Pb_ = sl.Pb[gi % 2]
                k.op('act', nc.scalar.activation, outs=[Pb_, rsum], ins=[pS_, negm], out=Pb_.t[:nq, :N],
                     in_=pS_.t[:nq, :N], func=AF.Exp, bias=negm.t[:nq, 0:1], scale=1.0, accum_out=rsum.t[:nq, :])
                if not first:
                    k.op('act', nc.scalar.activation, outs=[alpha], ins=[mcur, negm], out=alpha.t[:nq, :],
                         in_=mcur.t[:nq, :], func=AF.Exp, bias=negm.t[:nq, 0:1], scale=1.0)
                yield
                if not first:
                    k.op('dve', nc.vector.scalar_tensor_tensor, outs=[st_l], ins=[st_l, alpha, rsum],
                         out=st_l.t[:nq, :], in0=st_l.t[:nq, :], scalar=alpha.t[:nq, 0:1], in1=rsum.t[:nq, :],
                         op0=ALU.mult, op1=ALU.add)
                else:
                    k.op('dve', nc.vector.tensor_copy, outs=[st_l], ins=[rsum], out=st_l.t[:nq, :], in_=rsum.t[:nq, :])
                mcur = mnew
                PT_ = sl.PT[gi % 2]
                col = 0
                order = plain + ([grp[-1]] if len(plain) != len(grp) else [])
                for j, (kc0, nk_) in enumerate(order):
                    rt, rj = sl.reg[j]
                    k.tr(rt[:nk_, rj, :nq], Pb_.t[:nq, col:col + nk_], self.ident_b.t[:nq, :nq], outs=[sl.pT],
                         ins=[Pb_, self.ident_b])
                    col += nk_
                yield
                for j, (kc0, nk_) in enumerate(order):
                    rt, rj = sl.reg[j]
                    if j % 2 == 0:
                        k.op('act', nc.scalar.copy, outs=[PT_], ins=[sl.pT], out=PT_.t[:nk_, j, :nq], in_=rt[:nk_, rj, :nq])
                    else:
                        k.op('dve', nc.vector.tensor_copy, outs=[PT_], ins=[sl.pT], out=PT_.t[:nk_, j, :nq],
                             in_=rt[:nk_, rj, :nq])
                yield
                pO_ = sl.pO
                po_ap = pO_.t[:nq, sl.oi, :]
                k.mm(po_ap, [(PT_.t[:nk_, j, :nq], V.t[:nk_, kc0 // 128, :]) for j, (kc0, nk_) in enumerate(order)],
                     outs=[pO_], ins=[PT_, V])
                yield
                if first:
                    k.op('dve', nc.vector.tensor_copy, outs=[Oacc], ins=[pO_], out=Oacc.t[:nq, :], in_=po_ap)
                else:
                    k.op('dve', nc.vector.scalar_tensor_tensor, outs=[Oacc], ins=[Oacc, alpha, pO_],
                         out=Oacc.t[:nq, :], in0=Oacc.t[:nq, :], scalar=alpha.t[:nq, 0:1], in1=po_ap,
                         op0=ALU.mult, op1=ALU.add)
                yield
            k.op('dve', nc.vector.reciprocal, outs=[rsum], ins=[st_l], out=rsum.t[:nq, :], in_=st_l.t[:nq, :])
            yield
            k.op('dve', nc.vector.tensor_scalar, outs=[On], ins=[Oacc, rsum], out=On.t[:nq, :], in0=Oacc.t[:nq, :],
                 scalar1=rsum.t[:nq, 0:1], scalar2=None, op0=ALU.mult)
            yield
            rt, rj = sl.reg[4]
            k.tr(rt[:, rj, :nq], On.t[:nq, :], self.ident_b.t[:nq, :nq], outs=[sl.pT], ins=[On, self.ident_b])
            yield
            k.op('dve', nc.vector.tensor_tensor, outs=[oaT[s_]], ins=[sl.pT, gaT[s_]], out=oaT[s_].t[:, tq0:tq0 + nq],
                 in0=rt[:, rj, :nq], in1=gaT[s_].t[:, tq0:tq0 + nq], op=ALU.mult)

        def run_rr(gens):
            gens = list(gens)
            while gens:
                for g in list(gens):
                    try:
                        next(g)
                    except StopIteration:
                        gens.remove(g)

        for h in range(NH):
            s_ = h % 2
            load_head_w(h)
            k.dma('sp', 'gaT0', gaT[s_].t[:, :], self.s_gaT.t[h * 128:(h + 1) * 128, :], outs=[gaT[s_]],
                  ins=[self.s_gaT])
            build_kv(h, [(self.ckvT, 0, SEQ, 0)])
            build_q(h, 0, SEQ)
            nqt = SEQ // 128
            for q0 in range(0, nqt, NSL):
                gens = []
                for i, qt in enumerate(range(q0, min(nqt, q0 + NSL))):
                    tiles = [(j * 128, 128) for j in range(qt + 1)]
                    gens.append(attend(slots[i], h, qt * 128, 128, tiles, qt, self.krT, 0, qt * 128))
                run_rr(gens)
            if h == NH - 1 or True:
                pass
            self._oa_pending = getattr(self, '_oa_pending', {})
            self._oa_pending[h] = s_
            k.dma('sp', 'oaT0', self.s_oaT.t[h * 128:(h + 1) * 128, 0:SEQ], oaT[s_].t[:, 0:SEQ], ins=[oaT[s_]],
                  outs=[self.s_oaT])
        for sb in range(2):
            for a in range(0, PAST, 128):
                st_ = nx('stg', stg)
                k.dma('sp', f'stg{c["stg"] % 2}', st_.t[:, :], self.cache_ckv[sb, a:a + 128, :], outs=[st_])
                p = nx('pM', pM)
                for cc in range(4):
                    k.tr(p.t[:, cc * 128:(cc + 1) * 128], st_.t[:, cc * 128:(cc + 1) * 128], self.ident_f.t[:, :],
                         outs=[p], ins=[st_, self.ident_f])
                k.op('act', nc.scalar.copy, outs=[pastT], ins=[p], out=pastT.t[:, :, a:a + 128],
                     in_=p.t[:, :].rearrange("p (c t) -> p c t", t=128))
                st2 = nx('stg', stg)
                k.dma('sp', f'stg{c["stg"] % 2}', st2.t[:, :RD], self.cache_kr[sb, a:a + 128, :], outs=[st2])
                p = nx('pM', pM)
                k.tr(p.t[:RD, :128], st2.t[:, :RD], self.ident_f.t[:, :], outs=[p], ins=[st2, self.ident_f])
                k.op('dve', nc.vector.tensor_copy, outs=[krpT], ins=[p], out=krpT.t[:, a:a + 128], in_=p.t[:RD, :128])
            tq = SEQ + 16 * sb
            k.op('dve', nc.vector.tensor_copy, outs=[pastT], ins=[self.ckvT], out=pastT.t[:, :, PAST:PAST + 16],
                 in_=self.ckvT.t[:, :, tq:tq + 16])
            k.op('dve', nc.vector.tensor_copy, outs=[krpT], ins=[self.krT], out=krpT.t[:, PAST:PAST + 16],
                 in_=self.krT.t[:, tq:tq + 16])
            for h in range(NH):
                s_ = h % 2
                load_head_w(h)
                k.dma('sp', 'gaT0', gaT[s_].t[:, :16], self.s_gaT.t[h * 128:(h + 1) * 128, tq:tq + 16],
                      outs=[gaT[s_]], ins=[self.s_gaT])
                build_kv(h, [(pastT, 0, PAST + 16, 0)])
                build_q(h, tq, 16)
                tiles = [(j * 128, 128) for j in range(PAST // 128)] + [(PAST, 16)]
                run_rr([attend(slots[0], h, 0, 16, tiles, None, krpT, 0, 0)])
                k.dma('sp', 'oaT0', self.s_oaT.t[h * 128:(h + 1) * 128, tq:tq + 16], oaT[s_].t[:, 0:16],
                      ins=[oaT[s_]], outs=[self.s_oaT])

    def phase_c(self):
        nc, k = self.nc, self.k
        T, SEQ = self.T, self.SEQ
        HB = 8
        gpar = k.sb("gpar", [128, 33], F32)
        k.dma('sp', 'gpar', gpar.t[:, :], self.gpar.ap(), outs=[gpar])
        negA = k.sb("negA", [128, 16], F32)
        k.act(negA, negA.t[:, :], gpar, gpar.t[:, 0:16], AF.Exp)
        k.ts(negA, negA.t[:, :], negA, negA.t[:, :], None, -1.0, ALU.mult)
        gm = [k.sb(f"gm{v}", [128, 514], F32) for v in range(2)]
        for v in range(2):
            k.dma('sp', f'gm{v}', gm[v].t[:, :], self.gmask[v], outs=[gm[v]])
        ones_f = k.sb("ones_f", [128, 128], F32)
        k.op('dve', nc.vector.memset, outs=[ones_f], ap=ones_f.t[:, :], constant=1.0)
        gmb = [k.sb(f"gmb{v}", [128, 514], BF16) for v in range(2)]
        for v in range(2):
            k.cp('dve', gmb[v], gmb[v].t[:, :], gm[v], gm[v].t[:, :])
        g3 = k.sb("g3", [128, 3, 16], BF16)
        b2s = k.sb("b2s", [128, 2, 16], BF16)
        g3f = k.sb("g3f", [128, 3, 16], F32)
        b2f = k.sb("b2f", [128, 2, 16], F32)
        res16 = k.sb("res16", [128, 16], F32)
        G48 = k.sb("G48", [128, 48], F32)

        def split3(src, n, dst_b, dst_f, parts):
            cur = src
            for p in range(parts):
                k.cp('dve', dst_b, dst_b.t[:n, p, :], cur, cur.t[:n, :])
                k.cp('dve', dst_f, dst_f.t[:n, p, :], dst_b, dst_b.t[:n, p, :])
                if p < parts - 1:
                    k.tt(res16, res16.t[:n, :], cur, cur.t[:n, :], dst_f, dst_f.t[:n, p, :], ALU.subtract)
                    cur = res16
        Sf = [k.sb(f"Sf{i}", [128, NH, 128], F32) for i in range(3)]
        Sb = [k.sb(f"Sb{i}", [128, NH, 128], BF16) for i in range(3)]
        k.op('dve', nc.vector.memset, outs=[Sf[0]], ap=Sf[0].t[:, :, :], constant=0.0)
        k.op('dve', nc.vector.memset, outs=[Sb[0]], ap=Sb[0].t[:, :, :], constant=0.0)
        for i in (1, 2):
            k.dma('sp', f'Sf{i}', Sf[i].t[:, :, :], self.state_gdn[i - 1].rearrange("h p d -> p h d"), outs=[Sf[i]])
            k.cp('dve', Sb[i], Sb[i].t[:, :, :], Sf[i], Sf[i].t[:, :, :])
        qk_g = k.sb("qk_g", [128, HB, 2, 512], BF16)
        v_g = k.sb("v_g", [128, HB, 512], BF16)
        zg_g = k.sb("zg_g", [128, HB, 512], BF16)
        gb_g = k.sb("gb_g", [128, HB, 512], BF16)
        oa_g = k.sb("oa_g", [128, HB, 512], BF16)
        m_g = k.sb("m_g", [128, HB, 512], BF16)
        ab_t = k.sb("ab_t", [128, 32], F32)
        tmp16 = k.sb("tmp16", [128, 16], F32)
        g16 = k.sb("g16", [128, 16], F32)
        beta16 = k.sb("beta16", [128, 16], F32)
        G16 = k.sb("G16", [128, 16], F32)
        negG16 = k.sb("negG16", [128, 16], F32)
        GL16 = k.sb("GL16", [128, 16], F32)
        eG16 = k.sb("eG16", [128, 16], F32)
        be16 = k.sb("be16", [128, 16], F32)
        el16 = k.sb("el16", [128, 2, 16], F32)
        def per(name, shape, dt):
            return [k.sb(f"{name}{j}", shape, dt) for j in range(HB)]
        sq2 = per("sq2_", [128, 2, 128], BF16)
        r2 = per("r2_", [128, 2, 128], F32)
        qkn = per("qkn_", [128, 2, 128], BF16)
        kbe = per("kbe_", [128, 128], BF16)
        kdlm = per("kdlm_", [128, 2, 128], BF16)
        vb = per("vb_", [128, 128], BF16)
        rhsG = per("rhsG_", [128, 5, 128], BF16)
        argT = per("argT_", [128, 128], F32)
        decT = per("decT_", [128, 128], F32)
        eGbc = per("eGbc_", [128, 128], F32)
        bs = per("bs_", [128, 128], F32)
        t1 = per("t1_", [128, 128], F32)
        Nb = [per(f"Nb{q}_", [128, 128], BF16) for q in range(2)]
        Lb = [per(f"Lb{q}_", [128, 128], BF16) for q in range(2)]
        Pm = per("Pm_", [128, 128], BF16)
        AT = per("AT_", [128, 2, 128], BF16)
        u_f = per("u_f_", [128, 128], F32)
        wTm = per("wTm_", [128, 2, 128], BF16)
        qeTm = per("qeTm_", [128, 2, 128], BF16)
        vnew = per("vnew_", [128, 128], BF16)
        o_f = per("o_f_", [128, 128], F32)
        on_b = per("on_b_", [128, 128], BF16)
        oss = per("oss_", [128, 1], F32)
        ojunk = per("ojunk_", [128, 128], BF16)
        banks = [k.ps(f"pc{i}", [128, 4, 128], F32) for i in range(6)]
        bankb = [k.ps(f"pcb{i}", [128, 8, 128], BF16) for i in range(2)]
        bc = [0, 0]

        def pbank(j):
            b = banks[(bc[0] * 2 + j // 4) % 6]
            return b, b.t[:, j % 4, :]

        def step():
            bc[0] += 1

        def stepb():
            bc[1] += 1
            return bankb[bc[1] % 2]

        def run_tile(h0, t0, n, L, var, sidx, goff):
            TRI = gm[var].t[:n, 0:n]
            NEGU = gm[var].t[:n, 128:128 + n]
            SU = gm[var].t[:n, 256:256 + n]
            cm = gm[var]
            sl = slice(goff, goff + n)
            k.dma('sp', 'ab_t', ab_t.t[:n, :], self.s_ab.t[t0:t0 + n, :], outs=[ab_t], ins=[self.s_ab])
            k.tt(tmp16, tmp16.t[:n, :], ab_t, ab_t.t[:n, 0:16], gpar, gpar.t[:n, 16:32], ALU.add)
            k.act(tmp16, tmp16.t[:n, :], tmp16, tmp16.t[:n, :], AF.Exp)
            k.act(tmp16, tmp16.t[:n, :], tmp16, tmp16.t[:n, :], AF.Ln, bias=(ones_f, ones_f.t[:n, 0:1]))
            k.tt(g16, g16.t[:n, :], tmp16, tmp16.t[:n, :], negA, negA.t[:n, :], ALU.mult)
            k.act(beta16, beta16.t[:n, :], ab_t, ab_t.t[:n, 16:32], AF.Sigmoid)
            split3(g16, n, g3, g3f, 3)
            split3(beta16, n, b2s, b2f, 2)
            step()
            b_, p_ = pbank(0)
            k.mm(p_[:n, 0:48], [(gmb[var].t[:n, 0:n], g3.t[:n, :, :])], outs=[b_], ins=[gmb[var], g3])
            k.cp('dve', G48, G48.t[:n, :], b_, p_[:n, 0:48])
            k.tt(G16, G16.t[:n, :], G48, G48.t[:n, 0:16], G48, G48.t[:n, 16:32], ALU.add)
            k.tt(G16, G16.t[:n, :], G16, G16.t[:n, :], G48, G48.t[:n, 32:48], ALU.add)
            k.ts(negG16, negG16.t[:n, :], G16, G16.t[:n, :], None, -1.0, ALU.mult)
            b2, p2 = pbank(1)
            k.mm(p2[:n, 0:48], [(gmb[var].t[:n, 386:386 + n], g3.t[:n, :, :])], outs=[b2], ins=[gmb[var], g3])
            k.cp('dve', G48, G48.t[:n, :], b2, p2[:n, 0:48])
            k.tt(GL16, GL16.t[:n, :], G48, G48.t[:n, 0:16], G48, G48.t[:n, 16:32], ALU.add)
            k.tt(GL16, GL16.t[:n, :], GL16, GL16.t[:n, :], G48, G48.t[:n, 32:48], ALU.add)
            k.act(eG16, eG16.t[:n, :], G16, G16.t[:n, :], AF.Exp)
            k.tt(be16, be16.t[:n, :], eG16, eG16.t[:n, :], beta16, beta16.t[:n, :], ALU.mult)
            k.tt(tmp16, tmp16.t[:n, :], GL16, GL16.t[:n, :], G16, G16.t[:n, :], ALU.subtract)
            k.act(tmp16, tmp16.t[:n, :], tmp16, tmp16.t[:n, :], AF.Exp)
            for c_ in range(2):
                k.ts(el16, el16.t[:n, c_, :], tmp16, tmp16.t[:n, :], cm, cm.t[:n, 384 + c_:385 + c_], ALU.mult)
            if CSTOP <= 1:
                return
            hs = list(range(HB))
            step()
            for j in hs:
                k.act(sq2[j], sq2[j].t[:, :, :n], qk_g, qk_g.t[:, j, :, sl], AF.Square)
            if CSTOP <= 1.2:
                return
            for q in range(2):
                step()
                for j in hs:
                    b_, p_ = pbank(j)
                    k.mm(p_[:, :n], [(self.ones_b.t[:, :], sq2[j].t[:, q, :n])], outs=[b_], ins=[self.ones_b, sq2[j]])
                    k.ts(r2[j], r2[j].t[:, q, :n], b_, p_[:, :n], None, EPS, ALU.add)
            if CSTOP <= 1.4:
                return
            for j in hs:
                k.act(r2[j], r2[j].t[:, :, :n], r2[j], r2[j].t[:, :, :n], AF.Sqrt)
            if CSTOP <= 1.5:
                return
            for j in hs:
                k.op('dve', nc.vector.reciprocal, outs=[r2[j]], ins=[r2[j]], out=r2[j].t[:, :, :n], in_=r2[j].t[:, :, :n])
            for j in hs:
                k.ts(r2[j], r2[j].t[:, 0, :n], r2[j], r2[j].t[:, 0, :n], None, float(128.0 ** -0.5), ALU.mult)
            if CSTOP <= 1.6:
                return
            for j in hs:
                k.tt(qkn[j], qkn[j].t[:, :, :n], qk_g, qk_g.t[:, j, :, sl], r2[j], r2[j].t[:, :, :n], ALU.mult)
            if CSTOP <= 2:
                return
            pb_ = stepb()
            for j in hs:
                k.tr(pb_.t[:n, j, :], qkn[j].t[:, 1, :n], self.ident_b.t[:, :], outs=[pb_], ins=[qkn[j], self.ident_b])
            if CSTOP <= 2.2:
                return
            for j in hs:
                h = h0 + j
                k.ts(kbe[j], kbe[j].t[:n, :], pb_, pb_.t[:n, j, :], be16, be16.t[:n, h:h + 1], ALU.mult)
            if CSTOP <= 2.4:
                return
            for j in hs:
                h = h0 + j
                for c_ in range(2):
                    k.ts(kdlm[j], kdlm[j].t[:n, c_, :], pb_, pb_.t[:n, j, :], el16, el16.t[:n, c_, h:h + 1], ALU.mult)
            if CSTOP <= 2.6:
                return
            pb_ = stepb()
            for j in hs:
                k.tr(pb_.t[:n, j, :], v_g.t[:, j, sl], self.ident_b.t[:, :], outs=[pb_], ins=[v_g, self.ident_b])
            for j in hs:
                h = h0 + j
                k.ts(vb[j], vb[j].t[:n, :], pb_, pb_.t[:n, j, :], beta16, beta16.t[:n, h:h + 1], ALU.mult)
            if CSTOP <= 3:
                return
            for j in hs:
                h = h0 + j
                for p in range(3):
                    k.ts(rhsG[j], rhsG[j].t[:n, p, 0:n], gm[var], TRI, g3f, g3f.t[:n, p, h:h + 1], ALU.mult)
                for p in range(2):
                    k.ts(rhsG[j], rhsG[j].t[:n, 3 + p, 0:n], self.ident_f, self.ident_f.t[:n, :n], b2f,
                         b2f.t[:n, p, h:h + 1], ALU.mult)
            step()
            gb_slots = []
            for j in hs:
                b_, p_ = pbank(j)
                k.mm(p_[:, :n], [(self.ones_b.t[:n, :], rhsG[j].t[:n, p, 0:n]) for p in range(3)], outs=[b_],
                     ins=[self.ones_b, rhsG[j]])
                gb_slots.append((b_, p_))
            for j in hs:
                h = h0 + j
                b_, p_ = gb_slots[j]
                k.act(eGbc[j], eGbc[j].t[:, :n], b_, p_[:, :n], AF.Exp)
                k.stt(argT[j], argT[j].t[:n, :n], b_, p_[:n, :n], negG16, negG16.t[:n, h:h + 1], gm[var], NEGU,
                      ALU.add, ALU.add)
                k.act(decT[j], decT[j].t[:n, :n], argT[j], argT[j].t[:n, :n], AF.Exp)
            step()
            for j in hs:
                b_, p_ = pbank(j)
                k.mm(p_[:n, :n], [(self.ones_b.t[:n, :n], rhsG[j].t[:n, 3 + p, 0:n]) for p in range(2)], outs=[b_],
                     ins=[self.ones_b, rhsG[j]])
                k.tt(bs[j], bs[j].t[:n, :n], b_, p_[:n, :n], gm[var], SU, ALU.mult)
            if CSTOP <= 4:
                return
            step()
            for j in hs:
                b_, p_ = pbank(j)
                k.mm(p_[:n, :n], [(qkn[j].t[:, 1, :n], qkn[j].t[:, 1, :n])], outs=[b_], ins=[qkn[j]])
                k.tt(t1[j], t1[j].t[:n, :n], b_, p_[:n, :n], decT[j], decT[j].t[:n, :n], ALU.mult)
                k.tt(Nb[0][j], Nb[0][j].t[:n, :n], t1[j], t1[j].t[:n, :n], bs[j], bs[j].t[:n, :n], ALU.mult)
            step()
            for j in hs:
                b_, p_ = pbank(j)
                k.mm(p_[:n, :n], [(qkn[j].t[:, 1, :n], qkn[j].t[:, 0, :n])], outs=[b_], ins=[qkn[j]])
                k.op('dve', nc.vector.memset, outs=[AT[j]], ap=AT[j].t[:n, :, :n], constant=0.0)
                for c_ in range(2):
                    cs = slice(c_ * L, (c_ + 1) * L)
                    k.tt(AT[j], AT[j].t[:n, c_, cs], b_, p_[:n, cs], decT[j], decT[j].t[:n, cs], ALU.mult)
            if CSTOP <= 5:
                return
            pb_ = stepb()
            for j in hs:
                k.tr(pb_.t[:n, j, :n], Nb[0][j].t[:n, :n], self.ident_b.t[:n, :n], outs=[pb_], ins=[Nb[0][j], self.ident_b])
            for j in hs:
                k.cp('act', Lb[0][j], Lb[0][j].t[:n, :n], pb_, pb_.t[:n, j, :n])
                k.tt(Pm[j], Pm[j].t[:n, :n], self.ident_f, self.ident_f.t[:n, :n], Nb[0][j], Nb[0][j].t[:n, :n],
                     ALU.subtract)
            nlev = int(np.log2(L)) - 1
            cur = 0
            for lev in range(1, nlev + 1):
                nx_ = 1 - cur
                lastl = lev == nlev
                step()
                for j in hs:
                    b_, p_ = pbank(j)
                    k.mm(p_[:n, :n], [(Nb[cur][j].t[:n, :n], Lb[cur][j].t[:n, :n])], outs=[b_], ins=[Nb[cur][j], Lb[cur][j]])
                    k.cp('act', Lb[nx_][j], Lb[nx_][j].t[:n, :n], b_, p_[:n, :n])
                if not lastl:
                    step()
                    for j in hs:
                        b_, p_ = pbank(j)
                        k.mm(p_[:n, :n], [(Lb[cur][j].t[:n, :n], Nb[cur][j].t[:n, :n])], outs=[b_],
                             ins=[Nb[cur][j], Lb[cur][j]])
                        k.cp('act', Nb[nx_][j], Nb[nx_][j].t[:n, :n], b_, p_[:n, :n])
                step()
                for j in hs:
                    b_, p_ = pbank(j)
                    k.mm(p_[:n, :n], [(Lb[nx_][j].t[:n, :n], Pm[j].t[:n, :n])], outs=[b_], ins=[Lb[nx_][j], Pm[j]])
                    k.tt(Pm[j], Pm[j].t[:n, :n], Pm[j], Pm[j].t[:n, :n], b_, p_[:n, :n], ALU.add)
                cur = nx_
            if CSTOP <= 6:
                return
            step()
            for j in hs:
                b_, p_ = pbank(j)
                k.mm(p_[:n, :], [(Pm[j].t[:n, :n], vb[j].t[:n, :])], outs=[b_], ins=[Pm[j], vb[j]])
                k.cp('act', u_f[j], u_f[j].t[:n, :], b_, p_[:n, :])
            step()
            for j in hs:
                b_, p_ = pbank(j)
                k.mm(p_[:, :n], [(kbe[j].t[:n, :], Pm[j].t[:n, :n])], outs=[b_], ins=[Pm[j], kbe[j]])
                k.op('dve', nc.vector.memset, outs=[wTm[j]], ap=wTm[j].t[:, :, :n], constant=0.0)
                k.op('dve', nc.vector.memset, outs=[qeTm[j]], ap=qeTm[j].t[:, :, :n], constant=0.0)
                for c_ in range(2):
                    cs = slice(c_ * L, (c_ + 1) * L)
                    k.cp('act', wTm[j], wTm[j].t[:, c_, cs], b_, p_[:, cs])
                    k.tt(qeTm[j], qeTm[j].t[:, c_, cs], qkn[j], qkn[j].t[:, 0, cs], eGbc[j], eGbc[j].t[:, cs], ALU.mult)
            if CSTOP <= 7:
                return
            step()
            o_slots = [pbank(j) for j in hs]
            for c_ in range(2):
                Sfc, Sbc = Sf[sidx[c_]], Sb[sidx[c_]]
                step()
                ws = [pbank(j) for j in hs]
                for j in hs:
                    h = h0 + j
                    b_, p_ = ws[j]
                    k.mm(p_[:n, :], [(wTm[j].t[:, c_, :n], Sbc.t[:, h, :])], outs=[b_], ins=[wTm[j], Sbc])
                for j in hs:
                    b_, p_ = ws[j]
                    k.tt(vnew[j], vnew[j].t[:n, :], u_f[j], u_f[j].t[:n, :], b_, p_[:n, :], ALU.subtract)
                for j in hs:
                    h = h0 + j
                    b_, p_ = o_slots[j]
                    k.nc.tensor.matmul(p_[:n, :], lhsT=qeTm[j].t[:, c_, :n], rhs=Sbc.t[:, h, :], start=(c_ == 0), stop=False) if False else None
                step()
                ds = [pbank(j) for j in hs]
                for j in hs:
                    h = h0 + j
                    b_, p_ = ds[j]
                    k.mm(p_[:, :], [(kdlm[j].t[:n, c_, :], vnew[j].t[:n, :])], outs=[b_], ins=[kdlm[j], vnew[j]])
                step()
                oc = [pbank(j) for j in hs]
                for j in hs:
                    h = h0 + j
                    b_, p_ = oc[j]
                    k.mm(p_[:n, :], [(qeTm[j].t[:, c_, :n], Sbc.t[:, h, :]), (AT[j].t[:n, c_, :n], vnew[j].t[:n, :])],
                         outs=[b_], ins=[qeTm[j], Sbc, AT[j], vnew[j]])
                for j in hs:
                    b_, p_ = oc[j]
                    if c_ == 0:
                        k.cp('act', o_f[j], o_f[j].t[:n, :], b_, p_[:n, :])
                    else:
                        k.tt(o_f[j], o_f[j].t[:n, :], o_f[j], o_f[j].t[:n, :], b_, p_[:n, :], ALU.add)
                for j in hs:
                    h = h0 + j
                    b_, p_ = ds[j]
                    col = (c_ + 1) * L - 1
                    k.stt(Sfc, Sfc.t[:, h, :], Sfc, Sfc.t[:, h, :], eGbc[j], eGbc[j].t[:, col:col + 1], b_, p_[:, :],
                          ALU.mult, ALU.add)
                    k.cp('act', Sbc, Sbc.t[:, h, :], Sfc, Sfc.t[:, h, :])
            if CSTOP <= 8:
                return
            for j in hs:
                k.act(ojunk[j], ojunk[j].t[:n, :], o_f[j], o_f[j].t[:n, :], AF.Square, accum=(oss[j], oss[j].t[:n, :]))
            for j in hs:
                k.ts(oss[j], oss[j].t[:n, :], oss[j], oss[j].t[:n, :], None, 1.0 / 128.0, ALU.mult, sc2=EPS, op2=ALU.add)
            for j in hs:
                k.act(oss[j], oss[j].t[:n, :], oss[j], oss[j].t[:n, :], AF.Sqrt)
            for j in hs:
                k.op('dve', nc.vector.reciprocal, outs=[oss[j]], ins=[oss[j]], out=oss[j].t[:n, :], in_=oss[j].t[:n, :])
            for j in hs:
                k.ts(on_b[j], on_b[j].t[:n, :], o_f[j], o_f[j].t[:n, :], oss[j], oss[j].t[:n, 0:1], ALU.mult)
            pb_ = stepb()
            for j in hs:
                k.tr(pb_.t[:, j, :n], on_b[j].t[:n, :], self.ident_b.t[:n, :n], outs=[pb_], ins=[on_b[j], self.ident_b])
            for j in hs:
                k.stt(m_g, m_g.t[:, j, sl], pb_, pb_.t[:, j, :n], gpar, gpar.t[:, 32:33], zg_g, zg_g.t[:, j, sl],
                      ALU.mult, ALU.mult)
                k.tt(m_g, m_g.t[:, j, sl], m_g, m_g.t[:, j, sl], oa_g, oa_g.t[:, j, sl], ALU.add)

        groups = [(t, min(512, SEQ - t), 'p') for t in range(0, SEQ, 512)] + [(SEQ, NS, 's')]
        for h0 in range(0, NH, HB):
            for (g0, gn, kind) in groups:
                cs_ = slice(g0, g0 + gn)
                def rows(base):
                    return self.s_qkvT.t[base + h0 * 128: base + (h0 + HB) * 128, cs_].rearrange("(j p) t -> p j t", p=128)
                k.dma_group('sp', 'qk_g', [(qk_g.t[:, :, 0, :gn], rows(0)), (qk_g.t[:, :, 1, :gn], rows(2048))],
                            outs=[qk_g], ins=[self.s_qkvT])
                k.dma('sp', 'v_g', v_g.t[:, :, :gn], rows(4096), outs=[v_g], ins=[self.s_qkvT])
                def rows2(t_):
                    return t_.t[h0 * 128:(h0 + HB) * 128, cs_].rearrange("(j p) t -> p j t", p=128)
                k.dma('sp', 'zg_g', zg_g.t[:, :, :gn], rows2(self.s_zT), outs=[zg_g], ins=[self.s_zT])
                k.dma('sp', 'gb_g', gb_g.t[:, :, :gn], rows2(self.s_gbT), outs=[gb_g], ins=[self.s_gbT])
                k.dma('sp', 'oa_g', oa_g.t[:, :, :gn], rows2(self.s_oaT), outs=[oa_g], ins=[self.s_oaT])
                k.tt(zg_g, zg_g.t[:, :, :gn], zg_g, zg_g.t[:, :, :gn], gb_g, gb_g.t[:, :, :gn], ALU.mult)
                if kind == 'p':
                    for a in range(0, gn, 128):
                        run_tile(h0, g0 + a, min(128, gn - a), 64, 0, (0, 0), a)
                else:
                    run_tile(h0, g0, NS, 16, 1, (1, 2), 0)
                k.dma('sp', 'm_g', rows2(self.s_mT), m_g.t[:, :, :gn], ins=[m_g], outs=[self.s_mT])
        for i in range(3):
            self.out_toks.append(k.dma('sp', f'Sf{i}', self.o_gdn[i].rearrange("h p d -> p h d"), Sf[i].t[:, :, :],
                                       ins=[Sf[i]]))

    def phase_d(self):
        nc, k = self.nc, self.k
        T, NTL = self.T, self.NT
        CAP, NE = self.CAP, self.NE
        self.key = [k.sb(f"key{i}", [128, NTL], I32, persist=True) for i in range(2)]
        self.wgt = [k.sb(f"wgt{i}", [128, NTL], F32, persist=True) for i in range(2)]
        wo = k.sb("wo", [128, KC, D], BF16)
        wo_v = self.w_out.ap().rearrange("(kc p) c -> p kc c", p=128)
        k.dma_group('pool', 'wo', [(wo.t[:, q * 4:(q + 1) * 4, :], wo_v[:, q * 4:(q + 1) * 4, :]) for q in range(4)],
                    outs=[wo])
        gffn = k.sb("gffn", [128, D], F32)
        k.dma('sp', 'gffn', gffn.t[:, :], self.ffn_g.ap(), outs=[gffn])
        rb = k.sb("rb", [128, 72], F32)
        k.dma('sp', 'rb', rb.t[:, :], self.rbias.ap(), outs=[rb])
        mc = k.sb("mc", [128, 192], F32)
        k.dma('sp', 'mc', mc.t[:, :], self.mconst.ap(), outs=[mc])
        sut = k.sb("sut", [128, 128], BF16)
        k.cp('dve', sut, sut.t[:, :], mc, mc.t[:, 64:192])
        wrf = k.sb("wrf", [128, KC, 72], F32)
        k.dma('sp', 'wrf', wrf.t[:, :, :], self.w_rt.ap().rearrange("(kc p) c -> p kc c", p=128), outs=[wrf])
        wr_hi = k.sb("wr_hi", [128, KC, 72], BF16)
        wr_mid = k.sb("wr_mid", [128, KC, 72], BF16)
        wr_t = k.sb("wr_t", [128, KC, 72], F32)
        k.cp('dve', wr_hi, wr_hi.t[:, :, :], wrf, wrf.t[:, :, :])
        k.cp('dve', wr_t, wr_t.t[:, :, :], wr_hi, wr_hi.t[:, :, :])
        k.tt(wr_t, wr_t.t[:, :, :], wrf, wrf.t[:, :, :], wr_t, wr_t.t[:, :, :], ALU.subtract)
        k.cp('dve', wr_mid, wr_mid.t[:, :, :], wr_t, wr_t.t[:, :, :])
        carry = k.sb("carry", [128, 64], F32)
        k.op('dve', nc.vector.memset, outs=[carry], ap=carry.t[:, :], constant=0.0)
        mT = [k.sb(f"mT{i}", [128, KC, 128], BF16) for i in range(2)]
        xt = [k.sb(f"xt{i}", [128, D], F32) for i in range(2)]
        x1 = k.sb("x1", [128, D], F32)
        h2 = k.sb("h2", [128, D], F32)
        hhi = k.sb("hhi", [128, D], BF16)
        hmid = k.sb("hmid", [128, D], BF16)
        hTh = k.sb("hTh", [128, KC, 128], BF16)
        hTm = k.sb("hTm", [128, KC, 128], BF16)
        sc1 = [k.sb(f"dsc{i}", [128, 1], F32) for i in range(8)]
        lg = k.sb("lg", [128, 72], F32)
        e8 = k.sb("e8", [128, 8], F32)
        gm8 = k.sb("gm8", [128, 8], F32)
        lem = k.sb("lem", [128, 64], F32)
        m12 = [k.sb(f"m12_{i}", [128, 64], F32) for i in range(2)]
        Eb = k.sb("Eb", [128, 64], BF16)
        cntf = k.sb("cntf", [128, 64], F32)
        tmp64 = k.sb("tmp64", [128, 64], F32)
        keyf = k.sb("keyf", [128, 1], F32)
        pw = [k.ps(f"pw{i}", [128, 512], F32) for i in range(4)]
        ptb = [k.ps(f"ptb{i}", [128, 8, 128], BF16) for i in range(2)]
        pr = k.ps("pr", [128, 512], F32)
        pc = k.ps("pcnt", [128, 512], F32)
        mT_v = self.s_mT.t
        for ti in range(NTL):
            t0 = ti * 128
            n = min(128, T - t0)
            s_ = ti % 2
            k.dma('sp', f'mT{s_}', mT[s_].t[:, :, :n], mT_v[:, t0:t0 + n].rearrange("(c p) t -> p c t", p=128),
                  outs=[mT[s_]], ins=[self.s_mT])
            k.dma('sp', f'xt{s_}', xt[s_].t[:n, :], self.x_all[t0:t0 + n, :], outs=[xt[s_]])
            for cg in range(4):
                p = pw[cg]
                k.mm(p.t[:n, :], [(mT[s_].t[:, kc, :n], wo.t[:, kc, cg * 512:(cg + 1) * 512]) for kc in range(KC)],
                     outs=[p], ins=[mT[s_], wo])
                k.tt(x1, x1.t[:n, cg * 512:(cg + 1) * 512], p, p.t[:n, :], xt[s_], xt[s_].t[:n, cg * 512:(cg + 1) * 512],
                     ALU.add)
            k.dma('sp', 'x1', self.s_x1.t[t0:t0 + n, :], x1.t[:n, :], ins=[x1], outs=[self.s_x1])
            ss, rs = sc1[0], sc1[1]
            k.act(hmid, hmid.t[:n, :], x1, x1.t[:n, :], AF.Square, accum=(ss, ss.t[:n, :]))
            k.ts(rs, rs.t[:n, :], ss, ss.t[:n, :], None, 1.0 / D, ALU.mult, sc2=EPS, op2=ALU.add)
            k.act(rs, rs.t[:n, :], rs, rs.t[:n, :], AF.Sqrt)
            k.op('dve', nc.vector.reciprocal, outs=[rs], ins=[rs], out=rs.t[:n, :], in_=rs.t[:n, :])
            k.stt(h2, h2.t[:n, :], x1, x1.t[:n, :], rs, rs.t[:n, 0:1], gffn, gffn.t[:n, :], ALU.mult, ALU.mult)
            k.cp('act', hhi, hhi.t[:n, :], h2, h2.t[:n, :])
            k.tt(h2, h2.t[:n, :], h2, h2.t[:n, :], hhi, hhi.t[:n, :], ALU.subtract)
            k.cp('act', hmid, hmid.t[:n, :], h2, h2.t[:n, :])
            for (src, dst) in ((hhi, hTh), (hmid, hTm)):
                for q in range(2):
                    pb_ = ptb[q]
                    for j in range(8):
                        kc = q * 8 + j
                        k.tr(pb_.t[:, j, :n], src.t[:n, kc * 128:(kc + 1) * 128], self.ident_b.t[:n, :n], outs=[pb_],
                             ins=[src, self.ident_b])
                    k.cp('dve', dst, dst.t[:, q * 8:(q + 1) * 8, :n], pb_, pb_.t[:, :, :n])
            ops = []
            for kc in range(KC):
                ops += [(hTh.t[:, kc, :n], wr_hi.t[:, kc, :]), (hTh.t[:, kc, :n], wr_mid.t[:, kc, :]),
                        (hTm.t[:, kc, :n], wr_hi.t[:, kc, :])]
            k.mm(pr.t[:n, 0:72], ops, outs=[pr], ins=[hTh, hTm, wr_hi, wr_mid])
            k.tt(lg, lg.t[:n, :], pr, pr.t[:n, 0:72], rb, rb.t[:n, :], ALU.add)
            gmx, ngm, es, pg = sc1[2], sc1[3], sc1[4], sc1[5]
            k.op('dve', nc.vector.reduce_max, outs=[gmx], ins=[lg], out=gmx.t[:n, :], in_=lg.t[:n, 0:8], axis=AX.X)
            k.ts(ngm, ngm.t[:n, :], gmx, gmx.t[:n, :], None, -1.0, ALU.mult)
            k.act(e8, e8.t[:n, :], lg, lg.t[:n, 0:8], AF.Exp, bias=(ngm, ngm.t[:n, 0:1]), accum=(es, es.t[:n, :]))
            k.op('dve', nc.vector.reciprocal, outs=[pg], ins=[es], out=pg.t[:n, :], in_=es.t[:n, :])
            k.ts(gm8, gm8.t[:n, :], lg, lg.t[:n, 0:8], gmx, gmx.t[:n, 0:1], ALU.is_equal)
            k.ts(gm8, gm8.t[:n, :], gm8, gm8.t[:n, :], None, -1.0, ALU.add, sc2=30000.0, op2=ALU.mult)
            for g in range(8):
                k.ts(lem, lem.t[:n, g * 8:(g + 1) * 8], lg, lg.t[:n, 8 + g * 8:16 + g * 8], gm8, gm8.t[:n, g:g + 1], ALU.add)
            v1, v2 = sc1[6], sc1[7]
            k.op('dve', nc.vector.reduce_max, outs=[v1], ins=[lem], out=v1.t[:n, :], in_=lem.t[:n, :], axis=AX.X)
            k.ts(m12[0], m12[0].t[:n, :], lem, lem.t[:n, :], v1, v1.t[:n, 0:1], ALU.is_equal)
            k.stt(lem, lem.t[:n, :], m12[0], m12[0].t[:n, :], None, -60000.0, lem, lem.t[:n, :], ALU.mult, ALU.add) \
                if False else k.op('dve', nc.vector.scalar_tensor_tensor, outs=[lem], ins=[m12[0], lem], out=lem.t[:n, :],
                                   in0=m12[0].t[:n, :], scalar=-60000.0, in1=lem.t[:n, :], op0=ALU.mult, op1=ALU.add)
            k.op('dve', nc.vector.reduce_max, outs=[v2], ins=[lem], out=v2.t[:n, :], in_=lem.t[:n, :], axis=AX.X)
            k.ts(m12[1], m12[1].t[:n, :], lem, lem.t[:n, :], v2, v2.t[:n, 0:1], ALU.is_equal)
            tt_, den = sc1[0], sc1[1]
            k.tt(tt_, tt_.t[:n, :], v2, v2.t[:n, :], v1, v1.t[:n, :], ALU.subtract)
            k.act(tt_, tt_.t[:n, :], tt_, tt_.t[:n, :], AF.Exp)
            k.ts(den, den.t[:n, :], tt_, tt_.t[:n, :], None, 1.0, ALU.add)
            k.op('dve', nc.vector.reciprocal, outs=[den], ins=[den], out=den.t[:n, :], in_=den.t[:n, :])
            k.tt(self.wgt[0], self.wgt[0].t[:n, ti:ti + 1], pg, pg.t[:n, :], den, den.t[:n, :], ALU.mult)
            k.tt(self.wgt[1], self.wgt[1].t[:n, ti:ti + 1], self.wgt[0], self.wgt[0].t[:n, ti:ti + 1], tt_, tt_.t[:n, :],
                 ALU.mult)
            k.tt(tmp64, tmp64.t[:n, :], m12[0], m12[0].t[:n, :], m12[1], m12[1].t[:n, :], ALU.add)
            k.cp('dve', Eb, Eb.t[:n, :], tmp64, tmp64.t[:n, :])
            k.mm(pc.t[:n, 0:64], [(sut.t[:n, :n], Eb.t[:n, :])], outs=[pc], ins=[sut, Eb])
            k.tt(cntf, cntf.t[:n, :], pc, pc.t[:n, 0:64], carry, carry.t[:n, :], ALU.add)
            k.mm(pc.t[:, 64:128], [(self.ones_b.t[:n, :], Eb.t[:n, :])], outs=[pc], ins=[self.ones_b, Eb])
            k.tt(carry, carry.t[:, :], carry, carry.t[:, :], pc, pc.t[:, 64:128], ALU.add)
            for kk in range(2):
                rk, ov = sc1[2], sc1[3]
                k.tt(tmp64, tmp64.t[:n, :], cntf, cntf.t[:n, :], m12[kk], m12[kk].t[:n, :], ALU.mult)
                k.op('dve', nc.vector.reduce_sum, outs=[rk], ins=[tmp64], out=rk.t[:n, :], in_=tmp64.t[:n, :], axis=AX.X)
                k.tt(tmp64, tmp64.t[:n, :], mc, mc.t[:n, 0:64], m12[kk], m12[kk].t[:n, :], ALU.mult)
                k.op('dve', nc.vector.reduce_sum, outs=[keyf], ins=[tmp64], out=keyf.t[:n, :], in_=tmp64.t[:n, :], axis=AX.X)
                k.tt(keyf, keyf.t[:n, :], keyf, keyf.t[:n, :], rk, rk.t[:n, :], ALU.add)
                k.ts(ov, ov.t[:n, :], rk, rk.t[:n, :], None, float(CAP), ALU.is_ge, sc2=float(4 * self.NROW), op2=ALU.mult)
                k.tt(keyf, keyf.t[:n, :], keyf, keyf.t[:n, :], ov, ov.t[:n, :], ALU.add)
                k.cp('dve', self.key[kk], self.key[kk].t[:n, ti:ti + 1], keyf, keyf.t[:n, :])
                k.idma(f'sc{kk}', self.s_xs.t[:, :], self.key[kk].t[:n, ti:ti + 1], hhi.t[:n, :], None, self.NROW,
                       outs=[self.s_xs], ins=[hhi, self.key[kk]])

    def phase_e(self):
        nc, k = self.nc, self.k
        CAP, NE = self.CAP, self.NE
        NST = CAP // 128
        wg = [k.sb(f"wg{i}", [128, KC, 512], BF16) for i in range(2)]
        wu = [k.sb(f"wu{i}", [128, KC, 512], BF16) for i in range(2)]
        wd = [k.sb(f"wd{i}", [128, 4, D], BF16) for i in range(2)]
        xs_ = [k.sb(f"xse{i}", [128, D], BF16) for i in range(2)]
        xT = k.sb("xTe", [128, KC, CAP], BF16)
        sg = k.sb("sg", [128, CAP], BF16)
        hT = k.sb("hTe", [128, 4, CAP], BF16)
        eo = [k.sb(f"eo{i}", [128, D], F32) for i in range(2)]
        ptb = [k.ps(f"pte{i}", [128, 8, 128], BF16) for i in range(2)]
        pg_ = [k.ps(f"pge{i}", [128, 512], F32) for i in range(2)]
        pu_ = [k.ps(f"pue{i}", [128, 512], F32) for i in range(2)]
        pd_ = [k.ps(f"pde{i}", [128, 512], F32) for i in range(2)]
        cnt = [0]
        for e in range(NE):
            s_ = e % 2
            gv = self.w_gate[e].rearrange("(kc p) c -> p kc c", p=128)
            uv = self.w_up[e].rearrange("(kc p) c -> p kc c", p=128)
            dv = self.w_down[e].rearrange("(fc p) c -> p fc c", p=128)
            k.dma_group('pool', f'wg{s_}', [(wg[s_].t[:, q * 8:(q + 1) * 8, :], gv[:, q * 8:(q + 1) * 8, :]) for q in range(2)],
                        outs=[wg[s_]])
            k.dma_group('pool', f'wu{s_}', [(wu[s_].t[:, q * 8:(q + 1) * 8, :], uv[:, q * 8:(q + 1) * 8, :]) for q in range(2)],
                        outs=[wu[s_]])
            k.dma_group('pool', f'wd{s_}', [(wd[s_].t[:, q * 2:(q + 1) * 2, :], dv[:, q * 2:(q + 1) * 2, :]) for q in range(2)],
                        outs=[wd[s_]])
            for st in range(NST):
                r0 = e * CAP + st * 128
                x_ = xs_[st % 2]
                k.dma('sp', f'xse{st % 2}', x_.t[:, :], self.s_xs.t[r0:r0 + 128, :], outs=[x_], ins=[self.s_xs])
                for q in range(2):
                    pb_ = ptb[q]
                    for j in range(8):
                        kc = q * 8 + j
                        k.tr(pb_.t[:, j, :], x_.t[:, kc * 128:(kc + 1) * 128], self.ident_b.t[:, :], outs=[pb_],
                             ins=[x_, self.ident_b])
                    k.cp('dve' if q == 0 else 'act', xT, xT.t[:, q * 8:(q + 1) * 8, st * 128:(st + 1) * 128], pb_,
                         pb_.t[:, :, :])
            for fc in range(4):
                pg, pu = pg_[fc % 2], pu_[fc % 2]
                k.mm(pg.t[:, :CAP], [(wg[s_].t[:, kc, fc * 128:(fc + 1) * 128], xT.t[:, kc, :]) for kc in range(KC)],
                     outs=[pg], ins=[wg[s_], xT])
                k.mm(pu.t[:, :CAP], [(wu[s_].t[:, kc, fc * 128:(fc + 1) * 128], xT.t[:, kc, :]) for kc in range(KC)],
                     outs=[pu], ins=[wu[s_], xT])
                k.act(sg, sg.t[:, :], pg, pg.t[:, :CAP], AF.Silu)
                k.tt(hT, hT.t[:, fc, :], sg, sg.t[:, :], pu, pu.t[:, :CAP], ALU.mult)
            for st in range(NST):
                cnt[0] += 1
                o_ = eo[cnt[0] % 2]
                for cg in range(4):
                    pd = pd_[cg % 2]
                    k.mm(pd.t[:, :], [(hT.t[:, fc, st * 128:(st + 1) * 128], wd[s_].t[:, fc, cg * 512:(cg + 1) * 512])
                                      for fc in range(4)], outs=[pd], ins=[hT, wd[s_]])
                    k.cp('act' if cg % 2 else 'dve', o_, o_.t[:, cg * 512:(cg + 1) * 512], pd, pd.t[:, :])
                r0 = e * CAP + st * 128
                k.dma('sp', f'eo{cnt[0] % 2}', self.s_eo.t[r0:r0 + 128, :], o_.t[:, :], ins=[o_], outs=[self.s_eo])

    def phase_f(self):
        nc, k = self.nc, self.k
        T, NTL = self.T, self.NT
        gfin = k.sb("gfin", [128, D], F32)
        k.dma('sp', 'gfin', gfin.t[:, :], self.fin_g.ap(), outs=[gfin])
        g1 = [k.sb(f"g1_{i}", [128, D], F32) for i in range(2)]
        g2 = [k.sb(f"g2_{i}", [128, D], F32) for i in range(2)]
        x1 = [k.sb(f"x1f{i}", [128, D], F32) for i in range(2)]
        jk = k.sb("jkf", [128, D], BF16)
        ss = k.sb("ssf", [128, 1], F32)
        rs = k.sb("rsf", [128, 1], F32)
        for ti in range(NTL):
            t0 = ti * 128
            n = min(128, T - t0)
            s_ = ti % 2
            k.op('dve', nc.vector.memset, outs=[g1[s_]], ap=g1[s_].t[:n, :], constant=0.0)
            k.op('dve', nc.vector.memset, outs=[g2[s_]], ap=g2[s_].t[:n, :], constant=0.0)
            k.idma(f'g1_{s_}', g1[s_].t[:n, :], None, self.s_eo.t[:, :], self.key[0].t[:n, ti:ti + 1], self.NROW,
                   outs=[g1[s_]], ins=[self.s_eo, self.key[0]])
            k.idma(f'g2_{s_}', g2[s_].t[:n, :], None, self.s_eo.t[:, :], self.key[1].t[:n, ti:ti + 1], self.NROW,
                   outs=[g2[s_]], ins=[self.s_eo, self.key[1]])
            k.dma('sp', f'x1f{s_}', x1[s_].t[:n, :], self.s_x1.t[t0:t0 + n, :], outs=[x1[s_]], ins=[self.s_x1])
            k.stt(x1[s_], x1[s_].t[:n, :], g1[s_], g1[s_].t[:n, :], self.wgt[0], self.wgt[0].t[:n, ti:ti + 1], x1[s_],
                  x1[s_].t[:n, :], ALU.mult, ALU.add)
            k.stt(x1[s_], x1[s_].t[:n, :], g2[s_], g2[s_].t[:n, :], self.wgt[1], self.wgt[1].t[:n, ti:ti + 1], x1[s_],
                  x1[s_].t[:n, :], ALU.mult, ALU.add)
            k.act(jk, jk.t[:n, :], x1[s_], x1[s_].t[:n, :], AF.Square, accum=(ss, ss.t[:n, :]))
            k.ts(rs, rs.t[:n, :], ss, ss.t[:n, :], None, 1.0 / D, ALU.mult, sc2=EPS, op2=ALU.add)
            k.act(rs, rs.t[:n, :], rs, rs.t[:n, :], AF.Sqrt)
            k.op('dve', nc.vector.reciprocal, outs=[rs], ins=[rs], out=rs.t[:n, :], in_=rs.t[:n, :])
            k.stt(g1[s_], g1[s_].t[:n, :], x1[s_], x1[s_].t[:n, :], rs, rs.t[:n, 0:1], gfin, gfin.t[:n, :], ALU.mult,
                  ALU.mult)
            self.out_toks.append(k.dma('sp', f'yo{s_}', self.o_y[t0:t0 + n, :], g1[s_].t[:n, :], ins=[g1[s_]]))


def host_prep(inp, c, SEQ, PAST, NG=8, only=None):
    b = c % 4
    f = np.float32
    m = {}
    m["x_all"] = np.ascontiguousarray(np.concatenate(
        [inp["x_prompt"][b, :SEQ], inp["x_sample"][2 * c], inp["x_sample"][2 * c + 1]], axis=0), dtype=f)
    m["w_in"] = np.ascontiguousarray(inp["w_in"][0], dtype=f)
    m["attn_g"] = np.ascontiguousarray(inp["attn_norm_g"][0].reshape(KC, 128).T, dtype=f)
    m["qn_g"] = np.ascontiguousarray(inp["q_norm_g"][0].reshape(4, 128).T, dtype=f)
    m["kvn_g"] = np.ascontiguousarray(inp["kv_norm_g"][0].reshape(4, 128).T, dtype=f)
    m["ident"] = np.eye(128, dtype=f)
    pos = np.concatenate([np.arange(SEQ), PAST + np.arange(16), PAST + np.arange(16)])
    m["cosT"], m["sinT"] = rope_tables(pos)
    m["conv_w"] = np.ascontiguousarray(inp["conv_w"][0].reshape(4, 48, 128).transpose(2, 1, 0), dtype=f)
    m["conv0"] = np.ascontiguousarray(
        inp["state_conv"][0, 2 * c:2 * c + 2].reshape(2, 3, 48, 128).transpose(3, 2, 0, 1), dtype=f)
    m["w_uq"] = np.ascontiguousarray(inp["w_uq"][0].reshape(QL, NH * 192), dtype=f)
    m["w_uk"] = np.ascontiguousarray(inp["w_uk"][0].reshape(KVL, NH * 128), dtype=f)
    m["w_uv"] = np.ascontiguousarray(inp["w_uv"][0].reshape(KVL, NH * 128), dtype=f)
    m["cache_ckv"] = np.ascontiguousarray(inp["cache_ckv"][0, 2 * c:2 * c + 2, :PAST], dtype=f)
    m["cache_kr"] = np.ascontiguousarray(inp["cache_k_rope"][0, 2 * c:2 * c + 2, :PAST], dtype=f)
    m["gpar"] = np.ascontiguousarray(np.concatenate([
        np.broadcast_to(inp["a_log"][0][None, :], (128, 16)), np.broadcast_to(inp["dt_bias"][0][None, :], (128, 16)),
        inp["gdn_norm_g"][0].reshape(128, 1)], axis=1), dtype=f)
    gmk = np.zeros((2, 128, 514), f)
    for v, (n, L) in enumerate(((128, 64), (32, 16))):
        idx = np.arange(n)
        same = (idx[:, None] // L) == (idx[None, :] // L)
        tri = same & (idx[:, None] <= idx[None, :])
        gmk[v, :n, 0:n] = tri
        gmk[v, :, 128:256] = -30000.0
        gmk[v, :n, 128:128 + n] = np.where(tri, 0.0, -30000.0)
        gmk[v, :n, 256:256 + n] = same & (idx[:, None] < idx[None, :])
        gmk[v, :n, 384] = (idx // L) == 0
        gmk[v, :n, 385] = (idx // L) == 1
        gmk[v, :n, 386:386 + n] = same
    m["gmask"] = gmk
    m["state_gdn"] = np.ascontiguousarray(inp["state_gdn"][0, 2 * c:2 * c + 2], dtype=f)
    dm = np.zeros((128, 128), f)
    dm[:64, 64:] = -30000.0
    m["dmask"] = dm
    NE = NG * 8
    if "w_gate" not in inp:
        return m
    m["w_out"] = np.ascontiguousarray(inp["w_out"][0], dtype=f)
    m["ffn_g"] = np.ascontiguousarray(np.broadcast_to(inp["ffn_norm_g"][0][None, :], (128, D)), dtype=f)
    m["fin_g"] = np.ascontiguousarray(np.broadcast_to(inp["final_norm_g"][None, :], (128, D)), dtype=f)
    wr = inp["w_router"][0][:NG].transpose(1, 0, 2).reshape(D, NG * 8)
    wrt = np.zeros((D, 72), f)
    wrt[:, :NG] = inp["w_group"][0][:, :NG]
    wrt[:, 8:8 + NG * 8] = wr
    m["w_rt"] = wrt
    rbv = np.full((72,), -30000.0, f)
    rbv[:NG] = inp["b_group"][0][:NG]
    rbv[8:8 + NG * 8] = inp["b_router"][0][:NG].reshape(-1)
    m["rbias"] = np.ascontiguousarray(np.broadcast_to(rbv[None, :], (128, 72)), dtype=f)
    mcst = np.zeros((128, 192), f)
    mcst[:, :64] = (np.arange(64) * 256)[None, :]
    ii = np.arange(128)
    mcst[:, 64:] = (ii[:, None] < ii[None, :])
    m["mconst"] = mcst
    m["w_gate"] = inp["w_gate"][0][:NE]
    m["w_up"] = inp["w_up"][0][:NE]
    m["w_down"] = inp["w_down"][0][:NE]
    return m


SEQ_FULL, PAST_FULL = 4096, 4096
_PROG = {}


def kernel(**inputs):
    inp = {k_: np.asarray(v) for k_, v in inputs.items()}
    phases = "ABCD"
    if phases not in _PROG:
        _PROG[phases] = Prog(SEQ_FULL, PAST_FULL, phases=phases)
    P = _PROG[phases]
    maps = []
    for c in range(8):
        m = host_prep(inp, c, SEQ_FULL, PAST_FULL)
        maps.append({k_: v for k_, v in m.items() if k_ in P.inp})
    res = run_bass_kernel_spmd(P.nc, maps, core_ids=list(range(8))).results
    S = SEQ_FULL
    f = np.float32
    y_prompt = np.stack([res[b]["o_y"][:S] for b in range(4)])
    y_sample = np.stack([res[c]["o_y"][S + 16 * i:S + 16 * (i + 1)] for c in range(8) for i in range(2)])
    ckv_p = np.stack([res[b]["o_ckv"][:S] for b in range(4)])[None]
    kr_p = np.stack([res[b]["o_kr"][:S] for b in range(4)])[None]
    gdn_p = np.stack([res[b]["o_gdn"][0] for b in range(4)])[None]
    conv_p = np.stack([res[b]["o_conv"][0] for b in range(4)])[None]
    ckv_s = np.stack([res[c]["o_ckv"][S + 16 * i:S + 16 * (i + 1)] for c in range(8) for i in range(2)])[None]
    kr_s = np.stack([res[c]["o_kr"][S + 16 * i:S + 16 * (i + 1)] for c in range(8) for i in range(2)])[None]
    gdn_s = np.stack([res[c]["o_gdn"][1 + i] for c in range(8) for i in range(2)])[None]
    conv_s = np.stack([res[c]["o_conv"][1 + i] for c in range(8) for i in range(2)])[None]
    return tuple(np.ascontiguousarray(a, dtype=f) for a in
                 (y_prompt, y_sample, ckv_p, kr_p, gdn_p, conv_p, ckv_s, kr_s, gdn_s, conv_s))
```
